# Optimizing a Trainium2 kernel written in Bass

```python
import math
import jax
import jax.numpy as jnp
from jax import lax
import numpy as np

D_MODEL = 1024
BATCH = 8
SEQ = 8192
DEPTH = 2

GRID_W = 64
CTX_LEN = 256

N_MIXERS = 2
MIXER_DELTA = 0
MIXER_HYENA = 1
N_DELTA_LAYERS = (DEPTH - MIXER_DELTA + N_MIXERS - 1) // N_MIXERS
N_HYENA_LAYERS = (DEPTH - MIXER_HYENA + N_MIXERS - 1) // N_MIXERS

EPS = 1e-6

DN_HEAD_DIM = 128
DN_HEADS = D_MODEL // DN_HEAD_DIM
DN_INNER = DN_HEADS * DN_HEAD_DIM
DN_CONV = 5
DN_CHUNK = 64
DN_CHUNK_LOG2 = 6
DN_PROJ = 4 * DN_INNER + 4 * DN_HEADS

HY_WIDTH = D_MODEL
HY_SHORT = 3
HY_BANDS = 16
HY_EMB = 1 + 2 * HY_BANDS
HY_FF = 64
HY_FAST = 0.3
HY_SLOW = 1.5
HY_TARGET = 1e-2

PEER_HEADS = 8
PEER_NKEYS = 128
PEER_EXPERTS = PEER_NKEYS * PEER_NKEYS
PEER_DK = 256
PEER_TOPK = 16
PEER_BLOCK = 128

kernel_name = "hybrid_deltanet_hyena_peer_dit"


def _rmsnorm(x, g):
    xf = x.astype(jnp.float32)
    y = xf * lax.rsqrt(jnp.mean(xf * xf, axis=-1, keepdims=True) + EPS)
    return (y * g.astype(jnp.float32)).astype(x.dtype)


def _modulate(xn, shift, scale):
    return xn * (1 + scale) + shift


def _l2norm(x):
    xf = x.astype(jnp.float32)
    return xf * lax.rsqrt(jnp.sum(xf * xf, axis=-1, keepdims=True) + EPS)


def _rev(t):
    return jnp.flip(t, axis=2)


def _short_conv(x, w, n_rows):
    B, L, C = x.shape
    K = w.shape[0]
    xr = x.reshape(B * n_rows, L // n_rows, C)
    y = lax.conv_general_dilated(
        xr, w.astype(x.dtype)[:, None, :], window_strides=(1,),
        padding=[((K - 1) // 2, (K - 1) // 2)],
        dimension_numbers=('NWC', 'WIO', 'NWC'), feature_group_count=C)
    return y.reshape(B, L, C)


def _dn_inputs(hn, n_rows, w_in, conv_w, a_log, dt_bias):
    B, L, _ = hn.shape
    z = hn @ w_in
    qkv = jax.nn.silu(_short_conv(z[..., :3 * DN_INNER], conv_w, n_rows))
    gate = z[..., 3 * DN_INNER:4 * DN_INNER]
    ba = z[..., 4 * DN_INNER:].astype(jnp.float32).reshape(B, L, 2, 2, DN_HEADS)
    qkv = qkv.reshape(B, L, 3, DN_HEADS, DN_HEAD_DIM).transpose(2, 0, 3, 1, 4)
    q = _l2norm(qkv[0]) * (DN_HEAD_DIM ** -0.5)
    k = _l2norm(qkv[1])
    v = qkv[2].astype(jnp.float32)
    beta = jax.nn.sigmoid(ba[:, :, 0]).transpose(0, 2, 3, 1)
    g = (-jnp.exp(a_log.astype(jnp.float32)) *
         jax.nn.softplus(ba[:, :, 1] + dt_bias.astype(jnp.float32))).transpose(0, 2, 3, 1)
    return q, k, v, beta, g, gate


def _unit_lower_inverse(a):
    eye = jnp.eye(a.shape[-1], dtype=a.dtype)
    t = eye - a
    p = a
    for _ in range(DN_CHUNK_LOG2 - 1):
        p = p @ p
        t = t @ (eye + p)
    return t


def _gated_delta_chunked(k, v, beta, g, s0, q=None):
    f32 = jnp.float32
    with_out = q is not None
    B, H, L, DK = k.shape
    DV = v.shape[-1]
    C = DN_CHUNK
    n = L // C
    k = k.astype(f32).reshape(B, H, n, C, DK)
    v = v.astype(f32).reshape(B, H, n, C, DV)
    beta = beta.astype(f32).reshape(B, H, n, C)
    G = jnp.cumsum(g.astype(f32).reshape(B, H, n, C), axis=-1)
    incl = jnp.tril(jnp.ones((C, C), bool))
    strict = jnp.tril(jnp.ones((C, C), bool), -1)
    dmat = jnp.exp(jnp.where(incl, G[..., :, None] - G[..., None, :], -jnp.inf))
    kb = k * beta[..., None]
    a = jnp.where(strict, jnp.einsum('bhnid,bhnjd->bhnij', kb, k) * dmat, 0.0)
    t = _unit_lower_inverse(a)
    u = t @ (v * beta[..., None])
    w = t @ (kb * jnp.exp(G)[..., None])
    kd = k * jnp.exp(G[..., -1:] - G)[..., None]
    gl = jnp.exp(G[..., -1])
    xs = (jnp.moveaxis(w, 2, 0), jnp.moveaxis(u, 2, 0), jnp.moveaxis(kd, 2, 0), jnp.moveaxis(gl, 2, 0))
    if with_out:
        q = q.astype(f32).reshape(B, H, n, C, DK)
        qa = jnp.einsum('bhnid,bhnjd->bhnij', q, k) * dmat
        qg = q * jnp.exp(G)[..., None]
        xs = xs + (jnp.moveaxis(qg, 2, 0), jnp.moveaxis(qa, 2, 0))

    def step(s, inp):
        w_c, u_c, kd_c, gl_c = inp[:4]
        v_new = u_c - jnp.einsum('bhck,bhkv->bhcv', w_c, s)
        s_next = s * gl_c[..., None, None] + jnp.einsum('bhck,bhcv->bhkv', kd_c, v_new)
        if not with_out:
            return s_next, None
        qg_c, qa_c = inp[4:]
        o_c = jnp.einsum('bhck,bhkv->bhcv', qg_c, s) + jnp.einsum('bhij,bhjv->bhiv', qa_c, v_new)
        return s_next, o_c

    s_fin, o = lax.scan(step, s0.astype(f32), xs)
    if not with_out:
        return None, s_fin
    return jnp.moveaxis(o, 0, 2).reshape(B, H, L, DV), s_fin


def _gated_out(o, gate, g_norm, w_out):
    B, H, L, DV = o.shape
    o = jnp.swapaxes(o, 1, 2)
    on = o * lax.rsqrt(jnp.mean(o * o, axis=-1, keepdims=True) + EPS) * g_norm.astype(jnp.float32)
    y = on * jax.nn.silu(gate.astype(jnp.float32)).reshape(B, L, H, DV)
    return y.reshape(B, L, H * DV).astype(gate.dtype) @ w_out


def _deltanet(hn, hcn, n_rows, w_in, conv_w, a_log, dt_bias, o_norm_g, w_out, ctx_out):
    q, k, v, beta, g, gate = _dn_inputs(hn, n_rows, w_in, conv_w, a_log, dt_bias)
    qc, kc, vc, betac, gc, gatec = _dn_inputs(hcn, 1, w_in, conv_w, a_log, dt_bias)
    B = hn.shape[0]
    s0 = jnp.zeros((B, DN_HEADS, DN_HEAD_DIM, DN_HEAD_DIM), jnp.float32)
    oc_f, sc_f = _gated_delta_chunked(kc, vc, betac[:, 0], gc[:, 0], s0, qc if ctx_out else None)
    oc_b, sc_b = _gated_delta_chunked(_rev(kc), _rev(vc), _rev(betac[:, 1]), _rev(gc[:, 1]), s0,
                                      _rev(qc) if ctx_out else None)
    o_f, _ = _gated_delta_chunked(k, v, beta[:, 0], g[:, 0], sc_f, q)
    o_b, _ = _gated_delta_chunked(_rev(k), _rev(v), _rev(beta[:, 1]), _rev(g[:, 1]), sc_b, _rev(q))
    y = _gated_out(o_f + _rev(o_b), gate, o_norm_g, w_out)
    yc = _gated_out(oc_f + _rev(oc_b), gatec, o_norm_g, w_out) if ctx_out else None
    return y, yc


def _hyena_filters(L, w1, b1, w2, b2, w3, b3, w4, freq):
    f32 = jnp.float32
    D = w4.shape[-1] // 4
    pos = jnp.arange(L, dtype=f32)
    t = pos / max(L - 1, 1)
    ang = (2 * math.pi * pos / L)[:, None] * jnp.linspace(1e-4, HY_BANDS - 1, HY_BANDS, dtype=f32)[None]
    z = jnp.concatenate([t[:, None], jnp.cos(ang), -jnp.sin(ang)], axis=-1)
    fr = freq.astype(f32)
    hid = jnp.sin(fr * (z @ w1.astype(f32) + b1.astype(f32)))
    hid = jnp.sin(fr * (hid @ w2.astype(f32) + b2.astype(f32)))
    hid = jnp.sin(fr * (hid @ w3.astype(f32) + b3.astype(f32)))
    h = hid @ w4.astype(f32)
    deltas = jnp.abs(jnp.linspace(math.log(HY_TARGET) / HY_FAST, math.log(HY_TARGET) / HY_SLOW, D, dtype=f32))
    decay = jnp.exp(-t[:, None] * deltas[None])
    return (h.reshape(L, 2, 2, D) * decay[:, None, None, :]).transpose(1, 2, 0, 3)


def _long_conv(u, hf, hb, d):
    B, L, D = u.shape
    filt2 = jnp.concatenate([hf, jnp.zeros((1, D), jnp.float32), jnp.flip(hb[1:], axis=0)], axis=0)
    uf = jnp.fft.rfft(u.astype(jnp.float32), n=2 * L, axis=1)
    y = jnp.fft.irfft(uf * jnp.fft.rfft(filt2, axis=0)[None], n=2 * L, axis=1)[:, :L]
    return (y + u.astype(jnp.float32) * d.astype(jnp.float32)).astype(u.dtype)


def _hyena(hn, n_rows, w_in, conv_w, conv_b, f_w1, f_b1, f_w2, f_b2, f_w3, f_b3, f_w4, freq, skip, w_out):
    B, L, _ = hn.shape
    z = _short_conv(hn @ w_in, conv_w, n_rows) + conv_b.astype(hn.dtype)
    v, x1, x2 = jnp.split(z, 3, axis=-1)
    filt = _hyena_filters(L, f_w1, f_b1, f_w2, f_b2, f_w3, f_b3, f_w4, freq)
    z1 = x1 * _long_conv(v, filt[0, 0], filt[0, 1], skip[0])
    z2 = x2 * _long_conv(z1, filt[1, 0], filt[1, 1], skip[1])
    return z2 @ w_out


def _peer(x, w_q, sub_keys, u_tab, v_tab):
    B, L, D = x.shape
    T = B * L
    xt = x.reshape(T, D)
    q = (xt @ w_q).astype(jnp.float32).reshape(T, PEER_HEADS, 2, PEER_DK // 2)
    s = jnp.einsum('thpd,hpkd->thpk', q, sub_keys.astype(jnp.float32))
    s1, i1 = lax.top_k(s[:, :, 0], PEER_TOPK)
    s2, i2 = lax.top_k(s[:, :, 1], PEER_TOPK)
    n_cand = PEER_TOPK * PEER_TOPK
    cand_s = (s1[..., :, None] + s2[..., None, :]).reshape(T, PEER_HEADS, n_cand)
    cand_i = (i1[..., :, None] * PEER_NKEYS + i2[..., None, :]).reshape(T, PEER_HEADS, n_cand)
    top_s, pos = lax.top_k(cand_s, PEER_TOPK)
    idx = jnp.take_along_axis(cand_i, pos, axis=-1)
    gate = jax.nn.softmax(top_s, axis=-1)
    nb = T // PEER_BLOCK

    def expert_block(args):
        xb, ib, gb = args
        hid = jnp.einsum('td,thkd->thk', xb, u_tab[ib]).astype(jnp.float32)
        coef = (jax.nn.gelu(hid, approximate=False) * gb).astype(xb.dtype)
        return jnp.einsum('thk,thkd->td', coef, v_tab[ib])

    y = lax.map(expert_block, (xt.reshape(nb, PEER_BLOCK, D),
                               idx.reshape(nb, PEER_BLOCK, PEER_HEADS, PEER_TOPK),
                               gate.reshape(nb, PEER_BLOCK, PEER_HEADS, PEER_TOPK)))
    return y.reshape(B, L, D)


def setup_inputs(seed: int = 0) -> dict:
    key = jax.random.key(seed)
    ks = iter(jax.random.split(key, 32))
    f32 = jnp.float32
    D = D_MODEL

    def nrm(shape, scale):
        return jax.random.normal(next(ks), shape, f32) * scale

    x = nrm((BATCH, SEQ, D), 1.0)
    c = nrm((BATCH, D), 1.0)
    ctx = nrm((BATCH, CTX_LEN, D), 1.0)
    c_ctx = nrm((D,), 1.0)
    ada_w = nrm((DEPTH, D, 6 * D), 0.5 * D ** -0.5)
    ada_b = nrm((DEPTH, 6 * D), 0.02)
    norm_g = 1.0 + nrm((DEPTH, 2, D), 0.02)
    dn_w_in = nrm((N_DELTA_LAYERS, D, DN_PROJ), D ** -0.5)
    dn_conv_w = nrm((N_DELTA_LAYERS, DN_CONV, 3 * DN_INNER), DN_CONV ** -0.5)
    dn_a_log = jnp.log(jax.random.uniform(next(ks), (N_DELTA_LAYERS, 2, DN_HEADS), f32, 1.0, 16.0))
    dt = jnp.exp(jax.random.uniform(next(ks), (N_DELTA_LAYERS, 2, DN_HEADS), f32,
                                    math.log(1e-3), math.log(1e-1)))
    dn_dt_bias = dt + jnp.log(-jnp.expm1(-dt))
    dn_norm_g = 1.0 + nrm((N_DELTA_LAYERS, DN_HEAD_DIM), 0.02)
    dn_w_out = nrm((N_DELTA_LAYERS, DN_INNER, D), DN_INNER ** -0.5)
    hy_w_in = nrm((N_HYENA_LAYERS, D, 3 * HY_WIDTH), D ** -0.5)
    hy_conv_w = nrm((N_HYENA_LAYERS, HY_SHORT, 3 * HY_WIDTH), HY_SHORT ** -0.5)
    hy_conv_b = nrm((N_HYENA_LAYERS, 3 * HY_WIDTH), 0.02)
    hy_f_w1 = nrm((N_HYENA_LAYERS, HY_EMB, HY_FF), HY_EMB ** -0.5)
    hy_f_b1 = nrm((N_HYENA_LAYERS, HY_FF), 0.02)
    hy_f_w2 = nrm((N_HYENA_LAYERS, HY_FF, HY_FF), HY_FF ** -0.5)
    hy_f_b2 = nrm((N_HYENA_LAYERS, HY_FF), 0.02)
    hy_f_w3 = nrm((N_HYENA_LAYERS, HY_FF, HY_FF), HY_FF ** -0.5)
    hy_f_b3 = nrm((N_HYENA_LAYERS, HY_FF), 0.02)
    hy_f_w4 = nrm((N_HYENA_LAYERS, HY_FF, 4 * HY_WIDTH), 0.02 * HY_FF ** -0.5)
    hy_freq = 1.0 + nrm((N_HYENA_LAYERS, HY_FF), 0.02)
    hy_skip = nrm((N_HYENA_LAYERS, 2, HY_WIDTH), 1.0)
    hy_w_out = nrm((N_HYENA_LAYERS, HY_WIDTH, D), HY_WIDTH ** -0.5)
    peer_w_q = nrm((DEPTH, D, PEER_HEADS * PEER_DK), D ** -0.5)
    peer_keys = nrm((DEPTH, PEER_HEADS, 2, PEER_NKEYS, PEER_DK // 2), (PEER_DK // 2) ** -0.5)
    peer_u = nrm((DEPTH, PEER_EXPERTS, D), D ** -0.5)
    peer_v = nrm((DEPTH, PEER_EXPERTS, D), PEER_HEADS ** -0.5)
    final_g = 1.0 + nrm((D,), 0.02)
    return {"x": x, "c": c, "ctx": ctx, "c_ctx": c_ctx,
            "ada_w": ada_w, "ada_b": ada_b, "norm_g": norm_g,
            "dn_w_in": dn_w_in, "dn_conv_w": dn_conv_w, "dn_a_log": dn_a_log, "dn_dt_bias": dn_dt_bias,
            "dn_norm_g": dn_norm_g, "dn_w_out": dn_w_out,
            "hy_w_in": hy_w_in, "hy_conv_w": hy_conv_w, "hy_conv_b": hy_conv_b,
            "hy_f_w1": hy_f_w1, "hy_f_b1": hy_f_b1, "hy_f_w2": hy_f_w2, "hy_f_b2": hy_f_b2,
            "hy_f_w3": hy_f_w3, "hy_f_b3": hy_f_b3, "hy_f_w4": hy_f_w4, "hy_freq": hy_freq,
            "hy_skip": hy_skip, "hy_w_out": hy_w_out,
            "peer_w_q": peer_w_q, "peer_keys": peer_keys, "peer_u": peer_u, "peer_v": peer_v,
            "final_g": final_g}


def reference(x, c, ctx, c_ctx, ada_w, ada_b, norm_g,
              dn_w_in, dn_conv_w, dn_a_log, dn_dt_bias, dn_norm_g, dn_w_out,
              hy_w_in, hy_conv_w, hy_conv_b, hy_f_w1, hy_f_b1, hy_f_w2, hy_f_b2,
              hy_f_w3, hy_f_b3, hy_f_w4, hy_freq, hy_skip, hy_w_out,
              peer_w_q, peer_keys, peer_u, peer_v, final_g):
    f32 = jnp.float32
    B, L, D = x.shape
    rows = L // GRID_W
    silu_c = jax.nn.silu(c.astype(f32))
    silu_cc = jax.nn.silu(c_ctx.astype(f32))
    h, hc = x, ctx
    for i in range(DEPTH):
        kind = i % N_MIXERS
        j = i // N_MIXERS
        ctx_out = any(l % N_MIXERS == MIXER_DELTA for l in range(i + 1, DEPTH))
        ctx_in = kind == MIXER_DELTA or ctx_out
        w_ada = ada_w[i].astype(f32)
        b_ada = ada_b[i].astype(f32)
        mod = (silu_c @ w_ada + b_ada).astype(h.dtype).reshape(B, 6, 1, D)
        sh1, sc1, gt1, sh2, sc2, gt2 = [mod[:, m] for m in range(6)]
        hn = _modulate(_rmsnorm(h, norm_g[i, 0]), sh1, sc1)
        hcn = None
        if ctx_in:
            modc = (silu_cc @ w_ada + b_ada).astype(hc.dtype).reshape(6, D)
            hcn = _modulate(_rmsnorm(hc, norm_g[i, 0]), modc[0], modc[1])
        if kind == MIXER_DELTA:
            y, yc = _deltanet(hn, hcn, rows, dn_w_in[j], dn_conv_w[j], dn_a_log[j], dn_dt_bias[j],
                              dn_norm_g[j], dn_w_out[j], ctx_out)
        else:
            hy_args = (hy_w_in[j], hy_conv_w[j], hy_conv_b[j], hy_f_w1[j], hy_f_b1[j], hy_f_w2[j],
                       hy_f_b2[j], hy_f_w3[j], hy_f_b3[j], hy_f_w4[j], hy_freq[j], hy_skip[j], hy_w_out[j])
            y = _hyena(hn, rows, *hy_args)
            yc = _hyena(hcn, 1, *hy_args) if ctx_out else None
        h = h + gt1 * y
        peer_args = (peer_w_q[i], peer_keys[i], peer_u[i], peer_v[i])
        h = h + gt2 * _peer(_modulate(_rmsnorm(h, norm_g[i, 1]), sh2, sc2), *peer_args)
        if ctx_out:
            hc = hc + modc[2] * yc
            hc = hc + modc[5] * _peer(_modulate(_rmsnorm(hc, norm_g[i, 1]), modc[3], modc[4]), *peer_args)
    return _rmsnorm(h, final_g)
```

```python
import contextlib
import math
import numpy as np
import ml_dtypes
import concourse.bass as bass
import concourse.mybir as mybir
from concourse.bass_utils import run_bass_kernel_spmd

F32 = mybir.dt.float32
BF16 = mybir.dt.bfloat16
I32 = mybir.dt.int32
U32 = mybir.dt.uint32
AF = mybir.ActivationFunctionType
ALU = mybir.AluOpType
AX = mybir.AxisListType

N_DMA_SEMS = 12
L = 8192
D = 1024
CTX = 256
TT = L + CTX
EPS = 1e-6


class Buf:
    __slots__ = ("name", "w", "r", "psum", "acc")

    def __init__(self, name="", psum=False):
        self.name = name
        self.w = None
        self.r = []
        self.psum = psum
        self.acc = {}


class Op:
    __slots__ = ("eng", "fn", "deps", "is_dma", "signal", "val", "sem", "idx")


class Prog:
    ENGS = ("pe", "dve", "act", "pool", "sp")
    ENGOBJ = {"pe": "tensor", "dve": "vector", "act": "scalar", "pool": "gpsimd", "sp": "sync"}

    def __init__(self, nc, stack):
        self.nc = nc
        self.nops = 0
        self.cur = []
        self.sems = {e: stack.enter_context(nc.semaphore(f"s_{e}")) for e in ("pe", "dve", "act", "pool")}
        self.dma_sems = {e: [stack.enter_context(nc.semaphore(f"d_{e}_{i}")) for i in range(N_DMA_SEMS)]
                         for e in ("sp", "act", "pool")}
        self.cnt = {e: 0 for e in self.ENGS}
        self.dma_k = {e: 0 for e in self.ENGS}
        self.dma_vals = {e: [0] * N_DMA_SEMS for e in self.ENGS}
        self.seen = {e: {} for e in self.ENGS}
        self.last_op = {e: None for e in self.ENGS}
        self.prev_dmas = []
        self.pending = {}
        self.n_waits = 0

    def op(self, eng, fn, reads=(), writes=(), is_dma=False):
        o = Op()
        o.eng = eng
        o.fn = fn
        o.is_dma = is_dma
        o.signal = False
        o.val = None
        o.sem = None
        o.idx = self.nops
        self.nops += 1
        deps = set()
        for b in reads:
            if b.w is not None and b.w.fn is not None:
                deps.add(b.w)
        for b in writes:
            if b.w is not None and b.w.fn is not None:
                deps.add(b.w)
            for r in b.r:
                if r.fn is not None:
                    deps.add(r)
        for b in tuple(reads) + tuple(writes):
            if b.psum:
                for x, o2 in b.acc.items():
                    if x != eng and o2.fn is not None:
                        deps.add(o2)
                b.acc[eng] = o
        if eng in self.pending:
            deps |= self.pending.pop(eng)
        o.deps = deps
        for b in reads:
            b.r.append(o)
        for b in writes:
            b.w = o
            b.r = []
        self.cur.append(o)
        return o

    def dma(self, eng, out, in_, reads=(), writes=(), **kw):
        return self.op(eng, lambda e: e.dma_start(out=out, in_=in_, **kw), reads, writes, is_dma=True)

    def phase_end(self):
        nc = self.nc
        by_eng = {e: [] for e in self.ENGS}
        for o in self.cur:
            by_eng[o.eng].append(o)
        for o in self.cur:
            best = {}
            for d in o.deps:
                if d.is_dma or (d.eng == o.eng and o.eng == "pe"):
                    continue
                if d.eng not in best or d.idx > best[d.eng].idx:
                    best[d.eng] = d
            for d in best.values():
                if d.val is None:
                    d.signal = True
        for e in self.ENGS:
            for o in reversed(by_eng[e]):
                if not o.is_dma:
                    o.signal = True
                    break
        for e in self.ENGS:
            for o in by_eng[e]:
                if o.is_dma:
                    s = self.dma_k[e] % N_DMA_SEMS
                    self.dma_k[e] += 1
                    self.dma_vals[e][s] += 16
                    o.sem = (e, s)
                    o.val = self.dma_vals[e][s]
                elif o.signal:
                    self.cnt[e] += 1
                    o.val = self.cnt[e]

        def emit_engine(e, eng):
            seen = self.seen[e]

            def wait(key, sem, val):
                if seen.get(key, 0) >= val:
                    return
                eng.wait_ge(sem, val)
                self.n_waits += 1
                seen[key] = val

            for o in by_eng[e]:
                best = {}
                for d in o.deps:
                    if d.is_dma:
                        wait(("dma",) + d.sem, self.dma_sems[d.sem[0]][d.sem[1]], d.val)
                    else:
                        if d.eng == e and e == "pe":
                            continue
                        if d.eng not in best or d.idx > best[d.eng].idx:
                            best[d.eng] = d
                for x, d in best.items():
                    wait(x, self.sems[x], d.val)
                if o.is_dma:
                    if o.val > 16:
                        wait(("dma",) + o.sem, self.dma_sems[e][o.sem[1]], o.val - 16)
                    ins = o.fn(eng)
                    ins.then_inc(self.dma_sems[e][o.sem[1]], 16)
                else:
                    ins = o.fn(eng)
                    if o.signal:
                        ins.then_inc(self.sems[e], 1)
                o.fn = None

        with nc.Block() as block:
            for e in self.ENGS:
                if not by_eng[e]:
                    continue
                dec = getattr(block, self.ENGOBJ[e])

                def mk(e):
                    def _f(eng):
                        emit_engine(e, eng)
                    return _f
                dec(mk(e))
        for e in self.ENGS:
            for o in reversed(by_eng[e]):
                if not o.is_dma:
                    self.last_op[e] = o
                    break
        dmas = [o for o in self.cur if o.is_dma]
        bar = set(dmas)
        for e in self.ENGS:
            if self.last_op[e] is not None:
                bar.add(self.last_op[e])
        for e in self.ENGS:
            self.pending[e] = self.pending.get(e, set()) | bar
        for o in self.cur:
            o.deps = None
        self.cur = []

    def finish(self):
        nc = self.nc
        with nc.Block() as block:
            for e in ("sp", "act", "pool"):
                if self.dma_k[e] == 0:
                    continue
                dec = getattr(block, self.ENGOBJ[e])

                def mk(e):
                    def _f(eng):
                        for s in range(N_DMA_SEMS):
                            v = self.dma_vals[e][s]
                            if v > 0:
                                eng.wait_ge(self.dma_sems[e][s], v)
                    return _f
                dec(mk(e))


class Rot:
    def __init__(self, tiles):
        self.tiles = tiles
        self.bufs = [Buf() for _ in tiles]
        self.i = 0

    def next(self):
        t, b = self.tiles[self.i], self.bufs[self.i]
        self.i = (self.i + 1) % len(self.tiles)
        return t, b


class Ctx:
    pass


_UNIQ = [0]


def sb(st, nc, name, shape, dt):
    _UNIQ[0] += 1
    return st.enter_context(nc.sbuf_tensor(f"{name}_{_UNIQ[0]}", list(shape), dt))


def ps_banks(st, nc, n, prefix):
    _UNIQ[0] += 1
    r = Rot([st.enter_context(nc.psum_tensor(f"{prefix}{i}_{_UNIQ[0]}", [128, 512], F32)) for i in range(n)])
    for b in r.bufs:
        b.psum = True
    return r


def host_consts():
    c = {}
    c["ident"] = np.eye(128, dtype=np.float32)
    c["ones"] = np.ones((128, 128), dtype=np.float32)
    return c


def phase_xt(K):
    nc, P = K.nc, K.P
    with contextlib.ExitStack() as st:
        xin = Rot([sb(st, nc, f"xt_in{i}", [128, 4, D], F32) for i in range(2)])
        hout = Rot([sb(st, nc, f"xt_out{i}", [128, 8, 512], F32) for i in range(2)])
        psr = ps_banks(st, nc, 4, "xtps")
        jobs = [(K.x[g * 512:(g + 1) * 512, :], K.hT[:, :, g * 512:(g + 1) * 512], 4) for g in range(L // 512)]
        jobs.append((K.ctx[:, :], K.ctxT[:, :, :], 2))
        n = 0
        for src, dst, na in jobs:
            xt, xb = xin.next()
            ho, hb = hout.next()
            P.dma("sp", xt[:, 0:na, :], src.rearrange("(a p) d -> p a d", p=128), writes=[xb])
            for dc in range(8):
                ps, pb = psr.next()
                for a in range(na):
                    P.op("pe", lambda e, ps=ps, xt=xt, a=a, dc=dc: e.transpose(
                        out=ps[:, a * 128:(a + 1) * 128], in_=xt[:, a, dc * 128:(dc + 1) * 128], identity=K.ident[:]),
                        reads=[xb], writes=[pb])
                if n % 2 == 0:
                    P.op("act", lambda e, ps=ps, ho=ho, dc=dc, na=na: e.activation(
                        out=ho[:, dc, 0:na * 128], in_=ps[:, 0:na * 128], func=AF.Copy), reads=[pb], writes=[hb])
                else:
                    P.op("dve", lambda e, ps=ps, ho=ho, dc=dc, na=na: e.tensor_copy(
                        out=ho[:, dc, 0:na * 128], in_=ps[:, 0:na * 128]), reads=[pb], writes=[hb])
                n += 1
            P.dma("pool", dst.rearrange("c p t -> p c t"), ho[:, :, 0:na * 128], reads=[hb])
        P.phase_end()


def phase_mod(K):
    nc, P = K.nc, K.P
    with contextlib.ExitStack() as st:
        cin = sb(st, nc, "mod_cin", [128, 2, 8], F32)
        sc2 = sb(st, nc, "mod_sc2", [128, 8, 2], F32)
        ab = sb(st, nc, "mod_ab", [128, 2, 48], F32)
        wr = Rot([sb(st, nc, f"mod_w{i}", [128, 8, 1536], F32) for i in range(2)])
        psr = ps_banks(st, nc, 2, "modps")
        bc, bs, bab = Buf(), Buf(), Buf()
        P.dma("sp", cin[:, 0, :], K.c_l, writes=[bc])
        P.dma("sp", cin[:, 1, :], K.cc_l, writes=[bc])
        P.dma("sp", ab[:], K.adab_l, writes=[bab])
        P.op("act", lambda e: e.activation(out=sc2[:].rearrange("p k t -> p t k"), in_=cin[:], func=AF.Silu), reads=[bc], writes=[bs])
        for l in range(2):
            for nb in range(4):
                w, wb = wr.next()
                P.dma("sp", w[:], K.ada_w[l, :, nb * 1536:(nb + 1) * 1536].rearrange("(kc p) n -> p kc n", p=128),
                      writes=[wb])
                ps, pb = psr.next()
                for j in range(12):
                    for kc in range(8):
                        P.op("pe", lambda e, ps=ps, w=w, j=j, kc=kc: e.matmul(
                            ps[:, j * 2:(j + 1) * 2], lhsT=w[:, kc, j * 128:(j + 1) * 128], rhs=sc2[:, kc, :],
                            start=(kc == 0), stop=(kc == 7)), reads=[wb, bs], writes=[pb])
                P.op("dve", lambda e, ps=ps, l=l, nb=nb: e.tensor_tensor(
                    out=K.MOD[:, l, nb * 12:(nb + 1) * 12, :],
                    in0=ps[:, 0:24].rearrange("p (j t) -> p j t", t=2),
                    in1=ab[:, l, nb * 12:(nb + 1) * 12].unsqueeze(2).to_broadcast([128, 12, 2]),
                    op=ALU.add), reads=[pb, bab], writes=[K.bMOD])
        P.phase_end()


def phase_norm(K, l, s, src, dst, groups, col, gname):
    nc, P = K.nc, K.P
    with contextlib.ExitStack() as st:
        A = sb(st, nc, "nm_A", [128, 8], F32)
        g = sb(st, nc, "nm_g", [128, 8], F32)
        hin = Rot([sb(st, nc, f"nm_in{i}", [128, 8, 512], F32) for i in range(2)])
        sq = Rot([sb(st, nc, f"nm_sq{i}", [128, 8, 512], BF16) for i in range(2)])
        sd = Rot([sb(st, nc, f"nm_sd{i}", [128, 512], F32) for i in range(2)])
        tmp = Rot([sb(st, nc, f"nm_tmp{i}", [128, 512], F32) for i in range(3)])
        hout = Rot([sb(st, nc, f"nm_out{i}", [128, 8, 512], BF16) for i in range(2)])
        psr = ps_banks(st, nc, 2, "nmps")
        bA, bg = Buf(), Buf()
        P.dma("sp", g[:], gname, writes=[bg])
        msc = K.MOD[:, l, (3 * s + 1) * 8:(3 * s + 2) * 8, col]
        msh = K.MOD[:, l, (3 * s) * 8:(3 * s + 1) * 8, col]
        P.op("dve", lambda e: e.scalar_tensor_tensor(out=A[:], in0=msc, scalar=1.0, in1=g[:], op0=ALU.add, op1=ALU.mult),
             reads=[K.bMOD, bg], writes=[bA])
        for (so, do, Tg) in groups:
            h, hb = hin.next()
            P.dma("sp", h[:, :, 0:Tg], src[:, :, so:so + Tg].rearrange("c p t -> p c t"), writes=[hb])
            q, qb = sq.next()
            P.op("act", lambda e, q=q, h=h, Tg=Tg: e.activation(out=q[:, :, 0:Tg], in_=h[:, :, 0:Tg], func=AF.Square),
                 reads=[hb], writes=[qb])
            ps, pb = psr.next()
            for dc in range(8):
                P.op("pe", lambda e, ps=ps, q=q, dc=dc, Tg=Tg: e.matmul(
                    ps[:, 0:Tg], lhsT=K.onesb[:], rhs=q[:, dc, 0:Tg], start=(dc == 0), stop=(dc == 7)),
                    reads=[qb], writes=[pb])
            d_, db = sd.next()
            P.op("act", lambda e, d_=d_, ps=ps, Tg=Tg: e.activation(
                out=d_[:, 0:Tg], in_=ps[:, 0:Tg], func=AF.Sqrt, bias=K.epsc[:, 0:1], scale=1.0 / D),
                reads=[pb], writes=[db])
            P.op("dve", lambda e, d_=d_, Tg=Tg: e.reciprocal(out=d_[:, 0:Tg], in_=d_[:, 0:Tg]), reads=[db], writes=[db])
            ho, hob = hout.next()
            for dc in range(8):
                t, tb = tmp.next()
                P.op("dve", lambda e, t=t, h=h, d_=d_, dc=dc, Tg=Tg: e.tensor_tensor(
                    out=t[:, 0:Tg], in0=h[:, dc, 0:Tg], in1=d_[:, 0:Tg], op=ALU.mult), reads=[hb, db], writes=[tb])
                P.op("pool", lambda e, t=t, ho=ho, dc=dc, Tg=Tg: e.tensor_scalar(
                    out=ho[:, dc, 0:Tg], in0=t[:, 0:Tg], scalar1=A[:, dc:dc + 1], scalar2=msh[:, dc:dc + 1],
                    op0=ALU.mult, op1=ALU.add), reads=[tb, bA, K.bMOD], writes=[hob])
            P.dma("pool", dst[:, :, do:do + Tg].rearrange("c p t -> p c t"), ho[:, :, 0:Tg], reads=[hob])
        P.phase_end()


WEIGHTS = [
    ("ada_w", [2, D, 6 * D]),
    ("dn_w_in", [D, 4128]), ("dn_w_out", [D, D]),
    ("hy_w_in", [D, 3 * D]), ("hy_w_out", [D, D]),
    ("peer_w_q", [2, D, 2048]), ("peer_v", [2, 16384, D]),
]


def build(stages, debug=(), **kw):
    nc = bass.Bass("TRN2", target_bir_lowering=False)
    K = Ctx()
    for k_, v_ in kw.items():
        setattr(K, k_, v_)
    K.nc = nc
    K.debug = set(debug)

    def din(name, shape, dt=F32):
        return nc.dram_tensor(name, list(shape), dt, kind="ExternalInput").ap()

    def dscr(name, shape, dt=F32):
        if name in getattr(K, 'ext_in', ()):
            return nc.dram_tensor(name, list(shape), dt, kind="ExternalInput").ap()
        kind = {"kind": "ExternalOutput"} if name in K.debug else {}
        return nc.dram_tensor(name, list(shape), dt, **kind).ap()

    K.x = din("x", [L, D])
    K.ctx = din("ctx", [CTX, D])
    K.c_l = din("c_l", [128, 8])
    K.cc_l = din("cc_l", [128, 8])
    K.adab_l = din("adab_l", [128, 2, 48])
    K.normg_l = din("normg_l", [2, 2, 128, 8])
    K.finalg_l = din("finalg_l", [128, 8])
    K.cident = din("cident", [128, 128])
    K.dn_cw_l = din("dn_cw_l", [128, 24, 5])
    K.cscan = din("cscan", [6, 128, 128])
    K.dn_gn_l = din("dn_gn_l", [128, 1024])
    K.peer_u_l = din("peer_u_l", [2, 128, 8, 128, 128])
    K.peer_kT_l = din("peer_kT_l", [2, 128, 16, 128])
    K.hy_cw_l = din("hy_cw_l", [128, 24, 3])
    K.hy_cb_l = din("hy_cb_l", [128, 24])
    K.hy_f_w1 = din("hy_f_w1", [33, 64])
    K.hy_f_w2 = din("hy_f_w2", [64, 64])
    K.hy_f_w3 = din("hy_f_w3", [64, 64])
    K.hy_f_w4 = din("hy_f_w4", [64, 4096])
    K.hy_fpv_l = din("hy_fpv_l", [64, 4])
    K.hy_ndelta_l = din("hy_ndelta_l", [128, 8])
    K.hy_zpos = din("hy_zpos", [33, L])
    K.hy_tpos = din("hy_tpos", [128, L])
    K.hy_skip_l = din("hy_skip_l", [128, 2, 8])
    K.dn_alog_l = din("dn_alog_l", [128, 16])
    K.dn_dtb_l = din("dn_dtb_l", [128, 16])
    for name, shape in WEIGHTS:
        setattr(K, name, din(name, shape))
    K.out = nc.dram_tensor("out", [L, D], F32, kind="ExternalOutput").ap()
    if "hT" in getattr(K, 'ext_in', ()):
        K.hT_in = nc.dram_tensor("hT", [8, 128, L], F32, kind="ExternalInput").ap()
        K.hT = nc.dram_tensor("hTout", [8, 128, L], F32, kind="ExternalOutput").ap()
    else:
        K.hT = dscr("hT", [8, 128, L])
    K.ctxT = dscr("ctxT", [8, 128, CTX])
    K.hnT = dscr("hnT", [8, 128, TT], BF16)
    K.qT = dscr("qT", [8, 128, TT], BF16)
    K.kT = dscr("kT", [8, 128, TT], BF16)
    K.qkvtok = dscr("qkvtok", [TT, 3072])
    K.sgate = dscr("sgate", [TT, 1024])
    K.ba = dscr("ba", [TT, 32])
    K.o_dir = [dscr("o_f", [L, 1024]), dscr("o_b", [L, 1024])]
    K.ygT = dscr("ygT", [8, 128, L], BF16)
    K.Ub = dscr("Ub", [128, 128, 8, 128], BF16)
    K.zT = dscr("zT", [24, 128, L])
    K.filtT = dscr("filtT", [2, 2, 8, 128, L])
    K.z2T = dscr("z2T", [8, 128, L], BF16)
    K.Vb = dscr("Vb", [128, 128, 1024], BF16)
    if getattr(K, 'scan_dbg', False):
        K.dbgwk = nc.dram_tensor("dbgwk", [8, 128, 13, 128], F32, kind="ExternalOutput").ap()
        K.dbgsm = nc.dram_tensor("dbgsm", [128, 12, 8], F32, kind="ExternalOutput").ap()

    with contextlib.ExitStack() as st:
        P = Prog(nc, st)
        K.P = P
        K.ident = sb(st, nc, "ident", [128, 128], F32)
        K.identb = sb(st, nc, "identb", [128, 128], BF16)
        K.onesb = sb(st, nc, "onesb", [128, 128], BF16)
        K.onesf = sb(st, nc, "onesf", [128, 128], F32)
        K.epsc = sb(st, nc, "epsc", [128, 1], F32)
        K.onec = sb(st, nc, "onec", [128, 1], F32)
        K.MOD = sb(st, nc, "MOD", [128, 2, 48, 2], F32)
        K.bMOD = Buf()
        bi = Buf()
        P.dma("sp", K.ident[:], K.cident, writes=[bi])
        P.op("dve", lambda e: e.tensor_copy(out=K.identb[:], in_=K.ident[:]), reads=[bi], writes=[bi])
        P.op("dve", lambda e: e.memset(K.onesb[:], 1.0), writes=[bi])
        P.op("dve", lambda e: e.memset(K.onesf[:], 1.0), writes=[bi])
        P.op("dve", lambda e: e.memset(K.epsc[:], EPS), writes=[bi])
        P.op("dve", lambda e: e.memset(K.onec[:], 1.0), writes=[bi])
        P.phase_end()
        lat_groups = [(g * 512, CTX + g * 512, 512) for g in range(L // 512)]
        if "hT" in getattr(K, 'ext_in', ()):
            with contextlib.ExitStack() as st2:
                cpr = Rot([sb(st2, nc, f"cp{i}", [128, 8, 512], F32) for i in range(2)])
                for g in range(L // 512):
                    t, tb = cpr.next()
                    P.dma("sp", t[:], K.hT_in[:, :, g * 512:(g + 1) * 512].rearrange("c p t -> p c t"), writes=[tb])
                    P.dma("sp", K.hT[:, :, g * 512:(g + 1) * 512].rearrange("c p t -> p c t"), t[:], reads=[tb])
                P.phase_end()
        if "xt" in stages:
            phase_xt(K)
        if "mod" in stages:
            phase_mod(K)
        if "norm0" in stages:
            phase_norm(K, 0, 0, K.hT, K.hnT, lat_groups, 0, K.normg_l[0, 0])
            phase_norm(K, 0, 0, K.ctxT, K.hnT, [(0, 0, CTX)], 1, K.normg_l[0, 0])
        if "dnin" in stages:
            phase_dnin(K)
        if "dnscan" in stages:
            phase_dnscan(K)
        if "dnout" in stages:
            phase_dnout(K)
            phase_outproj(K, K.ygT, K.dn_w_out, 0, 2)
        if "peer0" in stages:
            phase_norm(K, 0, 1, K.hT, K.hnT, lat_groups, 0, K.normg_l[0, 1])
            phase_peer_tables(K, 0)
            phase_peer(K, 0)
        if "hyena" in stages:
            phase_norm(K, 1, 0, K.hT, K.hnT, lat_groups, 0, K.normg_l[1, 0])
            phase_hyin(K)
            phase_hyfilt(K)
            phase_hyconv_direct(K)
            phase_outproj(K, K.z2T, K.hy_w_out, 1, 2)
        if "peer1" in stages:
            phase_norm(K, 1, 1, K.hT, K.hnT, lat_groups, 0, K.normg_l[1, 1])
            phase_peer_tables(K, 1)
            phase_peer(K, 1)
        if "final" in stages:
            phase_final(K)
        if "dbgmod" in K.debug:
            dm = nc.dram_tensor("dbgmod", [128, 2 * 48 * 2], F32, kind="ExternalOutput").ap()
            P.dma("sp", dm, K.MOD[:].rearrange("p a b c -> p (a b c)"), reads=[K.bMOD])
            P.phase_end()
        P.finish()
        K.nops = P.nops
        K.nwaits = P.n_waits
    return nc, K


_HC = {}


def hyena_consts():
    if not _HC:
        f32 = np.float32
        pos = np.arange(L, dtype=f32)
        t = (pos / f32(L - 1)).astype(f32)
        bands = np.linspace(1e-4, 15, 16, dtype=f32)
        ang = ((f32(2 * math.pi) * pos / f32(L))[:, None] * bands[None]).astype(f32)
        z = np.concatenate([t[:, None], np.cos(ang), -np.sin(ang)], axis=-1).astype(f32)
        deltas = np.abs(np.linspace(math.log(1e-2) / 0.3, math.log(1e-2) / 1.5, D, dtype=f32)).astype(f32)
        _HC["hy_zpos"] = np.ascontiguousarray(z.T)
        _HC["hy_tpos"] = np.ascontiguousarray(np.tile(t[None, :], (128, 1)))
        _HC["hy_ndelta_l"] = np.ascontiguousarray(-deltas.reshape(8, 128).T)
    return _HC


def host_inputs(inputs, b):
    f = np.ascontiguousarray
    m = {}
    m["x"] = f(inputs["x"][b])
    m["ctx"] = f(inputs["ctx"][b])
    m["c_l"] = f(inputs["c"][b].reshape(8, 128).T)
    m["cc_l"] = f(inputs["c_ctx"].reshape(8, 128).T)
    m["adab_l"] = f(inputs["ada_b"].reshape(2, 48, 128).transpose(2, 0, 1))
    m["normg_l"] = f(inputs["norm_g"].reshape(2, 2, 8, 128).transpose(0, 1, 3, 2))
    m["finalg_l"] = f(inputs["final_g"].reshape(8, 128).T)
    m["cident"] = np.eye(128, dtype=np.float32)
    m["dn_cw_l"] = f(inputs["dn_conv_w"][0].reshape(5, 24, 128).transpose(2, 1, 0))
    i_ = np.arange(128)
    tri_f = (i_[:, None] <= i_[None, :]).astype(np.float32)
    BIG = 30000.0
    maska_f = np.where(i_[None, :] > i_[:, None], BIG, 0.0).astype(np.float32)
    m01s_f = (i_[None, :] < i_[:, None]).astype(np.float32)
    m["cscan"] = f(np.stack([tri_f, tri_f.T, maska_f, maska_f.T, m01s_f, m01s_f.T]))
    m["dn_alog_l"] = f(np.tile(inputs["dn_a_log"][0].reshape(1, 16), (128, 1)))
    m["dn_dtb_l"] = f(np.tile(inputs["dn_dt_bias"][0].reshape(1, 16), (128, 1)))
    m["dn_gn_l"] = f(np.tile(np.tile(inputs["dn_norm_g"][0], 8).reshape(1, 1024), (128, 1)))
    m["hy_cw_l"] = f(inputs["hy_conv_w"][0].reshape(3, 24, 128).transpose(2, 1, 0))
    m["hy_cb_l"] = f(inputs["hy_conv_b"][0].reshape(24, 128).T)
    m["hy_f_w1"] = f(inputs["hy_f_w1"][0]); m["hy_f_w2"] = f(inputs["hy_f_w2"][0])
    m["hy_f_w3"] = f(inputs["hy_f_w3"][0]); m["hy_f_w4"] = f(inputs["hy_f_w4"][0])
    m["hy_fpv_l"] = f(np.stack([inputs["hy_freq"][0], inputs["hy_f_b1"][0], inputs["hy_f_b2"][0], inputs["hy_f_b3"][0]], axis=1))
    m["hy_skip_l"] = f(inputs["hy_skip"][0].reshape(2, 8, 128).transpose(2, 0, 1))
    m.update(hyena_consts())
    m["ada_w"] = inputs["ada_w"]
    m["dn_w_in"] = inputs["dn_w_in"][0]
    m["dn_w_out"] = inputs["dn_w_out"][0]
    m["hy_w_in"] = inputs["hy_w_in"][0]
    m["hy_w_out"] = inputs["hy_w_out"][0]
    m["peer_w_q"] = inputs["peer_w_q"]
    m["peer_u_l"] = f(inputs["peer_u"].reshape(2, 128, 128, 8, 128).transpose(0, 1, 3, 4, 2))
    m["peer_kT_l"] = f(inputs["peer_keys"].reshape(2, 16, 128, 128).transpose(0, 3, 1, 2))
    m["peer_v"] = inputs["peer_v"]
    return m


def phase_dnin(K):
    nc, P = K.nc, K.P
    Tg = 256
    with contextlib.ExitStack() as st:
        Wb = sb(st, nc, "dn_Wb", [128, 8, 4128], BF16)
        cw = sb(st, nc, "dn_cw", [128, 24, 5], F32)
        hnr = Rot([sb(st, nc, f"dn_hn{i}", [128, 8, Tg], BF16) for i in range(2)])
        cvr = Rot([sb(st, nc, f"dn_cv{i}", [128, 24, Tg], F32) for i in range(1)])
        sqr = Rot([sb(st, nc, f"dn_sq{i}", [128, 16, Tg], BF16) for i in range(1)])
        qkr = Rot([sb(st, nc, f"dn_qk{i}", [128, 16, Tg], BF16) for i in range(2)])
        tokr = Rot([sb(st, nc, f"dn_tok{i}", [128, 2, 3072], F32) for i in range(1)])
        sgr = Rot([sb(st, nc, f"dn_sg{i}", [128, 2, 1024], F32) for i in range(1)])
        bar = Rot([sb(st, nc, f"dn_ba{i}", [128, 2, 32], F32) for i in range(2)])
        tmr = Rot([sb(st, nc, f"dn_tm{i}", [128, Tg], F32) for i in range(2)])
        zps = ps_banks(st, nc, 2, "dnz")
        sps = ps_banks(st, nc, 2, "dns")
        tps = ps_banks(st, nc, 2, "dnt")
        gps = ps_banks(st, nc, 2, "dng")
        bW, bcw = Buf(), Buf()
        for kc in range(8):
            for c0 in range(0, 4128, 1376):
                P.dma("pool", Wb[:, kc, c0:c0 + 1376], K.dn_w_in[kc * 128:(kc + 1) * 128, c0:c0 + 1376], writes=[bW])
        P.dma("sp", cw[:], K.dn_cw_l, writes=[bcw])
        groups = [(0, 256)] + [(CTX + g * Tg, 64) for g in range(L // Tg)]
        n = 0
        for off, rowlen in groups:
            hn, hb = hnr.next()
            P.dma("sp", hn[:], K.hnT[:, :, off:off + Tg].rearrange("c p t -> p c t"), writes=[hb])
            cv, cb = cvr.next()
            for cc in range(24):
                ps, pb = zps.next()
                for kc in range(8):
                    P.op("pe", lambda e, ps=ps, hn=hn, cc=cc, kc=kc: e.matmul(
                        ps[:, 0:Tg], lhsT=Wb[:, kc, cc * 128:(cc + 1) * 128], rhs=hn[:, kc, :],
                        start=(kc == 0), stop=(kc == 7)), reads=[bW, hb], writes=[pb])
                P.op("dve", lambda e, ps=ps, cv=cv, cc=cc: e.tensor_scalar(
                    out=cv[:, cc, :], in0=ps[:, 0:Tg], scalar1=cw[:, cc, 2:3], scalar2=None, op0=ALU.mult),
                    reads=[pb, bcw], writes=[cb])
                for k in (0, 1, 3, 4):
                    s = k - 2
                    lo, hi = max(0, -s), rowlen - max(0, s)
                    P.op("dve", lambda e, ps=ps, cv=cv, cc=cc, k=k, s=s, lo=lo, hi=hi, rowlen=rowlen: e.scalar_tensor_tensor(
                        out=cv[:, cc, :].rearrange("p (r w) -> p r w", w=rowlen)[:, :, lo:hi],
                        in0=ps[:, 0:Tg].rearrange("p (r w) -> p r w", w=rowlen)[:, :, lo + s:hi + s],
                        scalar=cw[:, cc, k:k + 1],
                        in1=cv[:, cc, :].rearrange("p (r w) -> p r w", w=rowlen)[:, :, lo:hi],
                        op0=ALU.mult, op1=ALU.add), reads=[pb, bcw], writes=[cb])
            P.op("act", lambda e, cv=cv: e.activation(out=cv[:], in_=cv[:], func=AF.Silu), reads=[cb], writes=[cb])
            sg, sgb = sgr.next()
            bt, btb = bar.next()
            for a in range(2):
                for (c0, wd) in ((0, 512), (512, 512), (1024, 32)):
                    ps, pb = gps.next()
                    for kc in range(8):
                        P.op("pe", lambda e, ps=ps, hn=hn, a=a, kc=kc, c0=c0, wd=wd: e.matmul(
                            ps[:, 0:wd], lhsT=hn[:, kc, a * 128:(a + 1) * 128], rhs=Wb[:, kc, 3072 + c0:3072 + c0 + wd],
                            start=(kc == 0), stop=(kc == 7)), reads=[bW, hb], writes=[pb])
                    if wd == 512:
                        P.op("act", lambda e, ps=ps, sg=sg, a=a, c0=c0: e.activation(
                            out=sg[:, a, c0:c0 + 512], in_=ps[:, :], func=AF.Silu), reads=[pb], writes=[sgb])
                    else:
                        P.op("dve", lambda e, ps=ps, bt=bt, a=a: e.tensor_copy(out=bt[:, a, :], in_=ps[:, 0:32]),
                             reads=[pb], writes=[btb])
            P.dma("pool", K.sgate[off:off + Tg, :].rearrange("(a p) c -> p a c", p=128), sg[:], reads=[sgb])
            P.dma("pool", K.ba[off:off + Tg, :].rearrange("(a p) c -> p a c", p=128), bt[:], reads=[btb])
            sq, sqb_ = sqr.next()
            P.op("act", lambda e, sq=sq, cv=cv: e.activation(out=sq[:], in_=cv[:, 0:16, :], func=AF.Square),
                 reads=[cb], writes=[sqb_])
            qk, qkb = qkr.next()
            for cc in range(16):
                ps, pb = sps.next()
                P.op("pe", lambda e, ps=ps, sq=sq, cc=cc: e.matmul(ps[:, 0:Tg], lhsT=K.onesb[:], rhs=sq[:, cc, :],
                                                                  start=True, stop=True), reads=[sqb_], writes=[pb])
                t, tb = tmr.next()
                P.op("act", lambda e, ps=ps, t=t: e.activation(out=t[:], in_=ps[:, 0:Tg], func=AF.Ln,
                                                               bias=K.epsc[:, 0:1], scale=1.0), reads=[pb], writes=[tb])
                P.op("act", lambda e, t=t: e.activation(out=t[:], in_=t[:], func=AF.Exp, scale=-0.5),
                     reads=[tb], writes=[tb])
                scl = (128.0 ** -0.5) if cc < 8 else 1.0
                P.op("dve", lambda e, cv=cv, cc=cc, t=t, scl=scl: e.scalar_tensor_tensor(
                    out=cv[:, cc, :], in0=cv[:, cc, :], scalar=scl, in1=t[:], op0=ALU.mult, op1=ALU.mult),
                    reads=[cb, tb], writes=[cb])
                P.op("pool", lambda e, cv=cv, qk=qk, cc=cc: e.tensor_copy(out=qk[:, cc, :], in_=cv[:, cc, :]),
                     reads=[cb], writes=[qkb])
            P.dma("pool", K.qT[:, :, off:off + Tg].rearrange("c p t -> p c t"), qk[:, 0:8, :], reads=[qkb])
            P.dma("pool", K.kT[:, :, off:off + Tg].rearrange("c p t -> p c t"), qk[:, 8:16, :], reads=[qkb])
            tok, tkb = tokr.next()
            for a in range(2):
                for g6 in range(6):
                    ps, pb = tps.next()
                    for j in range(4):
                        P.op("pe", lambda e, ps=ps, cv=cv, a=a, g6=g6, j=j: e.transpose(
                            out=ps[:, j * 128:(j + 1) * 128], in_=cv[:, g6 * 4 + j, a * 128:(a + 1) * 128],
                            identity=K.ident[:]), reads=[cb], writes=[pb])
                    if n % 2 == 0:
                        P.op("act", lambda e, ps=ps, tok=tok, a=a, g6=g6: e.activation(
                            out=tok[:, a, g6 * 512:(g6 + 1) * 512], in_=ps[:, :], func=AF.Copy), reads=[pb], writes=[tkb])
                    else:
                        P.op("dve", lambda e, ps=ps, tok=tok, a=a, g6=g6: e.tensor_copy(
                            out=tok[:, a, g6 * 512:(g6 + 1) * 512], in_=ps[:, :]), reads=[pb], writes=[tkb])
                    n += 1
            P.dma("pool", K.qkvtok[off:off + Tg, :].rearrange("(a p) c -> p a c", p=128), tok[:], reads=[tkb])
        P.phase_end()


def phase_dnscan(K):
    nc, P = K.nc, K.P
    NB = TT // 128
    lim = getattr(K, 'scan_maxops', None)
    if lim is not None:
        real_op = P.op
        cnt = [0]

        def lim_op(*a, **k):
            cnt[0] += 1
            if cnt[0] > lim:
                return None
            return real_op(*a, **k)
        P.op = lim_op
    with contextlib.ExitStack() as st:
        cs = sb(st, nc, "sc_consts", [128, 6, 128], F32)
        alg = sb(st, nc, "sc_alog", [128, 16], F32)
        dtb = sb(st, nc, "sc_dtb", [128, 16], F32)
        nea = sb(st, nc, "sc_nea", [128, 16], F32)
        S = [[sb(st, nc, f"sc_S{d}{h}", [128, 128], F32) for h in range(8)] for d in range(2)]
        bS = [[Buf() for h in range(8)] for d in range(2)]
        kTr = Rot([sb(st, nc, f"sc_kT{i}", [128, 8, 128], BF16) for i in range(2)])
        qTr = Rot([sb(st, nc, f"sc_qT{i}", [128, 8, 128], BF16) for i in range(2)])
        tokr = Rot([sb(st, nc, f"sc_tok{i}", [128, 3072], F32) for i in range(2)])
        bar = Rot([sb(st, nc, f"sc_ba{i}", [128, 32], F32) for i in range(2)])
        smr = Rot([sb(st, nc, f"sc_sm{i}", [128, 12, 8], F32) for i in range(2)])
        dgr = Rot([sb(st, nc, f"sc_dg{i}", [128, 5, 8, 128], F32) for i in range(2)])
        obr = Rot([sb(st, nc, f"sc_ob{i}", [128, 1024], F32) for i in range(2)])
        NW = 13
        wkr = [Rot([sb(st, nc, f"sc_wk{h}_{i}", [128, NW, 128], F32) for i in range(1)]) for h in range(8)]
        _pst = [st.enter_context(nc.psum_tensor(f"scps{i}", [128, 512], F32)) for i in range(8)]
        _bb = [Buf(psum=True) for i in range(8)]

        def mkpool(banks):
            r = Rot([_pst[i][:, j * 128:(j + 1) * 128] for i in banks for j in range(4)])
            r.bufs = [_bb[i] for i in banks for j in range(4)]
            return r
        psA = mkpool([0, 1, 2, 3])
        psD = mkpool([4, 5, 6, 7])
        bC = Buf()
        P.dma("sp", cs[:], K.cscan.rearrange("c p f -> p c f"), writes=[bC])
        P.dma("sp", alg[:], K.dn_alog_l, writes=[bC])
        P.dma("sp", dtb[:], K.dn_dtb_l, writes=[bC])
        P.op("act", lambda e: e.activation(out=nea[:], in_=alg[:], func=AF.Exp), reads=[bC], writes=[bC])
        P.op("dve", lambda e: e.tensor_scalar(out=nea[:], in0=nea[:], scalar1=-1.0, scalar2=None, op0=ALU.mult),
             reads=[bC], writes=[bC])
        for d in range(2):
            for h in range(8):
                P.op("pool", lambda e, d=d, h=h: e.memset(S[d][h][:], 0.0), writes=[bS[d][h]])
        TRI = [cs[:, 0, :], cs[:, 1, :]]
        MASKA = [cs[:, 2, :], cs[:, 3, :]]
        M01S = [cs[:, 4, :], cs[:, 5, :]]
        order = [list(range(NB)), [1, 0] + list(range(NB - 1, 1, -1))]
        evn = [0]

        def evac(out, in_, rd, wr, eng):
            if eng == "act":
                P.op("act", lambda e: e.activation(out=out, in_=in_, func=AF.Copy), reads=rd, writes=wr)
            else:
                P.op("dve", lambda e: e.tensor_copy(out=out, in_=in_), reads=rd, writes=wr)

        def job(blk, d):
            t0 = blk * 128
            with_out = blk >= 2
            kT, kTb = kTr.next()
            qT, qTb = qTr.next()
            tok, tkb = tokr.next()
            bt, btb = bar.next()
            sm, smb = smr.next()
            dg, dgb = dgr.next()
            P.dma("sp", kT[:], K.kT[:, :, t0:t0 + 128].rearrange("c p t -> p c t"), writes=[kTb])
            P.dma("sp", qT[:], K.qT[:, :, t0:t0 + 128].rearrange("c p t -> p c t"), writes=[qTb])
            P.dma("sp", tok[:], K.qkvtok[t0:t0 + 128, :], writes=[tkb])
            P.dma("sp", bt[:], K.ba[t0:t0 + 128, :], writes=[btb])
            BETA, NBETA, GG, EG, EKD, GL, BG, TMP, G_ = 0, 1, 2, 3, 4, 5, 6, 7, 8
            P.op("act", lambda e: e.activation(out=sm[:, TMP, :], in_=bt[:, d * 8:(d + 1) * 8], func=AF.Exp, scale=-1.0),
                 reads=[btb], writes=[smb])
            P.op("dve", lambda e: e.tensor_scalar(out=sm[:, TMP, :], in0=sm[:, TMP, :], scalar1=1.0, scalar2=None,
                                                  op0=ALU.add), reads=[smb], writes=[smb])
            P.op("dve", lambda e: e.reciprocal(out=sm[:, BETA, :], in_=sm[:, TMP, :]), reads=[smb], writes=[smb])
            P.op("dve", lambda e: e.tensor_scalar(out=sm[:, NBETA, :], in0=sm[:, BETA, :], scalar1=-1.0, scalar2=None,
                                                  op0=ALU.mult), reads=[smb], writes=[smb])
            P.op("dve", lambda e: e.tensor_tensor(out=sm[:, TMP, :], in0=bt[:, 16 + d * 8:16 + (d + 1) * 8],
                                                  in1=dtb[:, d * 8:(d + 1) * 8], op=ALU.add), reads=[btb, bC], writes=[smb])
            P.op("act", lambda e: e.activation(out=sm[:, TMP, :], in_=sm[:, TMP, :], func=AF.Exp), reads=[smb], writes=[smb])
            P.op("act", lambda e: e.activation(out=sm[:, TMP, :], in_=sm[:, TMP, :], func=AF.Ln, bias=K.onec[:, 0:1],
                                               scale=1.0), reads=[smb], writes=[smb])
            P.op("dve", lambda e: e.tensor_tensor(out=sm[:, G_, :], in0=sm[:, TMP, :], in1=nea[:, d * 8:(d + 1) * 8],
                                                  op=ALU.mult), reads=[smb, bC], writes=[smb])
            pg, pgb = psD.next()
            P.op("pe", lambda e: e.matmul(pg[:, 0:8], lhsT=TRI[d], rhs=sm[:, G_, :], start=True, stop=True),
                 reads=[smb, bC], writes=[pgb])
            P.op("pe", lambda e: e.matmul(pg[:, 8:16], lhsT=K.onesf[:], rhs=sm[:, G_, :], start=True, stop=True),
                 reads=[smb], writes=[pgb])
            P.op("dve", lambda e: e.tensor_copy(out=sm[:, GG, :], in_=pg[:, 0:8]), reads=[pgb], writes=[smb])
            P.op("act", lambda e: e.activation(out=sm[:, EG, :], in_=pg[:, 0:8], func=AF.Exp), reads=[pgb], writes=[smb])
            P.op("act", lambda e: e.activation(out=sm[:, GL, :], in_=pg[:, 8:16], func=AF.Exp), reads=[pgb], writes=[smb])
            P.op("dve", lambda e: e.tensor_tensor(out=sm[:, EKD, :], in0=pg[:, 8:16], in1=sm[:, GG, :], op=ALU.subtract),
                 reads=[pgb, smb], writes=[smb])
            P.op("act", lambda e: e.activation(out=sm[:, EKD, :], in_=sm[:, EKD, :], func=AF.Exp), reads=[smb], writes=[smb])
            P.op("dve", lambda e: e.tensor_tensor(out=sm[:, BG, :], in0=sm[:, BETA, :], in1=sm[:, EG, :], op=ALU.mult),
                 reads=[smb], writes=[smb])
            DIAGG, DIAGEG, KBG, KD, VB = 0, 1, 2, 3, 4
            idb = K.ident[:].unsqueeze(1).to_broadcast([128, 8, 128])

            def bc(i):
                return sm[:, i, :].unsqueeze(2).to_broadcast([128, 8, 128])
            ktok = tok[:, 1024:2048].rearrange("p (h c) -> p h c", c=128)
            vtok = tok[:, 2048:3072].rearrange("p (h c) -> p h c", c=128)
            P.op("pool", lambda e: e.tensor_tensor(out=dg[:, DIAGG], in0=idb, in1=bc(GG), op=ALU.mult),
                 reads=[smb], writes=[dgb])
            P.op("pool", lambda e: e.tensor_tensor(out=dg[:, DIAGEG], in0=idb, in1=bc(EG), op=ALU.mult),
                 reads=[smb], writes=[dgb])
            P.op("pool", lambda e: e.tensor_tensor(out=dg[:, KBG], in0=ktok, in1=bc(BG), op=ALU.mult),
                 reads=[smb, tkb], writes=[dgb])
            P.op("dve", lambda e: e.tensor_tensor(out=dg[:, KD], in0=ktok, in1=bc(EKD), op=ALU.mult),
                 reads=[smb, tkb], writes=[dgb])
            P.op("dve", lambda e: e.tensor_tensor(out=dg[:, VB], in0=vtok, in1=bc(BETA), op=ALU.mult),
                 reads=[smb, tkb], writes=[dgb])
            DM, DMS, X0, X1, Y0, Y1, QA, QAT, TT_, U, WT, QGT, VN = range(13)
            wk = []
            wb = []
            for h in range(8):
                t, b = wkr[h].next()
                wk.append(t)
                wb.append(b)
            if getattr(K, 'scan_stage', 99) <= 1:
                return
            if getattr(K, 'scan_serial', False):
                P.phase_end()
            pkk, pqk, pgr = [], [], []
            for h in range(8):
                a, ab = psD.next()
                P.op("pe", lambda e, a=a, h=h: e.matmul(a, lhsT=kT[:, h, :], rhs=kT[:, h, :], start=True, stop=True),
                     reads=[kTb], writes=[ab])
                pkk.append((a, ab))
                a, ab = psD.next()
                P.op("pe", lambda e, a=a, h=h: e.matmul(a, lhsT=qT[:, h, :], rhs=kT[:, h, :], start=True, stop=True),
                     reads=[kTb, qTb], writes=[ab])
                pqk.append((a, ab))
                a, ab = psA.next()
                P.op("pe", lambda e, a=a, h=h: e.matmul(a, lhsT=K.onesf[:], rhs=dg[:, DIAGG, h, :], start=True, stop=False),
                     reads=[dgb], writes=[ab])
                P.op("pe", lambda e, a=a, h=h: e.matmul(a, lhsT=K.ident[:], rhs=MASKA[d], start=False, stop=True),
                     reads=[bC], writes=[ab])
                pgr.append((a, ab))
            if getattr(K, 'scan_stage', 99) <= 2:
                return
            if getattr(K, 'scan_serial', False):
                P.phase_end()
            for h in range(8):
                w = wk[h]
                P.op("act", lambda e, w=w, h=h: e.activation(out=w[:, DM, :], in_=pgr[h][0], func=AF.Exp,
                                                             bias=sm[:, GG, h:h + 1], scale=-1.0),
                     reads=[pgr[h][1], smb], writes=[wb[h]])
                P.op("pool", lambda e, w=w: e.tensor_tensor(out=w[:, DMS, :], in0=w[:, DM, :], in1=M01S[d], op=ALU.mult),
                     reads=[wb[h], bC], writes=[wb[h]])
                P.op("dve", lambda e, w=w, h=h: e.scalar_tensor_tensor(
                    out=w[:, X0, :], in0=pkk[h][0], scalar=sm[:, NBETA, h:h + 1], in1=w[:, DMS, :],
                    op0=ALU.mult, op1=ALU.mult), reads=[pkk[h][1], smb, wb[h]], writes=[wb[h]])
                P.op("dve", lambda e, w=w, h=h: e.tensor_tensor(out=w[:, QA, :], in0=pqk[h][0], in1=w[:, DM, :], op=ALU.mult),
                     reads=[pqk[h][1], wb[h]], writes=[wb[h]])
            if getattr(K, 'scan_stage', 99) <= 3:
                return
            if getattr(K, 'scan_serial', False):
                P.phase_end()
            pa, pb_ = [], []
            for h in range(8):
                w = wk[h]
                a, ab = psA.next()
                P.op("pe", lambda e, a=a, w=w: e.transpose(out=a, in_=w[:, X0, :], identity=K.ident[:]),
                     reads=[wb[h]], writes=[ab])
                pa.append((a, ab))
                a, ab = psD.next()
                P.op("pe", lambda e, a=a, w=w: e.transpose(out=a, in_=w[:, QA, :], identity=K.ident[:]),
                     reads=[wb[h]], writes=[ab])
                pb_.append((a, ab))
            for h in range(8):
                w = wk[h]
                evac(w[:, Y0, :], pa[h][0], [pa[h][1]], [wb[h]], "act")
                evac(w[:, QAT, :], pb_[h][0], [pb_[h][1]], [wb[h]], "dve")
                P.op("pool", lambda e, w=w: e.tensor_tensor(out=w[:, TT_, :], in0=w[:, Y0, :], in1=K.ident[:], op=ALU.add),
                     reads=[wb[h]], writes=[wb[h]])
            if getattr(K, 'scan_dbg', False) and blk == 0 and d == 0:
                for h in range(8):
                    for i_ in (0, 1, 2, 4, 6, 7, 8):
                        P.dma("sp", K.dbgwk[h, :, i_, :], wk[h][:, i_, :], reads=[wb[h]])
                P.dma("sp", K.dbgsm[:, 0:9, :], sm[:, 0:9, :], reads=[smb])
            if getattr(K, 'scan_stage', 99) <= 4:
                return
            if lim is not None and blk == 0 and d == 0:
                print("ops before S5:", cnt[0])
            if getattr(K, 'scan_serial', False):
                P.phase_end()
            for lvl in range(1, 7):
                xo, yo = (X0, Y0) if lvl % 2 == 1 else (X1, Y1)
                xn, yn = (X1, Y1) if lvl % 2 == 1 else (X0, Y0)
                px, py = [], []
                for h in range(8):
                    w = wk[h]
                    a, ab = psA.next()
                    P.op("pe", lambda e, a=a, w=w, xo=xo, yo=yo: e.matmul(a, lhsT=w[:, yo, :], rhs=w[:, xo, :],
                                                                        start=True, stop=True), reads=[wb[h]], writes=[ab])
                    px.append((a, ab))
                    if lvl < 6:
                        a, ab = psD.next()
                        P.op("pe", lambda e, a=a, w=w, xo=xo, yo=yo: e.matmul(a, lhsT=w[:, xo, :], rhs=w[:, yo, :],
                                                                            start=True, stop=True), reads=[wb[h]], writes=[ab])
                        py.append((a, ab))
                for h in range(8):
                    w = wk[h]
                    P.op("act", lambda e, w=w, h=h, xn=xn, px=px: e.activation(out=w[:, xn, :], in_=px[h][0], func=AF.Copy),
                         reads=[px[h][1]], writes=[wb[h]])
                    if lvl < 6:
                        P.op("dve", lambda e, w=w, h=h, yn=yn, py=py: e.tensor_copy(out=w[:, yn, :], in_=py[h][0]),
                             reads=[py[h][1]], writes=[wb[h]])
                if lim is not None and blk == 0 and d == 0:
                    print("lvl", lvl, "ops before pu:", cnt[0])
                pu = []
                for h in range(8):
                    w = wk[h]
                    a, ab = psD.next()
                    P.op("pe", lambda e, a=a, w=w, xn=xn: e.matmul(a, lhsT=w[:, xn, :], rhs=w[:, TT_, :],
                                                                 start=True, stop=True), reads=[wb[h]], writes=[ab])
                    pu.append((a, ab))
                for h in range(8):
                    w = wk[h]
                    P.op("dve", lambda e, w=w, h=h, pu=pu: e.tensor_tensor(out=w[:, TT_, :], in0=pu[h][0], in1=w[:, TT_, :],
                                                                  op=ALU.add), reads=[pu[h][1], wb[h]], writes=[wb[h]])
            if getattr(K, 'scan_stage', 99) <= 5:
                return
            if getattr(K, 'scan_serial', False):
                P.phase_end()
            p1, p2, p3 = [], [], []
            qtok = tok[:, 0:1024].rearrange("p (h c) -> p h c", c=128)
            for h in range(8):
                w = wk[h]
                a, ab = psA.next()
                P.op("pe", lambda e, a=a, w=w, h=h: e.matmul(a, lhsT=w[:, TT_, :], rhs=dg[:, VB, h, :], start=True, stop=True),
                     reads=[wb[h], dgb], writes=[ab])
                p1.append((a, ab))
                a, ab = psD.next()
                P.op("pe", lambda e, a=a, w=w, h=h: e.matmul(a, lhsT=dg[:, KBG, h, :], rhs=w[:, TT_, :], start=True, stop=True),
                     reads=[wb[h], dgb], writes=[ab])
                p2.append((a, ab))
                a, ab = psA.next()
                P.op("pe", lambda e, a=a, h=h: e.matmul(a, lhsT=qtok[:, h, :], rhs=dg[:, DIAGEG, h, :], start=True, stop=True),
                     reads=[tkb, dgb], writes=[ab])
                p3.append((a, ab))
            for h in range(8):
                w = wk[h]
                evac(w[:, U, :], p1[h][0], [p1[h][1]], [wb[h]], "act")
                evac(w[:, WT, :], p2[h][0], [p2[h][1]], [wb[h]], "dve")
                evac(w[:, QGT, :], p3[h][0], [p3[h][1]], [wb[h]], "act")
            if getattr(K, 'scan_stage', 99) <= 6:
                return
            if getattr(K, 'scan_serial', False):
                P.phase_end()
            pv = []
            for h in range(8):
                w = wk[h]
                a, ab = psD.next()
                P.op("pe", lambda e, a=a, w=w, h=h: e.matmul(a, lhsT=w[:, WT, :], rhs=S[d][h][:], start=True, stop=True),
                     reads=[wb[h], bS[d][h]], writes=[ab])
                pv.append((a, ab))
            for h in range(8):
                w = wk[h]
                P.op("dve", lambda e, w=w, h=h: e.tensor_tensor(out=w[:, VN, :], in0=w[:, U, :], in1=pv[h][0], op=ALU.subtract),
                     reads=[pv[h][1], wb[h]], writes=[wb[h]])
            po, pd = [], []
            for h in range(8):
                w = wk[h]
                if with_out:
                    a, ab = psA.next()
                    P.op("pe", lambda e, a=a, w=w, h=h: e.matmul(a, lhsT=w[:, QGT, :], rhs=S[d][h][:], start=True, stop=False),
                         reads=[wb[h], bS[d][h]], writes=[ab])
                    P.op("pe", lambda e, a=a, w=w: e.matmul(a, lhsT=w[:, QAT, :], rhs=w[:, VN, :], start=False, stop=True),
                         reads=[wb[h]], writes=[ab])
                    po.append((a, ab))
                a, ab = psD.next()
                P.op("pe", lambda e, a=a, w=w, h=h: e.matmul(a, lhsT=dg[:, KD, h, :], rhs=w[:, VN, :], start=True, stop=True),
                     reads=[wb[h], dgb], writes=[ab])
                pd.append((a, ab))
            if with_out:
                ob, obb = obr.next()
            for h in range(8):
                P.op("dve", lambda e, h=h: e.scalar_tensor_tensor(
                    out=S[d][h][:], in0=S[d][h][:], scalar=sm[:, GL, h:h + 1], in1=pd[h][0], op0=ALU.mult, op1=ALU.add),
                    reads=[pd[h][1], smb], writes=[bS[d][h]])
                if with_out:
                    P.op("act", lambda e, h=h: e.activation(out=ob[:, h * 128:(h + 1) * 128], in_=po[h][0], func=AF.Copy),
                         reads=[po[h][1]], writes=[obb])
            if with_out:
                dst = K.o_dir[d]
                P.dma("sp", dst[t0 - CTX:t0 - CTX + 128, :], ob[:], reads=[obb])

        for s in range(getattr(K, 'scan_steps', NB)):
            job(order[0][s], 0)
            job(order[1][s], 1)
        P.phase_end()


def phase_dnout(K):
    nc, P = K.nc, K.P
    with contextlib.ExitStack() as st:
        gn = sb(st, nc, "do_gn", [128, 1024], F32)
        ofr = Rot([sb(st, nc, f"do_of{i}", [128, 1024], F32) for i in range(2)])
        obr = Rot([sb(st, nc, f"do_ob{i}", [128, 1024], F32) for i in range(2)])
        sgr = Rot([sb(st, nc, f"do_sg{i}", [128, 1024], F32) for i in range(2)])
        sqr = Rot([sb(st, nc, f"do_sq{i}", [128, 1024], F32) for i in range(2)])
        msr = Rot([sb(st, nc, f"do_ms{i}", [128, 8], F32) for i in range(2)])
        ygr = Rot([sb(st, nc, f"do_yg{i}", [128, 8, 128], BF16) for i in range(2)])
        psr = ps_banks(st, nc, 4, "dops")
        bg = Buf()
        P.dma("sp", gn[:], K.dn_gn_l, writes=[bg])
        n = 0
        for tt in range(L // 128):
            t0 = tt * 128
            of, ofb = ofr.next()
            ob, obb = obr.next()
            sg, sgb = sgr.next()
            sq, sqb = sqr.next()
            ms, msb = msr.next()
            yg, ygb = ygr.next()
            P.dma("sp", of[:], K.o_dir[0][t0:t0 + 128, :], writes=[ofb])
            P.dma("sp", ob[:], K.o_dir[1][t0:t0 + 128, :], writes=[obb])
            P.dma("sp", sg[:], K.sgate[CTX + t0:CTX + t0 + 128, :], writes=[sgb])
            P.op("dve", lambda e, of=of, ob=ob: e.tensor_tensor(out=of[:], in0=of[:], in1=ob[:], op=ALU.add),
                 reads=[obb], writes=[ofb])
            P.op("act", lambda e, of=of, sq=sq: e.activation(out=sq[:], in_=of[:], func=AF.Square), reads=[ofb], writes=[sqb])
            P.op("dve", lambda e, sq=sq, ms=ms: e.tensor_reduce(out=ms[:], in_=sq[:].rearrange("p (h c) -> p h c", c=128),
                                                               axis=AX.X, op=ALU.add), reads=[sqb], writes=[msb])
            P.op("act", lambda e, ms=ms: e.activation(out=ms[:], in_=ms[:], func=AF.Sqrt, bias=K.epsc[:, 0:1], scale=1.0 / 128),
                 reads=[msb], writes=[msb])
            P.op("dve", lambda e, ms=ms: e.reciprocal(out=ms[:], in_=ms[:]), reads=[msb], writes=[msb])
            P.op("dve", lambda e, of=of, ms=ms: e.tensor_tensor(
                out=of[:].rearrange("p (h c) -> p h c", c=128), in0=of[:].rearrange("p (h c) -> p h c", c=128),
                in1=ms[:].unsqueeze(2).to_broadcast([128, 8, 128]), op=ALU.mult), reads=[msb], writes=[ofb])
            P.op("pool", lambda e, of=of, sg=sg: e.tensor_tensor(out=sg[:], in0=sg[:], in1=gn[:], op=ALU.mult),
                 reads=[bg], writes=[sgb])
            P.op("pool", lambda e, of=of, sg=sg: e.tensor_tensor(out=of[:], in0=of[:], in1=sg[:], op=ALU.mult),
                 reads=[sgb], writes=[ofb])
            for half in range(2):
                ps, pb = psr.next()
                for j in range(4):
                    cc = half * 4 + j
                    P.op("pe", lambda e, ps=ps, of=of, j=j, cc=cc: e.transpose(
                        out=ps[:, j * 128:(j + 1) * 128], in_=of[:, cc * 128:(cc + 1) * 128], identity=K.ident[:]),
                        reads=[ofb], writes=[pb])
                if n % 2 == 0:
                    P.op("act", lambda e, ps=ps, yg=yg, half=half: e.activation(
                        out=yg[:, half * 4:(half + 1) * 4, :], in_=ps[:, :].rearrange("p (j t) -> p j t", t=128),
                        func=AF.Copy), reads=[pb], writes=[ygb])
                else:
                    P.op("dve", lambda e, ps=ps, yg=yg, half=half: e.tensor_copy(
                        out=yg[:, half * 4:(half + 1) * 4, :], in_=ps[:, :].rearrange("p (j t) -> p j t", t=128)),
                        reads=[pb], writes=[ygb])
                n += 1
            P.dma("sp", K.ygT[:, :, t0:t0 + 128].rearrange("c p t -> p c t"), yg[:], reads=[ygb])
        P.phase_end()


def phase_outproj(K, srcT, W, l, m):
    nc, P = K.nc, K.P
    with contextlib.ExitStack() as st:
        Wb = sb(st, nc, "op_W", [128, 8, 1024], BF16)
        srr = Rot([sb(st, nc, f"op_src{i}", [128, 8, 512], BF16) for i in range(2)])
        hr = Rot([sb(st, nc, f"op_h{i}", [128, 8, 512], F32) for i in range(2)])
        psr = ps_banks(st, nc, 4, "opps")
        bW = Buf()
        for cc in range(8):
            P.dma("pool", Wb[:, cc, :], W[cc * 128:(cc + 1) * 128, :], writes=[bW])
        for g in range(L // 512):
            src, sbb = srr.next()
            h, hb = hr.next()
            P.dma("sp", src[:], srcT[:, :, g * 512:(g + 1) * 512].rearrange("c p t -> p c t"), writes=[sbb])
            P.dma("sp", h[:], K.hT[:, :, g * 512:(g + 1) * 512].rearrange("c p t -> p c t"), writes=[hb])
            for dc in range(8):
                ps, pb = psr.next()
                for cc in range(8):
                    P.op("pe", lambda e, ps=ps, src=src, cc=cc, dc=dc: e.matmul(
                        ps[:, :], lhsT=Wb[:, cc, dc * 128:(dc + 1) * 128], rhs=src[:, cc, :],
                        start=(cc == 0), stop=(cc == 7)), reads=[bW, sbb], writes=[pb])
                P.op("dve", lambda e, ps=ps, h=h, dc=dc: e.scalar_tensor_tensor(
                    out=h[:, dc, :], in0=ps[:, :], scalar=K.MOD[:, l, m * 8 + dc, 0:1], in1=h[:, dc, :],
                    op0=ALU.mult, op1=ALU.add), reads=[pb, K.bMOD], writes=[hb])
            P.dma("sp", K.hT[:, :, g * 512:(g + 1) * 512].rearrange("c p t -> p c t"), h[:], reads=[hb])
        P.phase_end()


def phase_peer_tables(K, l):
    nc, P = K.nc, K.P
    with contextlib.ExitStack() as st:
        ur = Rot([sb(st, nc, f"pt_u{i}", [128, 8, 128], BF16) for i in range(3)])
        vr = Rot([sb(st, nc, f"pt_v{i}", [128, 1024], BF16) for i in range(3)])
        for i in range(128):
            u, ub = ur.next()
            v, vb = vr.next()
            for kc in range(8):
                P.dma("pool", u[:, kc, :], K.peer_u_l[l, i, kc], writes=[ub])
            P.dma("pool", v[:], K.peer_v[l, i * 128:(i + 1) * 128, :], writes=[vb])
            P.dma("sp", K.Ub[i], u[:], reads=[ub])
            P.dma("sp", K.Vb[i], v[:], reads=[vb])
        P.phase_end()


def phase_peer(K, l):
    nc, P = K.nc, K.P
    NEG = -1.0e30
    with contextlib.ExitStack() as st:
        Wq = sb(st, nc, "pe_Wq", [128, 8, 2048], BF16)
        kT = sb(st, nc, "pe_kT", [128, 16, 128], F32)
        xpr = Rot([sb(st, nc, f"pe_xp{i}", [128, 8, 128], BF16) for i in range(2)])
        qsb = sb(st, nc, "pe_q", [128, 16, 128], F32)
        sc = sb(st, nc, "pe_sc", [128, 16, 128], F32)
        sc2 = sb(st, nc, "pe_sc2", [128, 16, 128], F32)
        v16 = sb(st, nc, "pe_v16", [128, 16, 16], F32)
        cand = sb(st, nc, "pe_cand", [128, 8, 256], F32)
        cand2 = sb(st, nc, "pe_cand2", [128, 8, 256], F32)
        c16 = sb(st, nc, "pe_c16", [128, 8, 16], F32)
        e16 = sb(st, nc, "pe_e16", [128, 8, 16], F32)
        sm = sb(st, nc, "pe_sm", [128, 4, 8], F32)
        sumr = Rot([sb(st, nc, f"pe_sum{i}", [128, 2048], F32) for i in range(2)])
        mkr = Rot([sb(st, nc, f"pe_mk{i}", [128, 2048], F32) for i in range(2)])
        Er = Rot([sb(st, nc, f"pe_E{i}", [128, 2048], F32) for i in range(2)])
        acc = sb(st, nc, "pe_acc", [128, 2048], F32)
        WallT = sb(st, nc, "pe_WallT", [128, 128, 128], BF16)
        ur = Rot([sb(st, nc, f"pe_u{i}", [128, 8, 128], BF16) for i in range(3)])
        vr = Rot([sb(st, nc, f"pe_v{i}", [128, 1024], BF16) for i in range(3)])
        gr = Rot([sb(st, nc, f"pe_g{i}", [128, 128], F32) for i in range(2)])
        cfr = Rot([sb(st, nc, f"pe_cf{i}", [128, 128], BF16) for i in range(2)])
        ysb = sb(st, nc, "pe_y", [128, 1024], F32)
        hsb = sb(st, nc, "pe_h", [128, 8, 128], F32)
        gps = ps_banks(st, nc, 4, "pegps")
        hps = ps_banks(st, nc, 2, "pehps")
        yps = ps_banks(st, nc, 2, "peyps")
        bW, bk, bq, bsc, bsc2, bv, bc, bc2, bc16, be, bsm, bacc, bwall, by, bh = [Buf() for _ in range(15)]
        for kc in range(8):
            P.dma("pool", Wq[:, kc, :], K.peer_w_q[l, kc * 128:(kc + 1) * 128, :], writes=[bW])
        P.dma("sp", kT[:], K.peer_kT_l[l], writes=[bk])
        for g in range(getattr(K, 'peer_groups', L // 128)):
            t0 = g * 128
            xp, xb = xpr.next()
            P.dma("sp", xp[:], K.hnT[:, :, CTX + t0:CTX + t0 + 128].rearrange("c p t -> p c t"), writes=[xb])
            P.dma("sp", hsb[:], K.hT[:, :, t0:t0 + 128].rearrange("c p t -> p c t"), writes=[bh])
            for b4 in range(4):
                ps, pb = gps.next()
                for j in range(4):
                    hp = b4 * 4 + j
                    for kc in range(8):
                        P.op("pe", lambda e, ps=ps, xp=xp, j=j, hp=hp, kc=kc: e.matmul(
                            ps[:, j * 128:(j + 1) * 128], lhsT=Wq[:, kc, hp * 128:(hp + 1) * 128], rhs=xp[:, kc, :],
                            start=(kc == 0), stop=(kc == 7)), reads=[bW, xb], writes=[pb])
                P.op("act", lambda e, ps=ps, b4=b4: e.activation(
                    out=qsb[:, b4 * 4:(b4 + 1) * 4, :], in_=ps[:, :].rearrange("p (j t) -> p j t", t=128), func=AF.Copy),
                    reads=[pb], writes=[bq])
            for b4 in range(4):
                ps, pb = gps.next()
                for j in range(4):
                    hp = b4 * 4 + j
                    P.op("pe", lambda e, ps=ps, j=j, hp=hp: e.matmul(
                        ps[:, j * 128:(j + 1) * 128], lhsT=qsb[:, hp, :], rhs=kT[:, hp, :], start=True, stop=True),
                        reads=[bq, bk], writes=[pb])
                P.op("act", lambda e, ps=ps, b4=b4: e.activation(
                    out=sc[:, b4 * 4:(b4 + 1) * 4, :], in_=ps[:, :].rearrange("p (j t) -> p j t", t=128), func=AF.Copy),
                    reads=[pb], writes=[bsc])
            for hp in range(16):
                P.op("dve", lambda e, hp=hp: e.max(out=v16[:, hp, 0:8], in_=sc[:, hp, :]), reads=[bsc], writes=[bv])
                P.op("dve", lambda e, hp=hp: e.match_replace(out=sc2[:, hp, :], in_to_replace=v16[:, hp, 0:8],
                                                            in_values=sc[:, hp, :], imm_value=NEG),
                     reads=[bsc, bv], writes=[bsc2])
                P.op("dve", lambda e, hp=hp: e.max(out=v16[:, hp, 8:16], in_=sc2[:, hp, :]), reads=[bsc2], writes=[bv])
            v4 = v16[:].rearrange("p (h two) k -> p h two k", two=2)
            P.op("dve", lambda e: e.tensor_tensor(
                out=cand[:].rearrange("p h (a b) -> p h a b", b=16),
                in0=v4[:, :, 0, :].unsqueeze(3).to_broadcast([128, 8, 16, 16]),
                in1=v4[:, :, 1, :].unsqueeze(2).to_broadcast([128, 8, 16, 16]), op=ALU.add), reads=[bv], writes=[bc])
            for h in range(8):
                P.op("dve", lambda e, h=h: e.max(out=c16[:, h, 0:8], in_=cand[:, h, :]), reads=[bc], writes=[bc16])
                P.op("dve", lambda e, h=h: e.match_replace(out=cand2[:, h, :], in_to_replace=c16[:, h, 0:8],
                                                          in_values=cand[:, h, :], imm_value=NEG),
                     reads=[bc, bc16], writes=[bc2])
                P.op("dve", lambda e, h=h: e.max(out=c16[:, h, 8:16], in_=cand2[:, h, :]), reads=[bc2], writes=[bc16])
            P.op("dve", lambda e: e.tensor_copy(out=sm[:, 0, :], in_=c16[:, :, 15]), reads=[bc16], writes=[bsm])
            P.op("dve", lambda e: e.tensor_scalar(out=sm[:, 1, :], in0=c16[:, :, 0], scalar1=-1.0, scalar2=None,
                                                  op0=ALU.mult), reads=[bc16], writes=[bsm])
            P.op("dve", lambda e: e.tensor_tensor(out=e16[:], in0=c16[:], in1=sm[:, 1, :].unsqueeze(2).to_broadcast([128, 8, 16]),
                                                  op=ALU.add), reads=[bc16, bsm], writes=[be])
            P.op("act", lambda e: e.activation(out=e16[:], in_=e16[:], func=AF.Exp), reads=[be], writes=[be])
            P.op("dve", lambda e: e.tensor_reduce(out=sm[:, 2, :], in_=e16[:], axis=AX.X, op=ALU.add), reads=[be], writes=[bsm])
            P.op("dve", lambda e: e.reciprocal(out=sm[:, 3, :], in_=sm[:, 2, :]), reads=[bsm], writes=[bsm])
            for q4 in range(8):
                for h in range(8):
                    su, sub_ = sumr.next()
                    mk_, mkb = mkr.next()
                    E_, Eb = Er.next()
                    P.op("dve", lambda e, su=su, h=h, q4=q4: e.tensor_tensor(
                        out=su[:].rearrange("p (i j) -> p i j", j=128),
                        in0=sc[:, 2 * h, q4 * 16:(q4 + 1) * 16].unsqueeze(2).to_broadcast([128, 16, 128]),
                        in1=sc[:, 2 * h + 1, :].unsqueeze(1).to_broadcast([128, 16, 128]), op=ALU.add),
                        reads=[bsc], writes=[sub_])
                    P.op("dve", lambda e, su=su, mk_=mk_, h=h: e.tensor_scalar(
                        out=mk_[:], in0=su[:], scalar1=sm[:, 0, h:h + 1], scalar2=None, op0=ALU.is_ge),
                        reads=[sub_, bsm], writes=[mkb])
                    P.op("act", lambda e, su=su, E_=E_, h=h: e.activation(
                        out=E_[:], in_=su[:], func=AF.Exp, bias=sm[:, 1, h:h + 1], scale=1.0), reads=[sub_, bsm], writes=[Eb])
                    P.op("pool", lambda e, E_=E_, mk_=mk_: e.tensor_tensor(out=E_[:], in0=E_[:], in1=mk_[:], op=ALU.mult),
                         reads=[mkb], writes=[Eb])
                    if h == 0:
                        P.op("dve", lambda e, E_=E_, h=h: e.tensor_scalar(
                            out=acc[:], in0=E_[:], scalar1=sm[:, 3, h:h + 1], scalar2=None, op0=ALU.mult),
                            reads=[Eb, bsm], writes=[bacc])
                    else:
                        P.op("dve", lambda e, E_=E_, h=h: e.scalar_tensor_tensor(
                            out=acc[:], in0=E_[:], scalar=sm[:, 3, h:h + 1], in1=acc[:], op0=ALU.mult, op1=ALU.add),
                            reads=[Eb, bsm], writes=[bacc])
                for b8 in range(4):
                    ps, pb = gps.next()
                    for j in range(4):
                        ii = b8 * 4 + j
                        P.op("pe", lambda e, ps=ps, j=j, ii=ii: e.transpose(
                            out=ps[:, j * 128:(j + 1) * 128], in_=acc[:, ii * 128:(ii + 1) * 128], identity=K.ident[:]),
                            reads=[bacc], writes=[pb])
                    i0 = q4 * 16 + b8 * 4
                    P.op("act", lambda e, ps=ps, i0=i0: e.activation(
                        out=WallT[:, i0:i0 + 4, :], in_=ps[:, :].rearrange("p (j t) -> p j t", t=128), func=AF.Copy),
                        reads=[pb], writes=[bwall])
            y0, y0b = yps.next()
            y1, y1b = yps.next()
            for i in range(128):
                u, ub = ur.next()
                v, vb = vr.next()
                P.dma("sp", u[:], K.Ub[i], writes=[ub])
                P.dma("sp", v[:], K.Vb[i], writes=[vb])
                hp_, hpb = hps.next()
                for kc in range(8):
                    P.op("pe", lambda e, hp_=hp_, u=u, xp=xp, kc=kc: e.matmul(
                        hp_[:, 0:128], lhsT=u[:, kc, :], rhs=xp[:, kc, :], start=(kc == 0), stop=(kc == 7)),
                        reads=[ub, xb], writes=[hpb])
                g_, gb = gr.next()
                P.op("act", lambda e, hp_=hp_, g_=g_: e.activation(out=g_[:], in_=hp_[:, 0:128], func=AF.Gelu),
                     reads=[hpb], writes=[gb])
                cf, cfb = cfr.next()
                P.op("dve", lambda e, g_=g_, cf=cf, i=i: e.tensor_tensor(out=cf[:], in0=g_[:], in1=WallT[:, i, :], op=ALU.mult),
                     reads=[gb, bwall], writes=[cfb])
                P.op("pe", lambda e, cf=cf, v=v, i=i: e.matmul(y0[:, :], lhsT=cf[:], rhs=v[:, 0:512], start=(i == 0), stop=(i == 127)),
                     reads=[cfb, vb], writes=[y0b])
                P.op("pe", lambda e, cf=cf, v=v, i=i: e.matmul(y1[:, :], lhsT=cf[:], rhs=v[:, 512:1024], start=(i == 0), stop=(i == 127)),
                     reads=[cfb, vb], writes=[y1b])
            P.op("act", lambda e: e.activation(out=ysb[:, 0:512], in_=y0[:, :], func=AF.Copy), reads=[y0b], writes=[by])
            P.op("act", lambda e: e.activation(out=ysb[:, 512:1024], in_=y1[:, :], func=AF.Copy), reads=[y1b], writes=[by])
            for half in range(2):
                ps, pb = gps.next()
                for j in range(4):
                    dc = half * 4 + j
                    P.op("pe", lambda e, ps=ps, j=j, dc=dc: e.transpose(
                        out=ps[:, j * 128:(j + 1) * 128], in_=ysb[:, dc * 128:(dc + 1) * 128], identity=K.ident[:]),
                        reads=[by], writes=[pb])
                for j in range(4):
                    dc = half * 4 + j
                    P.op("dve", lambda e, ps=ps, j=j, dc=dc: e.scalar_tensor_tensor(
                        out=hsb[:, dc, :], in0=ps[:, j * 128:(j + 1) * 128], scalar=K.MOD[:, l, 5 * 8 + dc, 0:1],
                        in1=hsb[:, dc, :], op0=ALU.mult, op1=ALU.add), reads=[pb, K.bMOD], writes=[bh])
            P.dma("sp", K.hT[:, :, t0:t0 + 128].rearrange("c p t -> p c t"), hsb[:], reads=[bh])
        P.phase_end()


def phase_hyin(K):
    nc, P = K.nc, K.P
    with contextlib.ExitStack() as st:
        Wb = sb(st, nc, "hi_Wb", [128, 8, 3072], BF16)
        cw = sb(st, nc, "hi_cw", [128, 24, 3], F32)
        cb = sb(st, nc, "hi_cb", [128, 24], F32)
        hnr = Rot([sb(st, nc, f"hi_hn{i}", [128, 8, 512], BF16) for i in range(2)])
        ztr = Rot([sb(st, nc, f"hi_zt{i}", [128, 24, 512], F32) for i in range(2)])
        zps = ps_banks(st, nc, 4, "hiz")
        bW, bcw = Buf(), Buf()
        for kc in range(8):
            for c0 in range(0, 3072, 1536):
                P.dma("pool", Wb[:, kc, c0:c0 + 1536], K.hy_w_in[kc * 128:(kc + 1) * 128, c0:c0 + 1536], writes=[bW])
        P.dma("sp", cw[:], K.hy_cw_l, writes=[bcw])
        P.dma("sp", cb[:], K.hy_cb_l, writes=[bcw])
        for g in range(L // 512):
            hn, hb = hnr.next()
            zt, zb = ztr.next()
            P.dma("sp", hn[:], K.hnT[:, :, CTX + g * 512:CTX + (g + 1) * 512].rearrange("c p t -> p c t"), writes=[hb])
            for cc in range(24):
                ps, pb = zps.next()
                for kc in range(8):
                    P.op("pe", lambda e, ps=ps, hn=hn, cc=cc, kc=kc: e.matmul(
                        ps[:, :], lhsT=Wb[:, kc, cc * 128:(cc + 1) * 128], rhs=hn[:, kc, :],
                        start=(kc == 0), stop=(kc == 7)), reads=[bW, hb], writes=[pb])
                P.op("dve", lambda e, ps=ps, zt=zt, cc=cc: e.tensor_scalar(
                    out=zt[:, cc, :], in0=ps[:, :], scalar1=cw[:, cc, 1:2], scalar2=cb[:, cc:cc + 1],
                    op0=ALU.mult, op1=ALU.add), reads=[pb, bcw], writes=[zb])
                for k in (0, 2):
                    s = k - 1
                    lo, hi = max(0, -s), 64 - max(0, s)
                    P.op("dve", lambda e, ps=ps, zt=zt, cc=cc, k=k, s=s, lo=lo, hi=hi: e.scalar_tensor_tensor(
                        out=zt[:, cc, :].rearrange("p (r w) -> p r w", w=64)[:, :, lo:hi],
                        in0=ps[:, :].rearrange("p (r w) -> p r w", w=64)[:, :, lo + s:hi + s],
                        scalar=cw[:, cc, k:k + 1],
                        in1=zt[:, cc, :].rearrange("p (r w) -> p r w", w=64)[:, :, lo:hi],
                        op0=ALU.mult, op1=ALU.add), reads=[pb, bcw], writes=[zb])
            P.dma("sp", K.zT[:, :, g * 512:(g + 1) * 512].rearrange("c p t -> p c t"), zt[:], reads=[zb])
        P.phase_end()


def _wrap_pi(P, t, tb, tmpa, tab, n, cols):
    PI = math.pi
    for _ in range(2):
        P.op("dve", lambda e: e.tensor_scalar(out=tmpa[0:n, 0:cols], in0=t[0:n, 0:cols], scalar1=PI, scalar2=-2 * PI,
                                              op0=ALU.is_gt, op1=ALU.mult), reads=[tb], writes=[tab])
        P.op("dve", lambda e: e.tensor_tensor(out=t[0:n, 0:cols], in0=t[0:n, 0:cols], in1=tmpa[0:n, 0:cols], op=ALU.add),
             reads=[tab], writes=[tb])
        P.op("dve", lambda e: e.tensor_scalar(out=tmpa[0:n, 0:cols], in0=t[0:n, 0:cols], scalar1=-PI, scalar2=2 * PI,
                                              op0=ALU.is_lt, op1=ALU.mult), reads=[tb], writes=[tab])
        P.op("dve", lambda e: e.tensor_tensor(out=t[0:n, 0:cols], in0=t[0:n, 0:cols], in1=tmpa[0:n, 0:cols], op=ALU.add),
             reads=[tab], writes=[tb])


def phase_hyfilt(K):
    nc, P = K.nc, K.P
    with contextlib.ExitStack() as st:
        w1 = sb(st, nc, "hf_w1", [33, 64], F32)
        w2 = sb(st, nc, "hf_w2", [64, 64], F32)
        w3 = sb(st, nc, "hf_w3", [64, 64], F32)
        w4 = sb(st, nc, "hf_w4", [64, 4096], F32)
        pv = sb(st, nc, "hf_pv", [64, 8], F32)
        dl = sb(st, nc, "hf_dl", [128, 8], F32)
        zpr = Rot([sb(st, nc, f"hf_zp{i}", [33, 512], F32) for i in range(2)])
        tpr = Rot([sb(st, nc, f"hf_tp{i}", [128, 512], F32) for i in range(2)])
        hr = Rot([sb(st, nc, f"hf_h{i}", [64, 512], F32) for i in range(3)])
        tmpa = sb(st, nc, "hf_tmp", [64, 512], F32)
        dcr = Rot([sb(st, nc, f"hf_dc{i}", [128, 8, 512], F32) for i in range(2)])
        fr_ = Rot([sb(st, nc, f"hf_f{i}", [128, 512], F32) for i in range(4)])
        psr = ps_banks(st, nc, 4, "hfps")
        bw, btm = Buf(), Buf()
        P.dma("sp", w1[:], K.hy_f_w1, writes=[bw])
        P.dma("sp", w2[:], K.hy_f_w2, writes=[bw])
        P.dma("sp", w3[:], K.hy_f_w3, writes=[bw])
        P.dma("sp", w4[:], K.hy_f_w4, writes=[bw])
        P.dma("sp", pv[:, 0:4], K.hy_fpv_l, writes=[bw])
        P.dma("sp", dl[:], K.hy_ndelta_l, writes=[bw])
        P.op("dve", lambda e: e.tensor_tensor(out=pv[:, 4:7], in0=pv[:, 1:4], in1=pv[:, 0:1].to_broadcast([64, 3]), op=ALU.mult),
             reads=[bw], writes=[bw])
        ws = [w1, w2, w3]
        for pc in range(L // 512):
            zp, zpb = zpr.next()
            tp, tpb = tpr.next()
            P.dma("sp", zp[:], K.hy_zpos[:, pc * 512:(pc + 1) * 512], writes=[zpb])
            P.dma("sp", tp[:], K.hy_tpos[:, pc * 512:(pc + 1) * 512], writes=[tpb])
            cur, curb, kdim = zp, zpb, 33
            for li in range(3):
                ps, pb = psr.next()
                P.op("pe", lambda e, ps=ps, cur=cur, li=li, kdim=kdim: e.matmul(
                    ps[0:64, :], lhsT=ws[li][0:kdim, :], rhs=cur[0:kdim, :], start=True, stop=True),
                    reads=[bw, curb], writes=[pb])
                h, hb = hr.next()
                P.op("dve", lambda e, ps=ps, h=h, li=li: e.tensor_scalar(
                    out=h[:], in0=ps[0:64, :], scalar1=pv[:, 0:1], scalar2=pv[:, 4 + li:5 + li], op0=ALU.mult, op1=ALU.add),
                    reads=[pb, bw], writes=[hb])
                _wrap_pi(P, h, hb, tmpa, btm, 64, 512)
                P.op("act", lambda e, h=h: e.activation(out=h[:], in_=h[:], func=AF.Sin), reads=[hb], writes=[hb])
                cur, curb, kdim = h, hb, 64
            dc, dcb = dcr.next()
            for cc in range(8):
                P.op("act", lambda e, dc=dc, tp=tp, cc=cc: e.activation(out=dc[:, cc, :], in_=tp[:], func=AF.Exp,
                                                                        scale=dl[:, cc:cc + 1]), reads=[tpb, bw], writes=[dcb])
            for o in range(2):
                for d_ in range(2):
                    for cc in range(8):
                        col = o * 2048 + d_ * 1024 + cc * 128
                        ps, pb = psr.next()
                        P.op("pe", lambda e, ps=ps, cur=cur, col=col: e.matmul(
                            ps[:, :], lhsT=w4[:, col:col + 128], rhs=cur[:, :], start=True, stop=True),
                            reads=[bw, curb], writes=[pb])
                        f_, fb = fr_.next()
                        P.op("dve", lambda e, ps=ps, f_=f_, dc=dc, cc=cc: e.tensor_tensor(
                            out=f_[:], in0=ps[:, :], in1=dc[:, cc, :], op=ALU.mult), reads=[pb, dcb], writes=[fb])
                        P.dma("sp", K.filtT[o, d_, cc, :, pc * 512:(pc + 1) * 512], f_[:], reads=[fb])
        P.phase_end()


def phase_hyconv_direct(K):
    nc, P = K.nc, K.P
    with contextlib.ExitStack() as st:
        u = sb(st, nc, "hc_u", [128, L], F32)
        x = sb(st, nc, "hc_x", [128, L], F32)
        hf = sb(st, nc, "hc_hf", [128, L], F32)
        hb_ = sb(st, nc, "hc_hb", [128, L], F32)
        acc = sb(st, nc, "hc_acc", [128, L], F32)
        zo = sb(st, nc, "hc_zo", [128, L], BF16)
        sk = sb(st, nc, "hc_sk", [128, 2, 8], F32)
        s0 = sb(st, nc, "hc_s0", [128, 1], F32)
        bu, bx, bhf, bhb, bacc, bzo, bsk, bs0 = [Buf() for _ in range(8)]
        P.dma("sp", sk[:], K.hy_skip_l, writes=[bsk])
        nl = getattr(K, 'hy_nlags', L)
        for cc in range(getattr(K, 'hy_chunks', 8)):
            P.dma("sp", u[:], K.zT[cc], writes=[bu])
            for o in range(2):
                P.dma("sp", x[:], K.zT[8 * (o + 1) + cc], writes=[bx])
                P.dma("sp", hf[:], K.filtT[o, 0, cc], writes=[bhf])
                P.dma("sp", hb_[:], K.filtT[o, 1, cc], writes=[bhb])
                P.op("dve", lambda e, o=o, cc=cc: e.tensor_tensor(out=s0[:], in0=hf[:, 0:1], in1=sk[:, o, cc:cc + 1], op=ALU.add),
                     reads=[bhf, bsk], writes=[bs0])
                P.op("dve", lambda e: e.tensor_scalar(out=acc[:], in0=u[:], scalar1=s0[:, 0:1], scalar2=None, op0=ALU.mult),
                     reads=[bu, bs0], writes=[bacc])
                for lag in range(1, nl):
                    P.op("dve", lambda e, lag=lag: e.scalar_tensor_tensor(
                        out=acc[:, lag:L], in0=u[:, 0:L - lag], scalar=hf[:, lag:lag + 1], in1=acc[:, lag:L],
                        op0=ALU.mult, op1=ALU.add), reads=[bu, bhf], writes=[bacc])
                    P.op("dve", lambda e, lag=lag: e.scalar_tensor_tensor(
                        out=acc[:, 0:L - lag], in0=u[:, lag:L], scalar=hb_[:, lag:lag + 1], in1=acc[:, 0:L - lag],
                        op0=ALU.mult, op1=ALU.add), reads=[bu, bhb], writes=[bacc])
                if o == 0:
                    P.op("dve", lambda e: e.tensor_tensor(out=u[:], in0=acc[:], in1=x[:], op=ALU.mult),
                         reads=[bacc, bx], writes=[bu])
                else:
                    P.op("dve", lambda e: e.tensor_tensor(out=zo[:], in0=acc[:], in1=x[:], op=ALU.mult),
                         reads=[bacc, bx], writes=[bzo])
                    P.dma("sp", K.z2T[cc], zo[:], reads=[bzo])
        P.phase_end()


def phase_final(K):
    nc, P = K.nc, K.P
    with contextlib.ExitStack() as st:
        g = sb(st, nc, "fn_g", [128, 8], F32)
        hin = Rot([sb(st, nc, f"fn_in{i}", [128, 8, 512], F32) for i in range(2)])
        sq = Rot([sb(st, nc, f"fn_sq{i}", [128, 8, 512], BF16) for i in range(2)])
        sd = Rot([sb(st, nc, f"fn_sd{i}", [128, 512], F32) for i in range(2)])
        hn = Rot([sb(st, nc, f"fn_hn{i}", [128, 8, 512], F32) for i in range(2)])
        outr = Rot([sb(st, nc, f"fn_o{i}", [128, 4, 1024], F32) for i in range(2)])
        psr = ps_banks(st, nc, 2, "fnps")
        tpr = ps_banks(st, nc, 4, "fntp")
        bg = Buf()
        P.dma("sp", g[:], K.finalg_l, writes=[bg])
        n = 0
        for grp in range(L // 512):
            h, hb = hin.next()
            P.dma("sp", h[:], K.hT[:, :, grp * 512:(grp + 1) * 512].rearrange("c p t -> p c t"), writes=[hb])
            q, qb = sq.next()
            P.op("act", lambda e, q=q, h=h: e.activation(out=q[:], in_=h[:], func=AF.Square), reads=[hb], writes=[qb])
            ps, pb = psr.next()
            for dc in range(8):
                P.op("pe", lambda e, ps=ps, q=q, dc=dc: e.matmul(ps[:, :], lhsT=K.onesb[:], rhs=q[:, dc, :],
                                                               start=(dc == 0), stop=(dc == 7)), reads=[qb], writes=[pb])
            d_, db = sd.next()
            P.op("act", lambda e, d_=d_, ps=ps: e.activation(out=d_[:], in_=ps[:, :], func=AF.Sqrt, bias=K.epsc[:, 0:1],
                                                            scale=1.0 / D), reads=[pb], writes=[db])
            P.op("dve", lambda e, d_=d_: e.reciprocal(out=d_[:], in_=d_[:]), reads=[db], writes=[db])
            hn_, hnb = hn.next()
            for dc in range(8):
                P.op("dve", lambda e, hn_=hn_, h=h, d_=d_, dc=dc: e.scalar_tensor_tensor(
                    out=hn_[:, dc, :], in0=h[:, dc, :], scalar=g[:, dc:dc + 1], in1=d_[:], op0=ALU.mult, op1=ALU.mult),
                    reads=[hb, db, bg], writes=[hnb])
            ot, otb = outr.next()
            for a in range(4):
                for half in range(2):
                    tp, tpb = tpr.next()
                    for j in range(4):
                        dc = half * 4 + j
                        P.op("pe", lambda e, tp=tp, hn_=hn_, a=a, j=j, dc=dc: e.transpose(
                            out=tp[:, j * 128:(j + 1) * 128], in_=hn_[:, dc, a * 128:(a + 1) * 128], identity=K.ident[:]),
                            reads=[hnb], writes=[tpb])
                    if n % 2 == 0:
                        P.op("act", lambda e, tp=tp, ot=ot, a=a, half=half: e.activation(
                            out=ot[:, a, half * 512:(half + 1) * 512], in_=tp[:, :], func=AF.Copy), reads=[tpb], writes=[otb])
                    else:
                        P.op("dve", lambda e, tp=tp, ot=ot, a=a, half=half: e.tensor_copy(
                            out=ot[:, a, half * 512:(half + 1) * 512], in_=tp[:, :]), reads=[tpb], writes=[otb])
                    n += 1
            P.dma("sp", K.out[grp * 512:(grp + 1) * 512, :].rearrange("(a p) d -> p a d", p=128), ot[:], reads=[otb])
        P.phase_end()


STAGES = ["xt", "mod", "norm0", "dnin", "dnscan", "dnout", "peer0", "hyena", "peer1", "final"]
_CACHE = {}


def kernel(**inputs):
    inputs = {k: np.asarray(v) for k, v in inputs.items()}
    if "nc" not in _CACHE:
        _CACHE["nc"] = build(STAGES)[0]
    nc = _CACHE["nc"]
    shared = host_inputs(inputs, 0)
    in_maps = []
    for b in range(8):
        m = dict(shared)
        m["x"] = np.ascontiguousarray(inputs["x"][b])
        m["ctx"] = np.ascontiguousarray(inputs["ctx"][b])
        m["c_l"] = np.ascontiguousarray(inputs["c"][b].reshape(8, 128).T)
        in_maps.append(m)
    res = run_bass_kernel_spmd(nc, in_maps, core_ids=list(range(8)))
    out = np.stack([np.asarray(res.results[b]["out"]) for b in range(8)], axis=0)
    return out.astype(np.float32)
```

```python
import contextlib
import math
import numpy as np
import ml_dtypes
import concourse.bass as bass
import concourse.mybir as mybir
from concourse.bass_utils import run_bass_kernel_spmd

F32 = mybir.dt.float32
BF16 = mybir.dt.bfloat16
I32 = mybir.dt.int32
U32 = mybir.dt.uint32
AF = mybir.ActivationFunctionType
ALU = mybir.AluOpType
AX = mybir.AxisListType

N_DMA_SEMS = 12
L = 8192
D = 1024
CTX = 256
TT = L + CTX
EPS = 1e-6


class Buf:
    __slots__ = ("name", "w", "r", "psum", "acc")

    def __init__(self, name="", psum=False):
        self.name = name
        self.w = None
        self.r = []
        self.psum = psum
        self.acc = {}


class Op:
    __slots__ = ("eng", "fn", "deps", "is_dma", "signal", "val", "sem", "idx")


class Prog:
    ENGS = ("pe", "dve", "act", "pool", "sp")
    ENGOBJ = {"pe": "tensor", "dve": "vector", "act": "scalar", "pool": "gpsimd", "sp": "sync"}

    def __init__(self, nc, stack):
        self.nc = nc
        self.nops = 0
        self.cur = []
        self.sems = {e: stack.enter_context(nc.semaphore(f"s_{e}")) for e in ("pe", "dve", "act", "pool")}
        self.dma_sems = {e: [stack.enter_context(nc.semaphore(f"d_{e}_{i}")) for i in range(N_DMA_SEMS)]
                         for e in ("sp", "act", "pool")}
        self.cnt = {e: 0 for e in self.ENGS}
        self.dma_k = {e: 0 for e in self.ENGS}
        self.dma_vals = {e: [0] * N_DMA_SEMS for e in self.ENGS}
        self.seen = {e: {} for e in self.ENGS}
        self.last_op = {e: None for e in self.ENGS}
        self.prev_dmas = []
        self.pending = {}
        self.n_waits = 0

    def op(self, eng, fn, reads=(), writes=(), is_dma=False):
        o = Op()
        o.eng = eng
        o.fn = fn
        o.is_dma = is_dma
        o.signal = False
        o.val = None
        o.sem = None
        o.idx = self.nops
        self.nops += 1
        deps = set()
        for b in reads:
            if b.w is not None and b.w.fn is not None:
                deps.add(b.w)
        for b in writes:
            if b.w is not None and b.w.fn is not None:
                deps.add(b.w)
            for r in b.r:
                if r.fn is not None:
                    deps.add(r)
        for b in tuple(reads) + tuple(writes):
            if b.psum:
                for x, o2 in b.acc.items():
                    if x != eng and o2.fn is not None:
                        deps.add(o2)
                b.acc[eng] = o
        if eng in self.pending:
            deps |= self.pending.pop(eng)
        o.deps = deps
        for b in reads:
            b.r.append(o)
        for b in writes:
            b.w = o
            b.r = []
        self.cur.append(o)
        return o

    def dma(self, eng, out, in_, reads=(), writes=(), **kw):
        return self.op(eng, lambda e: e.dma_start(out=out, in_=in_, **kw), reads, writes, is_dma=True)

    def phase_end(self):
        nc = self.nc
        by_eng = {e: [] for e in self.ENGS}
        for o in self.cur:
            by_eng[o.eng].append(o)
        for o in self.cur:
            best = {}
            for d in o.deps:
                if d.is_dma or (d.eng == o.eng and o.eng == "pe"):
                    continue
                if d.eng not in best or d.idx > best[d.eng].idx:
                    best[d.eng] = d
            for d in best.values():
                if d.val is None:
                    d.signal = True
        for e in self.ENGS:
            for o in reversed(by_eng[e]):
                if not o.is_dma:
                    o.signal = True
                    break
        for e in self.ENGS:
            for o in by_eng[e]:
                if o.is_dma:
                    s = self.dma_k[e] % N_DMA_SEMS
                    self.dma_k[e] += 1
                    self.dma_vals[e][s] += 16
                    o.sem = (e, s)
                    o.val = self.dma_vals[e][s]
                elif o.signal:
                    self.cnt[e] += 1
                    o.val = self.cnt[e]

        def emit_engine(e, eng):
            seen = self.seen[e]

            def wait(key, sem, val):
                if seen.get(key, 0) >= val:
                    return
                eng.wait_ge(sem, val)
                self.n_waits += 1
                seen[key] = val

            for o in by_eng[e]:
                best = {}
                for d in o.deps:
                    if d.is_dma:
                        wait(("dma",) + d.sem, self.dma_sems[d.sem[0]][d.sem[1]], d.val)
                    else:
                        if d.eng == e and e == "pe":
                            continue
                        if d.eng not in best or d.idx > best[d.eng].idx:
                            best[d.eng] = d
                for x, d in best.items():
                    wait(x, self.sems[x], d.val)
                if o.is_dma:
                    if o.val > 16:
                        wait(("dma",) + o.sem, self.dma_sems[e][o.sem[1]], o.val - 16)
                    ins = o.fn(eng)
                    ins.then_inc(self.dma_sems[e][o.sem[1]], 16)
                else:
                    ins = o.fn(eng)
                    if o.signal:
                        ins.then_inc(self.sems[e], 1)
                o.fn = None

        with nc.Block() as block:
            for e in self.ENGS:
                if not by_eng[e]:
                    continue
                dec = getattr(block, self.ENGOBJ[e])

                def mk(e):
                    def _f(eng):
                        emit_engine(e, eng)
                    return _f
                dec(mk(e))
        for e in self.ENGS:
            for o in reversed(by_eng[e]):
                if not o.is_dma:
                    self.last_op[e] = o
                    break
        dmas = [o for o in self.cur if o.is_dma]
        bar = set(dmas)
        for e in self.ENGS:
            if self.last_op[e] is not None:
                bar.add(self.last_op[e])
        for e in self.ENGS:
            self.pending[e] = self.pending.get(e, set()) | bar
        for o in self.cur:
            o.deps = None
        self.cur = []

    def finish(self):
        nc = self.nc
        with nc.Block() as block:
            for e in ("sp", "act", "pool"):
                if self.dma_k[e] == 0:
                    continue
                dec = getattr(block, self.ENGOBJ[e])

                def mk(e):
                    def _f(eng):
                        for s in range(N_DMA_SEMS):
                            v = self.dma_vals[e][s]
                            if v > 0:
                                eng.wait_ge(self.dma_sems[e][s], v)
                    return _f
                dec(mk(e))


class Rot:
    def __init__(self, tiles):
        self.tiles = tiles
        self.bufs = [Buf() for _ in tiles]
        self.i = 0

    def next(self):
        t, b = self.tiles[self.i], self.bufs[self.i]
        self.i = (self.i + 1) % len(self.tiles)
        return t, b


class Ctx:
    pass


_UNIQ = [0]


def sb(st, nc, name, shape, dt):
    _UNIQ[0] += 1
    return st.enter_context(nc.sbuf_tensor(f"{name}_{_UNIQ[0]}", list(shape), dt))


def ps_banks(st, nc, n, prefix):
    _UNIQ[0] += 1
    r = Rot([st.enter_context(nc.psum_tensor(f"{prefix}{i}_{_UNIQ[0]}", [128, 512], F32)) for i in range(n)])
    for b in r.bufs:
        b.psum = True
    return r


def host_consts():
    c = {}
    c["ident"] = np.eye(128, dtype=np.float32)
    c["ones"] = np.ones((128, 128), dtype=np.float32)
    return c


def phase_xt(K):
    nc, P = K.nc, K.P
    with contextlib.ExitStack() as st:
        xin = Rot([sb(st, nc, f"xt_in{i}", [128, 4, D], F32) for i in range(2)])
        hout = Rot([sb(st, nc, f"xt_out{i}", [128, 8, 512], F32) for i in range(2)])
        psr = ps_banks(st, nc, 4, "xtps")
        jobs = [(K.x[g * 512:(g + 1) * 512, :], K.hT[:, :, g * 512:(g + 1) * 512], 4) for g in range(L // 512)]
        jobs.append((K.ctx[:, :], K.ctxT[:, :, :], 2))
        n = 0
        for src, dst, na in jobs:
            xt, xb = xin.next()
            ho, hb = hout.next()
            P.dma("sp", xt[:, 0:na, :], src.rearrange("(a p) d -> p a d", p=128), writes=[xb])
            for dc in range(8):
                ps, pb = psr.next()
                for a in range(na):
                    P.op("pe", lambda e, ps=ps, xt=xt, a=a, dc=dc: e.transpose(
                        out=ps[:, a * 128:(a + 1) * 128], in_=xt[:, a, dc * 128:(dc + 1) * 128], identity=K.ident[:]),
                        reads=[xb], writes=[pb])
                if n % 2 == 0:
                    P.op("act", lambda e, ps=ps, ho=ho, dc=dc, na=na: e.activation(
                        out=ho[:, dc, 0:na * 128], in_=ps[:, 0:na * 128], func=AF.Copy), reads=[pb], writes=[hb])
                else:
                    P.op("dve", lambda e, ps=ps, ho=ho, dc=dc, na=na: e.tensor_copy(
                        out=ho[:, dc, 0:na * 128], in_=ps[:, 0:na * 128]), reads=[pb], writes=[hb])
                n += 1
            P.dma("pool", dst.rearrange("c p t -> p c t"), ho[:, :, 0:na * 128], reads=[hb])
        P.phase_end()


def phase_mod(K):
    nc, P = K.nc, K.P
    with contextlib.ExitStack() as st:
        cin = sb(st, nc, "mod_cin", [128, 2, 8], F32)
        sc2 = sb(st, nc, "mod_sc2", [128, 8, 2], F32)
        ab = sb(st, nc, "mod_ab", [128, 2, 48], F32)
        wr = Rot([sb(st, nc, f"mod_w{i}", [128, 8, 1536], F32) for i in range(2)])
        psr = ps_banks(st, nc, 2, "modps")
        bc, bs, bab = Buf(), Buf(), Buf()
        P.dma("sp", cin[:, 0, :], K.c_l, writes=[bc])
        P.dma("sp", cin[:, 1, :], K.cc_l, writes=[bc])
        P.dma("sp", ab[:], K.adab_l, writes=[bab])
        P.op("act", lambda e: e.activation(out=sc2[:].rearrange("p k t -> p t k"), in_=cin[:], func=AF.Silu), reads=[bc], writes=[bs])
        for l in range(2):
            for nb in range(4):
                w, wb = wr.next()
                P.dma("sp", w[:], K.ada_w[l, :, nb * 1536:(nb + 1) * 1536].rearrange("(kc p) n -> p kc n", p=128),
                      writes=[wb])
                ps, pb = psr.next()
                for j in range(12):
                    for kc in range(8):
                        P.op("pe", lambda e, ps=ps, w=w, j=j, kc=kc: e.matmul(
                            ps[:, j * 2:(j + 1) * 2], lhsT=w[:, kc, j * 128:(j + 1) * 128], rhs=sc2[:, kc, :],
                            start=(kc == 0), stop=(kc == 7)), reads=[wb, bs], writes=[pb])
                P.op("dve", lambda e, ps=ps, l=l, nb=nb: e.tensor_tensor(
                    out=K.MOD[:, l, nb * 12:(nb + 1) * 12, :],
                    in0=ps[:, 0:24].rearrange("p (j t) -> p j t", t=2),
                    in1=ab[:, l, nb * 12:(nb + 1) * 12].unsqueeze(2).to_broadcast([128, 12, 2]),
                    op=ALU.add), reads=[pb, bab], writes=[K.bMOD])
        P.phase_end()


def phase_norm(K, l, s, src, dst, groups, col, gname):
    nc, P = K.nc, K.P
    with contextlib.ExitStack() as st:
        A = sb(st, nc, "nm_A", [128, 8], F32)
        g = sb(st, nc, "nm_g", [128, 8], F32)
        hin = Rot([sb(st, nc, f"nm_in{i}", [128, 8, 512], F32) for i in range(2)])
        sq = Rot([sb(st, nc, f"nm_sq{i}", [128, 8, 512], BF16) for i in range(2)])
        sd = Rot([sb(st, nc, f"nm_sd{i}", [128, 512], F32) for i in range(2)])
        tmp = Rot([sb(st, nc, f"nm_tmp{i}", [128, 512], F32) for i in range(3)])
        hout = Rot([sb(st, nc, f"nm_out{i}", [128, 8, 512], BF16) for i in range(2)])
        psr = ps_banks(st, nc, 2, "nmps")
        bA, bg = Buf(), Buf()
        P.dma("sp", g[:], gname, writes=[bg])
        msc = K.MOD[:, l, (3 * s + 1) * 8:(3 * s + 2) * 8, col]
        msh = K.MOD[:, l, (3 * s) * 8:(3 * s + 1) * 8, col]
        P.op("dve", lambda e: e.scalar_tensor_tensor(out=A[:], in0=msc, scalar=1.0, in1=g[:], op0=ALU.add, op1=ALU.mult),
             reads=[K.bMOD, bg], writes=[bA])
        for (so, do, Tg) in groups:
            h, hb = hin.next()
            P.dma("sp", h[:, :, 0:Tg], src[:, :, so:so + Tg].rearrange("c p t -> p c t"), writes=[hb])
            q, qb = sq.next()
            P.op("act", lambda e, q=q, h=h, Tg=Tg: e.activation(out=q[:, :, 0:Tg], in_=h[:, :, 0:Tg], func=AF.Square),
                 reads=[hb], writes=[qb])
            ps, pb = psr.next()
            for dc in range(8):
                P.op("pe", lambda e, ps=ps, q=q, dc=dc, Tg=Tg: e.matmul(
                    ps[:, 0:Tg], lhsT=K.onesb[:], rhs=q[:, dc, 0:Tg], start=(dc == 0), stop=(dc == 7)),
                    reads=[qb], writes=[pb])
            d_, db = sd.next()
            P.op("act", lambda e, d_=d_, ps=ps, Tg=Tg: e.activation(
                out=d_[:, 0:Tg], in_=ps[:, 0:Tg], func=AF.Sqrt, bias=K.epsc[:, 0:1], scale=1.0 / D),
                reads=[pb], writes=[db])
            P.op("dve", lambda e, d_=d_, Tg=Tg: e.reciprocal(out=d_[:, 0:Tg], in_=d_[:, 0:Tg]), reads=[db], writes=[db])
            ho, hob = hout.next()
            for dc in range(8):
                t, tb = tmp.next()
                P.op("dve", lambda e, t=t, h=h, d_=d_, dc=dc, Tg=Tg: e.tensor_tensor(
                    out=t[:, 0:Tg], in0=h[:, dc, 0:Tg], in1=d_[:, 0:Tg], op=ALU.mult), reads=[hb, db], writes=[tb])
                P.op("pool", lambda e, t=t, ho=ho, dc=dc, Tg=Tg: e.tensor_scalar(
                    out=ho[:, dc, 0:Tg], in0=t[:, 0:Tg], scalar1=A[:, dc:dc + 1], scalar2=msh[:, dc:dc + 1],
                    op0=ALU.mult, op1=ALU.add), reads=[tb, bA, K.bMOD], writes=[hob])
            P.dma("pool", dst[:, :, do:do + Tg].rearrange("c p t -> p c t"), ho[:, :, 0:Tg], reads=[hob])
        P.phase_end()


WEIGHTS = [
    ("ada_w", [2, D, 6 * D]),
    ("dn_w_in", [D, 4128]), ("dn_w_out", [D, D]),
    ("hy_w_in", [D, 3 * D]), ("hy_w_out", [D, D]),
    ("peer_w_q", [2, D, 2048]), ("peer_v", [2, 16384, D]),
]


def build(stages, debug=(), **kw):
    nc = bass.Bass("TRN2", target_bir_lowering=False)
    K = Ctx()
    for k_, v_ in kw.items():
        setattr(K, k_, v_)
    K.nc = nc
    K.debug = set(debug)

    def din(name, shape, dt=F32):
        return nc.dram_tensor(name, list(shape), dt, kind="ExternalInput").ap()

    def dscr(name, shape, dt=F32):
        if name in getattr(K, 'ext_in', ()):
            return nc.dram_tensor(name, list(shape), dt, kind="ExternalInput").ap()
        kind = {"kind": "ExternalOutput"} if name in K.debug else {}
        return nc.dram_tensor(name, list(shape), dt, **kind).ap()

    K.x = din("x", [L, D])
    K.ctx = din("ctx", [CTX, D])
    K.c_l = din("c_l", [128, 8])
    K.cc_l = din("cc_l", [128, 8])
    K.adab_l = din("adab_l", [128, 2, 48])
    K.normg_l = din("normg_l", [2, 2, 128, 8])
    K.finalg_l = din("finalg_l", [128, 8])
    K.cident = din("cident", [128, 128])
    K.dn_cw_l = din("dn_cw_l", [128, 24, 5])
    K.cscan = din("cscan", [6, 128, 128])
    K.dn_gn_l = din("dn_gn_l", [128, 1024])
    K.peer_u_l = din("peer_u_l", [2, 128, 8, 128, 128])
    K.peer_kT_l = din("peer_kT_l", [2, 128, 16, 128])
    K.hy_cw_l = din("hy_cw_l", [128, 24, 3])
    K.hy_cb_l = din("hy_cb_l", [128, 24])
    K.hy_f_w1 = din("hy_f_w1", [33, 64])
    K.hy_f_w2 = din("hy_f_w2", [64, 64])
    K.hy_f_w3 = din("hy_f_w3", [64, 64])
    K.hy_f_w4 = din("hy_f_w4", [64, 4096])
    K.hy_fpv_l = din("hy_fpv_l", [64, 4])
    K.hy_ndelta_l = din("hy_ndelta_l", [128, 8])
    K.hy_zpos = din("hy_zpos", [33, L])
    K.hy_tpos = din("hy_tpos", [128, L])
    K.hy_skip_l = din("hy_skip_l", [128, 2, 8])
    K.hy_skip_h = din("hy_skip_h", [64, 2, 16])
    K.fft_mats = din("fft_mats", [128, 4, 128])
    K.fft_wide = din("fft_wide", [128, 3, 256])
    K.fft_tw = din("fft_tw", [128, 2, 256])
    K.dn_alog_l = din("dn_alog_l", [128, 16])
    K.dn_dtb_l = din("dn_dtb_l", [128, 16])
    for name, shape in WEIGHTS:
        setattr(K, name, din(name, shape))
    K.out = nc.dram_tensor("out", [L, D], F32, kind="ExternalOutput").ap()
    if "hT" in getattr(K, 'ext_in', ()):
        K.hT_in = nc.dram_tensor("hT", [8, 128, L], F32, kind="ExternalInput").ap()
        K.hT = nc.dram_tensor("hTout", [8, 128, L], F32, kind="ExternalOutput").ap()
    else:
        K.hT = dscr("hT", [8, 128, L])
    K.ctxT = dscr("ctxT", [8, 128, CTX])
    K.hnT = dscr("hnT", [8, 128, TT], BF16)
    K.qT = dscr("qT", [8, 128, TT], BF16)
    K.kT = dscr("kT", [8, 128, TT], BF16)
    K.qkvtok = dscr("qkvtok", [TT, 3072])
    K.sgate = dscr("sgate", [TT, 1024])
    K.ba = dscr("ba", [TT, 32])
    K.o_dir = [dscr("o_f", [L, 1024]), dscr("o_b", [L, 1024])]
    K.ygT = dscr("ygT", [8, 128, L], BF16)
    K.Ub = dscr("Ub", [128, 128, 8, 128], BF16)
    K.zT = dscr("zT", [24, 128, L])
    K.filtT = dscr("filtT", [2, 2, 8, 128, L])
    K.z2T = dscr("z2T", [8, 128, L], BF16)
    K.Vb = dscr("Vb", [128, 128, 1024], BF16)
    if getattr(K, 'scan_dbg', False):
        K.dbgwk = nc.dram_tensor("dbgwk", [8, 128, 13, 128], F32, kind="ExternalOutput").ap()
        K.dbgsm = nc.dram_tensor("dbgsm", [128, 12, 8], F32, kind="ExternalOutput").ap()

    with contextlib.ExitStack() as st:
        P = Prog(nc, st)
        K.P = P
        K.ident = sb(st, nc, "ident", [128, 128], F32)
        K.identb = sb(st, nc, "identb", [128, 128], BF16)
        K.onesb = sb(st, nc, "onesb", [128, 128], BF16)
        K.onesf = sb(st, nc, "onesf", [128, 128], F32)
        K.epsc = sb(st, nc, "epsc", [128, 1], F32)
        K.onec = sb(st, nc, "onec", [128, 1], F32)
        K.MOD = sb(st, nc, "MOD", [128, 2, 48, 2], F32)
        K.bMOD = Buf()
        bi = Buf()
        P.dma("sp", K.ident[:], K.cident, writes=[bi])
        P.op("dve", lambda e: e.tensor_copy(out=K.identb[:], in_=K.ident[:]), reads=[bi], writes=[bi])
        P.op("dve", lambda e: e.memset(K.onesb[:], 1.0), writes=[bi])
        P.op("dve", lambda e: e.memset(K.onesf[:], 1.0), writes=[bi])
        P.op("dve", lambda e: e.memset(K.epsc[:], EPS), writes=[bi])
        P.op("dve", lambda e: e.memset(K.onec[:], 1.0), writes=[bi])
        P.phase_end()
        lat_groups = [(g * 512, CTX + g * 512, 512) for g in range(L // 512)]
        if "hT" in getattr(K, 'ext_in', ()):
            with contextlib.ExitStack() as st2:
                cpr = Rot([sb(st2, nc, f"cp{i}", [128, 8, 512], F32) for i in range(2)])
                for g in range(L // 512):
                    t, tb = cpr.next()
                    P.dma("sp", t[:], K.hT_in[:, :, g * 512:(g + 1) * 512].rearrange("c p t -> p c t"), writes=[tb])
                    P.dma("sp", K.hT[:, :, g * 512:(g + 1) * 512].rearrange("c p t -> p c t"), t[:], reads=[tb])
                P.phase_end()
        if "xt" in stages:
            phase_xt(K)
        if "mod" in stages:
            phase_mod(K)
        if "norm0" in stages:
            phase_norm(K, 0, 0, K.hT, K.hnT, lat_groups, 0, K.normg_l[0, 0])
            phase_norm(K, 0, 0, K.ctxT, K.hnT, [(0, 0, CTX)], 1, K.normg_l[0, 0])
        if "dnin" in stages:
            phase_dnin(K)
        if "dnscan" in stages:
            phase_dnscan(K)
        if "dnout" in stages:
            phase_dnout(K)
            phase_outproj(K, K.ygT, K.dn_w_out, 0, 2)
        if "peer0" in stages:
            phase_norm(K, 0, 1, K.hT, K.hnT, lat_groups, 0, K.normg_l[0, 1])
            phase_peer_tables(K, 0)
            phase_peer(K, 0)
        if "hyena" in stages:
            phase_norm(K, 1, 0, K.hT, K.hnT, lat_groups, 0, K.normg_l[1, 0])
            phase_hyin(K)
            phase_hyfilt(K)
            if getattr(K, 'hy_direct', False):
                phase_hyconv_direct(K)
            else:
                phase_hyconv_fft(K)
            phase_outproj(K, K.z2T, K.hy_w_out, 1, 2)
        if "peer1" in stages:
            phase_norm(K, 1, 1, K.hT, K.hnT, lat_groups, 0, K.normg_l[1, 1])
            phase_peer_tables(K, 1)
            phase_peer(K, 1)
        if "final" in stages:
            phase_final(K)
        if "dbgmod" in K.debug:
            dm = nc.dram_tensor("dbgmod", [128, 2 * 48 * 2], F32, kind="ExternalOutput").ap()
            P.dma("sp", dm, K.MOD[:].rearrange("p a b c -> p (a b c)"), reads=[K.bMOD])
            P.phase_end()
        P.finish()
        K.nops = P.nops
        K.nwaits = P.n_waits
    return nc, K


_HC = {}


def hyena_consts():
    if not _HC:
        f32 = np.float32
        pos = np.arange(L, dtype=f32)
        t = (pos / f32(L - 1)).astype(f32)
        bands = np.linspace(1e-4, 15, 16, dtype=f32)
        ang = ((f32(2 * math.pi) * pos / f32(L))[:, None] * bands[None]).astype(f32)
        z = np.concatenate([t[:, None], np.cos(ang), -np.sin(ang)], axis=-1).astype(f32)
        deltas = np.abs(np.linspace(math.log(1e-2) / 0.3, math.log(1e-2) / 1.5, D, dtype=f32)).astype(f32)
        _HC["hy_zpos"] = np.ascontiguousarray(z.T)
        _HC["hy_tpos"] = np.ascontiguousarray(np.tile(t[None, :], (128, 1)))
        _HC["hy_ndelta_l"] = np.ascontiguousarray(-deltas.reshape(8, 128).T)
    return _HC


def host_inputs(inputs, b):
    f = np.ascontiguousarray
    m = {}
    m["x"] = f(inputs["x"][b])
    m["ctx"] = f(inputs["ctx"][b])
    m["c_l"] = f(inputs["c"][b].reshape(8, 128).T)
    m["cc_l"] = f(inputs["c_ctx"].reshape(8, 128).T)
    m["adab_l"] = f(inputs["ada_b"].reshape(2, 48, 128).transpose(2, 0, 1))
    m["normg_l"] = f(inputs["norm_g"].reshape(2, 2, 8, 128).transpose(0, 1, 3, 2))
    m["finalg_l"] = f(inputs["final_g"].reshape(8, 128).T)
    m["cident"] = np.eye(128, dtype=np.float32)
    m["dn_cw_l"] = f(inputs["dn_conv_w"][0].reshape(5, 24, 128).transpose(2, 1, 0))
    i_ = np.arange(128)
    tri_f = (i_[:, None] <= i_[None, :]).astype(np.float32)
    BIG = 30000.0
    maska_f = np.where(i_[None, :] > i_[:, None], BIG, 0.0).astype(np.float32)
    m01s_f = (i_[None, :] < i_[:, None]).astype(np.float32)
    m["cscan"] = f(np.stack([tri_f, tri_f.T, maska_f, maska_f.T, m01s_f, m01s_f.T]))
    m["dn_alog_l"] = f(np.tile(inputs["dn_a_log"][0].reshape(1, 16), (128, 1)))
    m["dn_dtb_l"] = f(np.tile(inputs["dn_dt_bias"][0].reshape(1, 16), (128, 1)))
    m["dn_gn_l"] = f(np.tile(np.tile(inputs["dn_norm_g"][0], 8).reshape(1, 1024), (128, 1)))
    m["hy_cw_l"] = f(inputs["hy_conv_w"][0].reshape(3, 24, 128).transpose(2, 1, 0))
    m["hy_cb_l"] = f(inputs["hy_conv_b"][0].reshape(24, 128).T)
    m["hy_f_w1"] = f(inputs["hy_f_w1"][0]); m["hy_f_w2"] = f(inputs["hy_f_w2"][0])
    m["hy_f_w3"] = f(inputs["hy_f_w3"][0]); m["hy_f_w4"] = f(inputs["hy_f_w4"][0])
    m["hy_fpv_l"] = f(np.stack([inputs["hy_freq"][0], inputs["hy_f_b1"][0], inputs["hy_f_b2"][0], inputs["hy_f_b3"][0]], axis=1))
    m["hy_skip_l"] = f(inputs["hy_skip"][0].reshape(2, 8, 128).transpose(2, 0, 1))
    m.update(hyena_consts())
    m["hy_skip_h"] = f(inputs["hy_skip"][0].reshape(2, 16, 64).transpose(2, 0, 1))
    m.update(fft_consts())
    m["ada_w"] = inputs["ada_w"]
    m["dn_w_in"] = inputs["dn_w_in"][0]
    m["dn_w_out"] = inputs["dn_w_out"][0]
    m["hy_w_in"] = inputs["hy_w_in"][0]
    m["hy_w_out"] = inputs["hy_w_out"][0]
    m["peer_w_q"] = inputs["peer_w_q"]
    m["peer_u_l"] = f(inputs["peer_u"].reshape(2, 128, 128, 8, 128).transpose(0, 1, 3, 4, 2))
    m["peer_kT_l"] = f(inputs["peer_keys"].reshape(2, 16, 128, 128).transpose(0, 3, 1, 2))
    m["peer_v"] = inputs["peer_v"]
    return m


def phase_dnin(K):
    nc, P = K.nc, K.P
    Tg = 256
    with contextlib.ExitStack() as st:
        Wb = sb(st, nc, "dn_Wb", [128, 8, 4128], BF16)
        cw = sb(st, nc, "dn_cw", [128, 24, 5], F32)
        hnr = Rot([sb(st, nc, f"dn_hn{i}", [128, 8, Tg], BF16) for i in range(2)])
        cvr = Rot([sb(st, nc, f"dn_cv{i}", [128, 24, Tg], F32) for i in range(1)])
        sqr = Rot([sb(st, nc, f"dn_sq{i}", [128, 16, Tg], BF16) for i in range(1)])
        qkr = Rot([sb(st, nc, f"dn_qk{i}", [128, 16, Tg], BF16) for i in range(2)])
        tokr = Rot([sb(st, nc, f"dn_tok{i}", [128, 2, 3072], F32) for i in range(1)])
        sgr = Rot([sb(st, nc, f"dn_sg{i}", [128, 2, 1024], F32) for i in range(1)])
        bar = Rot([sb(st, nc, f"dn_ba{i}", [128, 2, 32], F32) for i in range(2)])
        tmr = Rot([sb(st, nc, f"dn_tm{i}", [128, Tg], F32) for i in range(2)])
        zps = ps_banks(st, nc, 2, "dnz")
        sps = ps_banks(st, nc, 2, "dns")
        tps = ps_banks(st, nc, 2, "dnt")
        gps = ps_banks(st, nc, 2, "dng")
        bW, bcw = Buf(), Buf()
        for kc in range(8):
            for c0 in range(0, 4128, 1376):
                P.dma("pool", Wb[:, kc, c0:c0 + 1376], K.dn_w_in[kc * 128:(kc + 1) * 128, c0:c0 + 1376], writes=[bW])
        P.dma("sp", cw[:], K.dn_cw_l, writes=[bcw])
        groups = [(0, 256)] + [(CTX + g * Tg, 64) for g in range(L // Tg)]
        n = 0
        for off, rowlen in groups:
            hn, hb = hnr.next()
            P.dma("sp", hn[:], K.hnT[:, :, off:off + Tg].rearrange("c p t -> p c t"), writes=[hb])
            cv, cb = cvr.next()
            for cc in range(24):
                ps, pb = zps.next()
                for kc in range(8):
                    P.op("pe", lambda e, ps=ps, hn=hn, cc=cc, kc=kc: e.matmul(
                        ps[:, 0:Tg], lhsT=Wb[:, kc, cc * 128:(cc + 1) * 128], rhs=hn[:, kc, :],
                        start=(kc == 0), stop=(kc == 7)), reads=[bW, hb], writes=[pb])
                P.op("dve", lambda e, ps=ps, cv=cv, cc=cc: e.tensor_scalar(
                    out=cv[:, cc, :], in0=ps[:, 0:Tg], scalar1=cw[:, cc, 2:3], scalar2=None, op0=ALU.mult),
                    reads=[pb, bcw], writes=[cb])
                for k in (0, 1, 3, 4):
                    s = k - 2
                    lo, hi = max(0, -s), rowlen - max(0, s)
                    P.op("dve", lambda e, ps=ps, cv=cv, cc=cc, k=k, s=s, lo=lo, hi=hi, rowlen=rowlen: e.scalar_tensor_tensor(
                        out=cv[:, cc, :].rearrange("p (r w) -> p r w", w=rowlen)[:, :, lo:hi],
                        in0=ps[:, 0:Tg].rearrange("p (r w) -> p r w", w=rowlen)[:, :, lo + s:hi + s],
                        scalar=cw[:, cc, k:k + 1],
                        in1=cv[:, cc, :].rearrange("p (r w) -> p r w", w=rowlen)[:, :, lo:hi],
                        op0=ALU.mult, op1=ALU.add), reads=[pb, bcw], writes=[cb])
            P.op("act", lambda e, cv=cv: e.activation(out=cv[:], in_=cv[:], func=AF.Silu), reads=[cb], writes=[cb])
            sg, sgb = sgr.next()
            bt, btb = bar.next()
            for a in range(2):
                for (c0, wd) in ((0, 512), (512, 512), (1024, 32)):
                    ps, pb = gps.next()
                    for kc in range(8):
                        P.op("pe", lambda e, ps=ps, hn=hn, a=a, kc=kc, c0=c0, wd=wd: e.matmul(
                            ps[:, 0:wd], lhsT=hn[:, kc, a * 128:(a + 1) * 128], rhs=Wb[:, kc, 3072 + c0:3072 + c0 + wd],
                            start=(kc == 0), stop=(kc == 7)), reads=[bW, hb], writes=[pb])
                    if wd == 512:
                        P.op("act", lambda e, ps=ps, sg=sg, a=a, c0=c0: e.activation(
                            out=sg[:, a, c0:c0 + 512], in_=ps[:, :], func=AF.Silu), reads=[pb], writes=[sgb])
                    else:
                        P.op("dve", lambda e, ps=ps, bt=bt, a=a: e.tensor_copy(out=bt[:, a, :], in_=ps[:, 0:32]),
                             reads=[pb], writes=[btb])
            P.dma("pool", K.sgate[off:off + Tg, :].rearrange("(a p) c -> p a c", p=128), sg[:], reads=[sgb])
            P.dma("pool", K.ba[off:off + Tg, :].rearrange("(a p) c -> p a c", p=128), bt[:], reads=[btb])
            sq, sqb_ = sqr.next()
            P.op("act", lambda e, sq=sq, cv=cv: e.activation(out=sq[:], in_=cv[:, 0:16, :], func=AF.Square),
                 reads=[cb], writes=[sqb_])
            qk, qkb = qkr.next()
            for cc in range(16):
                ps, pb = sps.next()
                P.op("pe", lambda e, ps=ps, sq=sq, cc=cc: e.matmul(ps[:, 0:Tg], lhsT=K.onesb[:], rhs=sq[:, cc, :],
                                                                  start=True, stop=True), reads=[sqb_], writes=[pb])
                t, tb = tmr.next()
                P.op("act", lambda e, ps=ps, t=t: e.activation(out=t[:], in_=ps[:, 0:Tg], func=AF.Ln,
                                                               bias=K.epsc[:, 0:1], scale=1.0), reads=[pb], writes=[tb])
                P.op("act", lambda e, t=t: e.activation(out=t[:], in_=t[:], func=AF.Exp, scale=-0.5),
                     reads=[tb], writes=[tb])
                scl = (128.0 ** -0.5) if cc < 8 else 1.0
                P.op("dve", lambda e, cv=cv, cc=cc, t=t, scl=scl: e.scalar_tensor_tensor(
                    out=cv[:, cc, :], in0=cv[:, cc, :], scalar=scl, in1=t[:], op0=ALU.mult, op1=ALU.mult),
                    reads=[cb, tb], writes=[cb])
                P.op("pool", lambda e, cv=cv, qk=qk, cc=cc: e.tensor_copy(out=qk[:, cc, :], in_=cv[:, cc, :]),
                     reads=[cb], writes=[qkb])
            P.dma("pool", K.qT[:, :, off:off + Tg].rearrange("c p t -> p c t"), qk[:, 0:8, :], reads=[qkb])
            P.dma("pool", K.kT[:, :, off:off + Tg].rearrange("c p t -> p c t"), qk[:, 8:16, :], reads=[qkb])
            tok, tkb = tokr.next()
            for a in range(2):
                for g6 in range(6):
                    ps, pb = tps.next()
                    for j in range(4):
                        P.op("pe", lambda e, ps=ps, cv=cv, a=a, g6=g6, j=j: e.transpose(
                            out=ps[:, j * 128:(j + 1) * 128], in_=cv[:, g6 * 4 + j, a * 128:(a + 1) * 128],
                            identity=K.ident[:]), reads=[cb], writes=[pb])
                    if n % 2 == 0:
                        P.op("act", lambda e, ps=ps, tok=tok, a=a, g6=g6: e.activation(
                            out=tok[:, a, g6 * 512:(g6 + 1) * 512], in_=ps[:, :], func=AF.Copy), reads=[pb], writes=[tkb])
                    else:
                        P.op("dve", lambda e, ps=ps, tok=tok, a=a, g6=g6: e.tensor_copy(
                            out=tok[:, a, g6 * 512:(g6 + 1) * 512], in_=ps[:, :]), reads=[pb], writes=[tkb])
                    n += 1
            P.dma("pool", K.qkvtok[off:off + Tg, :].rearrange("(a p) c -> p a c", p=128), tok[:], reads=[tkb])
        P.phase_end()


def phase_dnscan(K):
    nc, P = K.nc, K.P
    NB = TT // 128
    lim = getattr(K, 'scan_maxops', None)
    if lim is not None:
        real_op = P.op
        cnt = [0]

        def lim_op(*a, **k):
            cnt[0] += 1
            if cnt[0] > lim:
                return None
            return real_op(*a, **k)
        P.op = lim_op
    with contextlib.ExitStack() as st:
        cs = sb(st, nc, "sc_consts", [128, 6, 128], F32)
        alg = sb(st, nc, "sc_alog", [128, 16], F32)
        dtb = sb(st, nc, "sc_dtb", [128, 16], F32)
        nea = sb(st, nc, "sc_nea", [128, 16], F32)
        S = [[sb(st, nc, f"sc_S{d}{h}", [128, 128], F32) for h in range(8)] for d in range(2)]
        bS = [[Buf() for h in range(8)] for d in range(2)]
        kTr = Rot([sb(st, nc, f"sc_kT{i}", [128, 8, 128], BF16) for i in range(2)])
        qTr = Rot([sb(st, nc, f"sc_qT{i}", [128, 8, 128], BF16) for i in range(2)])
        tokr = Rot([sb(st, nc, f"sc_tok{i}", [128, 3072], F32) for i in range(2)])
        bar = Rot([sb(st, nc, f"sc_ba{i}", [128, 32], F32) for i in range(2)])
        smr = Rot([sb(st, nc, f"sc_sm{i}", [128, 12, 8], F32) for i in range(2)])
        dgr = Rot([sb(st, nc, f"sc_dg{i}", [128, 5, 8, 128], F32) for i in range(2)])
        obr = Rot([sb(st, nc, f"sc_ob{i}", [128, 1024], F32) for i in range(2)])
        NW = 13
        wkr = [Rot([sb(st, nc, f"sc_wk{h}_{i}", [128, NW, 128], F32) for i in range(1)]) for h in range(8)]
        _pst = [st.enter_context(nc.psum_tensor(f"scps{i}", [128, 512], F32)) for i in range(8)]
        _bb = [Buf(psum=True) for i in range(8)]

        def mkpool(banks):
            r = Rot([_pst[i][:, j * 128:(j + 1) * 128] for i in banks for j in range(4)])
            r.bufs = [_bb[i] for i in banks for j in range(4)]
            return r
        psA = mkpool([0, 1, 2, 3])
        psD = mkpool([4, 5, 6, 7])
        bC = Buf()
        P.dma("sp", cs[:], K.cscan.rearrange("c p f -> p c f"), writes=[bC])
        P.dma("sp", alg[:], K.dn_alog_l, writes=[bC])
        P.dma("sp", dtb[:], K.dn_dtb_l, writes=[bC])
        P.op("act", lambda e: e.activation(out=nea[:], in_=alg[:], func=AF.Exp), reads=[bC], writes=[bC])
        P.op("dve", lambda e: e.tensor_scalar(out=nea[:], in0=nea[:], scalar1=-1.0, scalar2=None, op0=ALU.mult),
             reads=[bC], writes=[bC])
        for d in range(2):
            for h in range(8):
                P.op("pool", lambda e, d=d, h=h: e.memset(S[d][h][:], 0.0), writes=[bS[d][h]])
        TRI = [cs[:, 0, :], cs[:, 1, :]]
        MASKA = [cs[:, 2, :], cs[:, 3, :]]
        M01S = [cs[:, 4, :], cs[:, 5, :]]
        order = [list(range(NB)), [1, 0] + list(range(NB - 1, 1, -1))]
        evn = [0]

        def evac(out, in_, rd, wr, eng):
            if eng == "act":
                P.op("act", lambda e: e.activation(out=out, in_=in_, func=AF.Copy), reads=rd, writes=wr)
            else:
                P.op("dve", lambda e: e.tensor_copy(out=out, in_=in_), reads=rd, writes=wr)

        def job(blk, d):
            t0 = blk * 128
            with_out = blk >= 2
            kT, kTb = kTr.next()
            qT, qTb = qTr.next()
            tok, tkb = tokr.next()
            bt, btb = bar.next()
            sm, smb = smr.next()
            dg, dgb = dgr.next()
            P.dma("sp", kT[:], K.kT[:, :, t0:t0 + 128].rearrange("c p t -> p c t"), writes=[kTb])
            P.dma("sp", qT[:], K.qT[:, :, t0:t0 + 128].rearrange("c p t -> p c t"), writes=[qTb])
            P.dma("sp", tok[:], K.qkvtok[t0:t0 + 128, :], writes=[tkb])
            P.dma("sp", bt[:], K.ba[t0:t0 + 128, :], writes=[btb])
            BETA, NBETA, GG, EG, EKD, GL, BG, TMP, G_ = 0, 1, 2, 3, 4, 5, 6, 7, 8
            P.op("act", lambda e: e.activation(out=sm[:, TMP, :], in_=bt[:, d * 8:(d + 1) * 8], func=AF.Exp, scale=-1.0),
                 reads=[btb], writes=[smb])
            P.op("dve", lambda e: e.tensor_scalar(out=sm[:, TMP, :], in0=sm[:, TMP, :], scalar1=1.0, scalar2=None,
                                                  op0=ALU.add), reads=[smb], writes=[smb])
            P.op("dve", lambda e: e.reciprocal(out=sm[:, BETA, :], in_=sm[:, TMP, :]), reads=[smb], writes=[smb])
            P.op("dve", lambda e: e.tensor_scalar(out=sm[:, NBETA, :], in0=sm[:, BETA, :], scalar1=-1.0, scalar2=None,
                                                  op0=ALU.mult), reads=[smb], writes=[smb])
            P.op("dve", lambda e: e.tensor_tensor(out=sm[:, TMP, :], in0=bt[:, 16 + d * 8:16 + (d + 1) * 8],
                                                  in1=dtb[:, d * 8:(d + 1) * 8], op=ALU.add), reads=[btb, bC], writes=[smb])
            P.op("act", lambda e: e.activation(out=sm[:, TMP, :], in_=sm[:, TMP, :], func=AF.Exp), reads=[smb], writes=[smb])
            P.op("act", lambda e: e.activation(out=sm[:, TMP, :], in_=sm[:, TMP, :], func=AF.Ln, bias=K.onec[:, 0:1],
                                               scale=1.0), reads=[smb], writes=[smb])
            P.op("dve", lambda e: e.tensor_tensor(out=sm[:, G_, :], in0=sm[:, TMP, :], in1=nea[:, d * 8:(d + 1) * 8],
                                                  op=ALU.mult), reads=[smb, bC], writes=[smb])
            pg, pgb = psD.next()
            P.op("pe", lambda e: e.matmul(pg[:, 0:8], lhsT=TRI[d], rhs=sm[:, G_, :], start=True, stop=True),
                 reads=[smb, bC], writes=[pgb])
            P.op("pe", lambda e: e.matmul(pg[:, 8:16], lhsT=K.onesf[:], rhs=sm[:, G_, :], start=True, stop=True),
                 reads=[smb], writes=[pgb])
            P.op("dve", lambda e: e.tensor_copy(out=sm[:, GG, :], in_=pg[:, 0:8]), reads=[pgb], writes=[smb])
            P.op("act", lambda e: e.activation(out=sm[:, EG, :], in_=pg[:, 0:8], func=AF.Exp), reads=[pgb], writes=[smb])
            P.op("act", lambda e: e.activation(out=sm[:, GL, :], in_=pg[:, 8:16], func=AF.Exp), reads=[pgb], writes=[smb])
            P.op("dve", lambda e: e.tensor_tensor(out=sm[:, EKD, :], in0=pg[:, 8:16], in1=sm[:, GG, :], op=ALU.subtract),
                 reads=[pgb, smb], writes=[smb])
            P.op("act", lambda e: e.activation(out=sm[:, EKD, :], in_=sm[:, EKD, :], func=AF.Exp), reads=[smb], writes=[smb])
            P.op("dve", lambda e: e.tensor_tensor(out=sm[:, BG, :], in0=sm[:, BETA, :], in1=sm[:, EG, :], op=ALU.mult),
                 reads=[smb], writes=[smb])
            DIAGG, DIAGEG, KBG, KD, VB = 0, 1, 2, 3, 4
            idb = K.ident[:].unsqueeze(1).to_broadcast([128, 8, 128])

            def bc(i):
                return sm[:, i, :].unsqueeze(2).to_broadcast([128, 8, 128])
            ktok = tok[:, 1024:2048].rearrange("p (h c) -> p h c", c=128)
            vtok = tok[:, 2048:3072].rearrange("p (h c) -> p h c", c=128)
            P.op("pool", lambda e: e.tensor_tensor(out=dg[:, DIAGG], in0=idb, in1=bc(GG), op=ALU.mult),
                 reads=[smb], writes=[dgb])
            P.op("pool", lambda e: e.tensor_tensor(out=dg[:, DIAGEG], in0=idb, in1=bc(EG), op=ALU.mult),
                 reads=[smb], writes=[dgb])
            P.op("pool", lambda e: e.tensor_tensor(out=dg[:, KBG], in0=ktok, in1=bc(BG), op=ALU.mult),
                 reads=[smb, tkb], writes=[dgb])
            P.op("dve", lambda e: e.tensor_tensor(out=dg[:, KD], in0=ktok, in1=bc(EKD), op=ALU.mult),
                 reads=[smb, tkb], writes=[dgb])
            P.op("dve", lambda e: e.tensor_tensor(out=dg[:, VB], in0=vtok, in1=bc(BETA), op=ALU.mult),
                 reads=[smb, tkb], writes=[dgb])
            DM, DMS, X0, X1, Y0, Y1, QA, QAT, TT_, U, WT, QGT, VN = range(13)
            wk = []
            wb = []
            for h in range(8):
                t, b = wkr[h].next()
                wk.append(t)
                wb.append(b)
            if getattr(K, 'scan_stage', 99) <= 1:
                return
            if getattr(K, 'scan_serial', False):
                P.phase_end()
            pkk, pqk, pgr = [], [], []
            for h in range(8):
                a, ab = psD.next()
                P.op("pe", lambda e, a=a, h=h: e.matmul(a, lhsT=kT[:, h, :], rhs=kT[:, h, :], start=True, stop=True),
                     reads=[kTb], writes=[ab])
                pkk.append((a, ab))
                a, ab = psD.next()
                P.op("pe", lambda e, a=a, h=h: e.matmul(a, lhsT=qT[:, h, :], rhs=kT[:, h, :], start=True, stop=True),
                     reads=[kTb, qTb], writes=[ab])
                pqk.append((a, ab))
                a, ab = psA.next()
                P.op("pe", lambda e, a=a, h=h: e.matmul(a, lhsT=K.onesf[:], rhs=dg[:, DIAGG, h, :], start=True, stop=False),
                     reads=[dgb], writes=[ab])
                P.op("pe", lambda e, a=a, h=h: e.matmul(a, lhsT=K.ident[:], rhs=MASKA[d], start=False, stop=True),
                     reads=[bC], writes=[ab])
                pgr.append((a, ab))
            if getattr(K, 'scan_stage', 99) <= 2:
                return
            if getattr(K, 'scan_serial', False):
                P.phase_end()
            for h in range(8):
                w = wk[h]
                P.op("act", lambda e, w=w, h=h: e.activation(out=w[:, DM, :], in_=pgr[h][0], func=AF.Exp,
                                                             bias=sm[:, GG, h:h + 1], scale=-1.0),
                     reads=[pgr[h][1], smb], writes=[wb[h]])
                P.op("pool", lambda e, w=w: e.tensor_tensor(out=w[:, DMS, :], in0=w[:, DM, :], in1=M01S[d], op=ALU.mult),
                     reads=[wb[h], bC], writes=[wb[h]])
                P.op("dve", lambda e, w=w, h=h: e.scalar_tensor_tensor(
                    out=w[:, X0, :], in0=pkk[h][0], scalar=sm[:, NBETA, h:h + 1], in1=w[:, DMS, :],
                    op0=ALU.mult, op1=ALU.mult), reads=[pkk[h][1], smb, wb[h]], writes=[wb[h]])
                P.op("dve", lambda e, w=w, h=h: e.tensor_tensor(out=w[:, QA, :], in0=pqk[h][0], in1=w[:, DM, :], op=ALU.mult),
                     reads=[pqk[h][1], wb[h]], writes=[wb[h]])
            if getattr(K, 'scan_stage', 99) <= 3:
                return
            if getattr(K, 'scan_serial', False):
                P.phase_end()
            pa, pb_ = [], []
            for h in range(8):
                w = wk[h]
                a, ab = psA.next()
                P.op("pe", lambda e, a=a, w=w: e.transpose(out=a, in_=w[:, X0, :], identity=K.ident[:]),
                     reads=[wb[h]], writes=[ab])
                pa.append((a, ab))
                a, ab = psD.next()
                P.op("pe", lambda e, a=a, w=w: e.transpose(out=a, in_=w[:, QA, :], identity=K.ident[:]),
                     reads=[wb[h]], writes=[ab])
                pb_.append((a, ab))
            for h in range(8):
                w = wk[h]
                evac(w[:, Y0, :], pa[h][0], [pa[h][1]], [wb[h]], "act")
                evac(w[:, QAT, :], pb_[h][0], [pb_[h][1]], [wb[h]], "dve")
                P.op("pool", lambda e, w=w: e.tensor_tensor(out=w[:, TT_, :], in0=w[:, Y0, :], in1=K.ident[:], op=ALU.add),
                     reads=[wb[h]], writes=[wb[h]])
            if getattr(K, 'scan_dbg', False) and blk == 0 and d == 0:
                for h in range(8):
                    for i_ in (0, 1, 2, 4, 6, 7, 8):
                        P.dma("sp", K.dbgwk[h, :, i_, :], wk[h][:, i_, :], reads=[wb[h]])
                P.dma("sp", K.dbgsm[:, 0:9, :], sm[:, 0:9, :], reads=[smb])
            if getattr(K, 'scan_stage', 99) <= 4:
                return
            if lim is not None and blk == 0 and d == 0:
                print("ops before S5:", cnt[0])
            if getattr(K, 'scan_serial', False):
                P.phase_end()
            for lvl in range(1, 7):
                xo, yo = (X0, Y0) if lvl % 2 == 1 else (X1, Y1)
                xn, yn = (X1, Y1) if lvl % 2 == 1 else (X0, Y0)
                px, py = [], []
                for h in range(8):
                    w = wk[h]
                    a, ab = psA.next()
                    P.op("pe", lambda e, a=a, w=w, xo=xo, yo=yo: e.matmul(a, lhsT=w[:, yo, :], rhs=w[:, xo, :],
                                                                        start=True, stop=True), reads=[wb[h]], writes=[ab])
                    px.append((a, ab))
                    if lvl < 6:
                        a, ab = psD.next()
                        P.op("pe", lambda e, a=a, w=w, xo=xo, yo=yo: e.matmul(a, lhsT=w[:, xo, :], rhs=w[:, yo, :],
                                                                            start=True, stop=True), reads=[wb[h]], writes=[ab])
                        py.append((a, ab))
                for h in range(8):
                    w = wk[h]
                    P.op("act", lambda e, w=w, h=h, xn=xn, px=px: e.activation(out=w[:, xn, :], in_=px[h][0], func=AF.Copy),
                         reads=[px[h][1]], writes=[wb[h]])
                    if lvl < 6:
                        P.op("dve", lambda e, w=w, h=h, yn=yn, py=py: e.tensor_copy(out=w[:, yn, :], in_=py[h][0]),
                             reads=[py[h][1]], writes=[wb[h]])
                if lim is not None and blk == 0 and d == 0:
                    print("lvl", lvl, "ops before pu:", cnt[0])
                pu = []
                for h in range(8):
                    w = wk[h]
                    a, ab = psD.next()
                    P.op("pe", lambda e, a=a, w=w, xn=xn: e.matmul(a, lhsT=w[:, xn, :], rhs=w[:, TT_, :],
                                                                 start=True, stop=True), reads=[wb[h]], writes=[ab])
                    pu.append((a, ab))
                for h in range(8):
                    w = wk[h]
                    P.op("dve", lambda e, w=w, h=h, pu=pu: e.tensor_tensor(out=w[:, TT_, :], in0=pu[h][0], in1=w[:, TT_, :],
                                                                  op=ALU.add), reads=[pu[h][1], wb[h]], writes=[wb[h]])
            if getattr(K, 'scan_stage', 99) <= 5:
                return
            if getattr(K, 'scan_serial', False):
                P.phase_end()
            p1, p2, p3 = [], [], []
            qtok = tok[:, 0:1024].rearrange("p (h c) -> p h c", c=128)
            for h in range(8):
                w = wk[h]
                a, ab = psA.next()
                P.op("pe", lambda e, a=a, w=w, h=h: e.matmul(a, lhsT=w[:, TT_, :], rhs=dg[:, VB, h, :], start=True, stop=True),
                     reads=[wb[h], dgb], writes=[ab])
                p1.append((a, ab))
                a, ab = psD.next()
                P.op("pe", lambda e, a=a, w=w, h=h: e.matmul(a, lhsT=dg[:, KBG, h, :], rhs=w[:, TT_, :], start=True, stop=True),
                     reads=[wb[h], dgb], writes=[ab])
                p2.append((a, ab))
                a, ab = psA.next()
                P.op("pe", lambda e, a=a, h=h: e.matmul(a, lhsT=qtok[:, h, :], rhs=dg[:, DIAGEG, h, :], start=True, stop=True),
                     reads=[tkb, dgb], writes=[ab])
                p3.append((a, ab))
            for h in range(8):
                w = wk[h]
                evac(w[:, U, :], p1[h][0], [p1[h][1]], [wb[h]], "act")
                evac(w[:, WT, :], p2[h][0], [p2[h][1]], [wb[h]], "dve")
                evac(w[:, QGT, :], p3[h][0], [p3[h][1]], [wb[h]], "act")
            if getattr(K, 'scan_stage', 99) <= 6:
                return
            if getattr(K, 'scan_serial', False):
                P.phase_end()
            pv = []
            for h in range(8):
                w = wk[h]
                a, ab = psD.next()
                P.op("pe", lambda e, a=a, w=w, h=h: e.matmul(a, lhsT=w[:, WT, :], rhs=S[d][h][:], start=True, stop=True),
                     reads=[wb[h], bS[d][h]], writes=[ab])
                pv.append((a, ab))
            for h in range(8):
                w = wk[h]
                P.op("dve", lambda e, w=w, h=h: e.tensor_tensor(out=w[:, VN, :], in0=w[:, U, :], in1=pv[h][0], op=ALU.subtract),
                     reads=[pv[h][1], wb[h]], writes=[wb[h]])
            po, pd = [], []
            for h in range(8):
                w = wk[h]
                if with_out:
                    a, ab = psA.next()
                    P.op("pe", lambda e, a=a, w=w, h=h: e.matmul(a, lhsT=w[:, QGT, :], rhs=S[d][h][:], start=True, stop=False),
                         reads=[wb[h], bS[d][h]], writes=[ab])
                    P.op("pe", lambda e, a=a, w=w: e.matmul(a, lhsT=w[:, QAT, :], rhs=w[:, VN, :], start=False, stop=True),
                         reads=[wb[h]], writes=[ab])
                    po.append((a, ab))
                a, ab = psD.next()
                P.op("pe", lambda e, a=a, w=w, h=h: e.matmul(a, lhsT=dg[:, KD, h, :], rhs=w[:, VN, :], start=True, stop=True),
                     reads=[wb[h], dgb], writes=[ab])
                pd.append((a, ab))
            if with_out:
                ob, obb = obr.next()
            for h in range(8):
                P.op("dve", lambda e, h=h: e.scalar_tensor_tensor(
                    out=S[d][h][:], in0=S[d][h][:], scalar=sm[:, GL, h:h + 1], in1=pd[h][0], op0=ALU.mult, op1=ALU.add),
                    reads=[pd[h][1], smb], writes=[bS[d][h]])
                if with_out:
                    P.op("act", lambda e, h=h: e.activation(out=ob[:, h * 128:(h + 1) * 128], in_=po[h][0], func=AF.Copy),
                         reads=[po[h][1]], writes=[obb])
            if with_out:
                dst = K.o_dir[d]
                P.dma("sp", dst[t0 - CTX:t0 - CTX + 128, :], ob[:], reads=[obb])

        for s in range(getattr(K, 'scan_steps', NB)):
            job(order[0][s], 0)
            job(order[1][s], 1)
        P.phase_end()


def phase_dnout(K):
    nc, P = K.nc, K.P
    with contextlib.ExitStack() as st:
        gn = sb(st, nc, "do_gn", [128, 1024], F32)
        ofr = Rot([sb(st, nc, f"do_of{i}", [128, 1024], F32) for i in range(2)])
        obr = Rot([sb(st, nc, f"do_ob{i}", [128, 1024], F32) for i in range(2)])
        sgr = Rot([sb(st, nc, f"do_sg{i}", [128, 1024], F32) for i in range(2)])
        sqr = Rot([sb(st, nc, f"do_sq{i}", [128, 1024], F32) for i in range(2)])
        msr = Rot([sb(st, nc, f"do_ms{i}", [128, 8], F32) for i in range(2)])
        ygr = Rot([sb(st, nc, f"do_yg{i}", [128, 8, 128], BF16) for i in range(2)])
        psr = ps_banks(st, nc, 4, "dops")
        bg = Buf()
        P.dma("sp", gn[:], K.dn_gn_l, writes=[bg])
        n = 0
        for tt in range(L // 128):
            t0 = tt * 128
            of, ofb = ofr.next()
            ob, obb = obr.next()
            sg, sgb = sgr.next()
            sq, sqb = sqr.next()
            ms, msb = msr.next()
            yg, ygb = ygr.next()
            P.dma("sp", of[:], K.o_dir[0][t0:t0 + 128, :], writes=[ofb])
            P.dma("sp", ob[:], K.o_dir[1][t0:t0 + 128, :], writes=[obb])
            P.dma("sp", sg[:], K.sgate[CTX + t0:CTX + t0 + 128, :], writes=[sgb])
            P.op("dve", lambda e, of=of, ob=ob: e.tensor_tensor(out=of[:], in0=of[:], in1=ob[:], op=ALU.add),
                 reads=[obb], writes=[ofb])
            P.op("act", lambda e, of=of, sq=sq: e.activation(out=sq[:], in_=of[:], func=AF.Square), reads=[ofb], writes=[sqb])
            P.op("dve", lambda e, sq=sq, ms=ms: e.tensor_reduce(out=ms[:], in_=sq[:].rearrange("p (h c) -> p h c", c=128),
                                                               axis=AX.X, op=ALU.add), reads=[sqb], writes=[msb])
            P.op("act", lambda e, ms=ms: e.activation(out=ms[:], in_=ms[:], func=AF.Sqrt, bias=K.epsc[:, 0:1], scale=1.0 / 128),
                 reads=[msb], writes=[msb])
            P.op("dve", lambda e, ms=ms: e.reciprocal(out=ms[:], in_=ms[:]), reads=[msb], writes=[msb])
            P.op("dve", lambda e, of=of, ms=ms: e.tensor_tensor(
                out=of[:].rearrange("p (h c) -> p h c", c=128), in0=of[:].rearrange("p (h c) -> p h c", c=128),
                in1=ms[:].unsqueeze(2).to_broadcast([128, 8, 128]), op=ALU.mult), reads=[msb], writes=[ofb])
            P.op("pool", lambda e, of=of, sg=sg: e.tensor_tensor(out=sg[:], in0=sg[:], in1=gn[:], op=ALU.mult),
                 reads=[bg], writes=[sgb])
            P.op("pool", lambda e, of=of, sg=sg: e.tensor_tensor(out=of[:], in0=of[:], in1=sg[:], op=ALU.mult),
                 reads=[sgb], writes=[ofb])
            for half in range(2):
                ps, pb = psr.next()
                for j in range(4):
                    cc = half * 4 + j
                    P.op("pe", lambda e, ps=ps, of=of, j=j, cc=cc: e.transpose(
                        out=ps[:, j * 128:(j + 1) * 128], in_=of[:, cc * 128:(cc + 1) * 128], identity=K.ident[:]),
                        reads=[ofb], writes=[pb])
                if n % 2 == 0:
                    P.op("act", lambda e, ps=ps, yg=yg, half=half: e.activation(
                        out=yg[:, half * 4:(half + 1) * 4, :], in_=ps[:, :].rearrange("p (j t) -> p j t", t=128),
                        func=AF.Copy), reads=[pb], writes=[ygb])
                else:
                    P.op("dve", lambda e, ps=ps, yg=yg, half=half: e.tensor_copy(
                        out=yg[:, half * 4:(half + 1) * 4, :], in_=ps[:, :].rearrange("p (j t) -> p j t", t=128)),
                        reads=[pb], writes=[ygb])
                n += 1
            P.dma("sp", K.ygT[:, :, t0:t0 + 128].rearrange("c p t -> p c t"), yg[:], reads=[ygb])
        P.phase_end()


def phase_outproj(K, srcT, W, l, m):
    nc, P = K.nc, K.P
    with contextlib.ExitStack() as st:
        Wb = sb(st, nc, "op_W", [128, 8, 1024], BF16)
        srr = Rot([sb(st, nc, f"op_src{i}", [128, 8, 512], BF16) for i in range(2)])
        hr = Rot([sb(st, nc, f"op_h{i}", [128, 8, 512], F32) for i in range(2)])
        psr = ps_banks(st, nc, 4, "opps")
        bW = Buf()
        for cc in range(8):
            P.dma("pool", Wb[:, cc, :], W[cc * 128:(cc + 1) * 128, :], writes=[bW])
        for g in range(L // 512):
            src, sbb = srr.next()
            h, hb = hr.next()
            P.dma("sp", src[:], srcT[:, :, g * 512:(g + 1) * 512].rearrange("c p t -> p c t"), writes=[sbb])
            P.dma("sp", h[:], K.hT[:, :, g * 512:(g + 1) * 512].rearrange("c p t -> p c t"), writes=[hb])
            for dc in range(8):
                ps, pb = psr.next()
                for cc in range(8):
                    P.op("pe", lambda e, ps=ps, src=src, cc=cc, dc=dc: e.matmul(
                        ps[:, :], lhsT=Wb[:, cc, dc * 128:(dc + 1) * 128], rhs=src[:, cc, :],
                        start=(cc == 0), stop=(cc == 7)), reads=[bW, sbb], writes=[pb])
                P.op("dve", lambda e, ps=ps, h=h, dc=dc: e.scalar_tensor_tensor(
                    out=h[:, dc, :], in0=ps[:, :], scalar=K.MOD[:, l, m * 8 + dc, 0:1], in1=h[:, dc, :],
                    op0=ALU.mult, op1=ALU.add), reads=[pb, K.bMOD], writes=[hb])
            P.dma("sp", K.hT[:, :, g * 512:(g + 1) * 512].rearrange("c p t -> p c t"), h[:], reads=[hb])
        P.phase_end()


def phase_peer_tables(K, l):
    nc, P = K.nc, K.P
    with contextlib.ExitStack() as st:
        ur = Rot([sb(st, nc, f"pt_u{i}", [128, 8, 128], BF16) for i in range(3)])
        vr = Rot([sb(st, nc, f"pt_v{i}", [128, 1024], BF16) for i in range(3)])
        for i in range(128):
            u, ub = ur.next()
            v, vb = vr.next()
            for kc in range(8):
                P.dma("pool", u[:, kc, :], K.peer_u_l[l, i, kc], writes=[ub])
            P.dma("pool", v[:], K.peer_v[l, i * 128:(i + 1) * 128, :], writes=[vb])
            P.dma("sp", K.Ub[i], u[:], reads=[ub])
            P.dma("sp", K.Vb[i], v[:], reads=[vb])
        P.phase_end()


def phase_peer(K, l):
    nc, P = K.nc, K.P
    NEG = -1.0e30
    with contextlib.ExitStack() as st:
        Wq = sb(st, nc, "pe_Wq", [128, 8, 2048], BF16)
        kT = sb(st, nc, "pe_kT", [128, 16, 128], F32)
        xpr = Rot([sb(st, nc, f"pe_xp{i}", [128, 8, 128], BF16) for i in range(2)])
        qsb = sb(st, nc, "pe_q", [128, 16, 128], F32)
        sc = sb(st, nc, "pe_sc", [128, 16, 128], F32)
        sc2 = sb(st, nc, "pe_sc2", [128, 16, 128], F32)
        v16 = sb(st, nc, "pe_v16", [128, 16, 16], F32)
        cand = sb(st, nc, "pe_cand", [128, 8, 256], F32)
        cand2 = sb(st, nc, "pe_cand2", [128, 8, 256], F32)
        c16 = sb(st, nc, "pe_c16", [128, 8, 16], F32)
        e16 = sb(st, nc, "pe_e16", [128, 8, 16], F32)
        sm = sb(st, nc, "pe_sm", [128, 4, 8], F32)
        sumr = Rot([sb(st, nc, f"pe_sum{i}", [128, 2048], F32) for i in range(2)])
        mkr = Rot([sb(st, nc, f"pe_mk{i}", [128, 2048], F32) for i in range(2)])
        Er = Rot([sb(st, nc, f"pe_E{i}", [128, 2048], F32) for i in range(2)])
        acc = sb(st, nc, "pe_acc", [128, 2048], F32)
        WallT = sb(st, nc, "pe_WallT", [128, 128, 128], BF16)
        ur = Rot([sb(st, nc, f"pe_u{i}", [128, 8, 128], BF16) for i in range(3)])
        vr = Rot([sb(st, nc, f"pe_v{i}", [128, 1024], BF16) for i in range(3)])
        gr = Rot([sb(st, nc, f"pe_g{i}", [128, 128], F32) for i in range(2)])
        cfr = Rot([sb(st, nc, f"pe_cf{i}", [128, 128], BF16) for i in range(2)])
        ysb = sb(st, nc, "pe_y", [128, 1024], F32)
        hsb = sb(st, nc, "pe_h", [128, 8, 128], F32)
        gps = ps_banks(st, nc, 4, "pegps")
        hps = ps_banks(st, nc, 2, "pehps")
        yps = ps_banks(st, nc, 2, "peyps")
        bW, bk, bq, bsc, bsc2, bv, bc, bc2, bc16, be, bsm, bacc, bwall, by, bh = [Buf() for _ in range(15)]
        for kc in range(8):
            P.dma("pool", Wq[:, kc, :], K.peer_w_q[l, kc * 128:(kc + 1) * 128, :], writes=[bW])
        P.dma("sp", kT[:], K.peer_kT_l[l], writes=[bk])
        for g in range(getattr(K, 'peer_groups', L // 128)):
            t0 = g * 128
            xp, xb = xpr.next()
            P.dma("sp", xp[:], K.hnT[:, :, CTX + t0:CTX + t0 + 128].rearrange("c p t -> p c t"), writes=[xb])
            P.dma("sp", hsb[:], K.hT[:, :, t0:t0 + 128].rearrange("c p t -> p c t"), writes=[bh])
            for b4 in range(4):
                ps, pb = gps.next()
                for j in range(4):
                    hp = b4 * 4 + j
                    for kc in range(8):
                        P.op("pe", lambda e, ps=ps, xp=xp, j=j, hp=hp, kc=kc: e.matmul(
                            ps[:, j * 128:(j + 1) * 128], lhsT=Wq[:, kc, hp * 128:(hp + 1) * 128], rhs=xp[:, kc, :],
                            start=(kc == 0), stop=(kc == 7)), reads=[bW, xb], writes=[pb])
                P.op("act", lambda e, ps=ps, b4=b4: e.activation(
                    out=qsb[:, b4 * 4:(b4 + 1) * 4, :], in_=ps[:, :].rearrange("p (j t) -> p j t", t=128), func=AF.Copy),
                    reads=[pb], writes=[bq])
            for b4 in range(4):
                ps, pb = gps.next()
                for j in range(4):
                    hp = b4 * 4 + j
                    P.op("pe", lambda e, ps=ps, j=j, hp=hp: e.matmul(
                        ps[:, j * 128:(j + 1) * 128], lhsT=qsb[:, hp, :], rhs=kT[:, hp, :], start=True, stop=True),
                        reads=[bq, bk], writes=[pb])
                P.op("act", lambda e, ps=ps, b4=b4: e.activation(
                    out=sc[:, b4 * 4:(b4 + 1) * 4, :], in_=ps[:, :].rearrange("p (j t) -> p j t", t=128), func=AF.Copy),
                    reads=[pb], writes=[bsc])
            for hp in range(16):
                P.op("dve", lambda e, hp=hp: e.max(out=v16[:, hp, 0:8], in_=sc[:, hp, :]), reads=[bsc], writes=[bv])
                P.op("dve", lambda e, hp=hp: e.match_replace(out=sc2[:, hp, :], in_to_replace=v16[:, hp, 0:8],
                                                            in_values=sc[:, hp, :], imm_value=NEG),
                     reads=[bsc, bv], writes=[bsc2])
                P.op("dve", lambda e, hp=hp: e.max(out=v16[:, hp, 8:16], in_=sc2[:, hp, :]), reads=[bsc2], writes=[bv])
            v4 = v16[:].rearrange("p (h two) k -> p h two k", two=2)
            P.op("dve", lambda e: e.tensor_tensor(
                out=cand[:].rearrange("p h (a b) -> p h a b", b=16),
                in0=v4[:, :, 0, :].unsqueeze(3).to_broadcast([128, 8, 16, 16]),
                in1=v4[:, :, 1, :].unsqueeze(2).to_broadcast([128, 8, 16, 16]), op=ALU.add), reads=[bv], writes=[bc])
            for h in range(8):
                P.op("dve", lambda e, h=h: e.max(out=c16[:, h, 0:8], in_=cand[:, h, :]), reads=[bc], writes=[bc16])
                P.op("dve", lambda e, h=h: e.match_replace(out=cand2[:, h, :], in_to_replace=c16[:, h, 0:8],
                                                          in_values=cand[:, h, :], imm_value=NEG),
                     reads=[bc, bc16], writes=[bc2])
                P.op("dve", lambda e, h=h: e.max(out=c16[:, h, 8:16], in_=cand2[:, h, :]), reads=[bc2], writes=[bc16])
            P.op("dve", lambda e: e.tensor_copy(out=sm[:, 0, :], in_=c16[:, :, 15]), reads=[bc16], writes=[bsm])
            P.op("dve", lambda e: e.tensor_scalar(out=sm[:, 1, :], in0=c16[:, :, 0], scalar1=-1.0, scalar2=None,
                                                  op0=ALU.mult), reads=[bc16], writes=[bsm])
            P.op("dve", lambda e: e.tensor_tensor(out=e16[:], in0=c16[:], in1=sm[:, 1, :].unsqueeze(2).to_broadcast([128, 8, 16]),
                                                  op=ALU.add), reads=[bc16, bsm], writes=[be])
            P.op("act", lambda e: e.activation(out=e16[:], in_=e16[:], func=AF.Exp), reads=[be], writes=[be])
            P.op("dve", lambda e: e.tensor_reduce(out=sm[:, 2, :], in_=e16[:], axis=AX.X, op=ALU.add), reads=[be], writes=[bsm])
            P.op("dve", lambda e: e.reciprocal(out=sm[:, 3, :], in_=sm[:, 2, :]), reads=[bsm], writes=[bsm])
            for q4 in range(8):
                for h in range(8):
                    su, sub_ = sumr.next()
                    mk_, mkb = mkr.next()
                    E_, Eb = Er.next()
                    P.op("dve", lambda e, su=su, h=h, q4=q4: e.tensor_tensor(
                        out=su[:].rearrange("p (i j) -> p i j", j=128),
                        in0=sc[:, 2 * h, q4 * 16:(q4 + 1) * 16].unsqueeze(2).to_broadcast([128, 16, 128]),
                        in1=sc[:, 2 * h + 1, :].unsqueeze(1).to_broadcast([128, 16, 128]), op=ALU.add),
                        reads=[bsc], writes=[sub_])
                    P.op("dve", lambda e, su=su, mk_=mk_, h=h: e.tensor_scalar(
                        out=mk_[:], in0=su[:], scalar1=sm[:, 0, h:h + 1], scalar2=None, op0=ALU.is_ge),
                        reads=[sub_, bsm], writes=[mkb])
                    P.op("act", lambda e, su=su, E_=E_, h=h: e.activation(
                        out=E_[:], in_=su[:], func=AF.Exp, bias=sm[:, 1, h:h + 1], scale=1.0), reads=[sub_, bsm], writes=[Eb])
                    P.op("pool", lambda e, E_=E_, mk_=mk_: e.tensor_tensor(out=E_[:], in0=E_[:], in1=mk_[:], op=ALU.mult),
                         reads=[mkb], writes=[Eb])
                    if h == 0:
                        P.op("dve", lambda e, E_=E_, h=h: e.tensor_scalar(
                            out=acc[:], in0=E_[:], scalar1=sm[:, 3, h:h + 1], scalar2=None, op0=ALU.mult),
                            reads=[Eb, bsm], writes=[bacc])
                    else:
                        P.op("dve", lambda e, E_=E_, h=h: e.scalar_tensor_tensor(
                            out=acc[:], in0=E_[:], scalar=sm[:, 3, h:h + 1], in1=acc[:], op0=ALU.mult, op1=ALU.add),
                            reads=[Eb, bsm], writes=[bacc])
                for b8 in range(4):
                    ps, pb = gps.next()
                    for j in range(4):
                        ii = b8 * 4 + j
                        P.op("pe", lambda e, ps=ps, j=j, ii=ii: e.transpose(
                            out=ps[:, j * 128:(j + 1) * 128], in_=acc[:, ii * 128:(ii + 1) * 128], identity=K.ident[:]),
                            reads=[bacc], writes=[pb])
                    i0 = q4 * 16 + b8 * 4
                    P.op("act", lambda e, ps=ps, i0=i0: e.activation(
                        out=WallT[:, i0:i0 + 4, :], in_=ps[:, :].rearrange("p (j t) -> p j t", t=128), func=AF.Copy),
                        reads=[pb], writes=[bwall])
            y0, y0b = yps.next()
            y1, y1b = yps.next()
            for i in range(128):
                u, ub = ur.next()
                v, vb = vr.next()
                P.dma("sp", u[:], K.Ub[i], writes=[ub])
                P.dma("sp", v[:], K.Vb[i], writes=[vb])
                hp_, hpb = hps.next()
                for kc in range(8):
                    P.op("pe", lambda e, hp_=hp_, u=u, xp=xp, kc=kc: e.matmul(
                        hp_[:, 0:128], lhsT=u[:, kc, :], rhs=xp[:, kc, :], start=(kc == 0), stop=(kc == 7)),
                        reads=[ub, xb], writes=[hpb])
                g_, gb = gr.next()
                P.op("act", lambda e, hp_=hp_, g_=g_: e.activation(out=g_[:], in_=hp_[:, 0:128], func=AF.Gelu),
                     reads=[hpb], writes=[gb])
                cf, cfb = cfr.next()
                P.op("dve", lambda e, g_=g_, cf=cf, i=i: e.tensor_tensor(out=cf[:], in0=g_[:], in1=WallT[:, i, :], op=ALU.mult),
                     reads=[gb, bwall], writes=[cfb])
                P.op("pe", lambda e, cf=cf, v=v, i=i: e.matmul(y0[:, :], lhsT=cf[:], rhs=v[:, 0:512], start=(i == 0), stop=(i == 127)),
                     reads=[cfb, vb], writes=[y0b])
                P.op("pe", lambda e, cf=cf, v=v, i=i: e.matmul(y1[:, :], lhsT=cf[:], rhs=v[:, 512:1024], start=(i == 0), stop=(i == 127)),
                     reads=[cfb, vb], writes=[y1b])
            P.op("act", lambda e: e.activation(out=ysb[:, 0:512], in_=y0[:, :], func=AF.Copy), reads=[y0b], writes=[by])
            P.op("act", lambda e: e.activation(out=ysb[:, 512:1024], in_=y1[:, :], func=AF.Copy), reads=[y1b], writes=[by])
            for half in range(2):
                ps, pb = gps.next()
                for j in range(4):
                    dc = half * 4 + j
                    P.op("pe", lambda e, ps=ps, j=j, dc=dc: e.transpose(
                        out=ps[:, j * 128:(j + 1) * 128], in_=ysb[:, dc * 128:(dc + 1) * 128], identity=K.ident[:]),
                        reads=[by], writes=[pb])
                for j in range(4):
                    dc = half * 4 + j
                    P.op("dve", lambda e, ps=ps, j=j, dc=dc: e.scalar_tensor_tensor(
                        out=hsb[:, dc, :], in0=ps[:, j * 128:(j + 1) * 128], scalar=K.MOD[:, l, 5 * 8 + dc, 0:1],
                        in1=hsb[:, dc, :], op0=ALU.mult, op1=ALU.add), reads=[pb, K.bMOD], writes=[bh])
            P.dma("sp", K.hT[:, :, t0:t0 + 128].rearrange("c p t -> p c t"), hsb[:], reads=[bh])
        P.phase_end()


def phase_hyin(K):
    nc, P = K.nc, K.P
    with contextlib.ExitStack() as st:
        Wb = sb(st, nc, "hi_Wb", [128, 8, 3072], BF16)
        cw = sb(st, nc, "hi_cw", [128, 24, 3], F32)
        cb = sb(st, nc, "hi_cb", [128, 24], F32)
        hnr = Rot([sb(st, nc, f"hi_hn{i}", [128, 8, 512], BF16) for i in range(2)])
        ztr = Rot([sb(st, nc, f"hi_zt{i}", [128, 24, 512], F32) for i in range(2)])
        zps = ps_banks(st, nc, 4, "hiz")
        bW, bcw = Buf(), Buf()
        for kc in range(8):
            for c0 in range(0, 3072, 1536):
                P.dma("pool", Wb[:, kc, c0:c0 + 1536], K.hy_w_in[kc * 128:(kc + 1) * 128, c0:c0 + 1536], writes=[bW])
        P.dma("sp", cw[:], K.hy_cw_l, writes=[bcw])
        P.dma("sp", cb[:], K.hy_cb_l, writes=[bcw])
        for g in range(L // 512):
            hn, hb = hnr.next()
            zt, zb = ztr.next()
            P.dma("sp", hn[:], K.hnT[:, :, CTX + g * 512:CTX + (g + 1) * 512].rearrange("c p t -> p c t"), writes=[hb])
            for cc in range(24):
                ps, pb = zps.next()
                for kc in range(8):
                    P.op("pe", lambda e, ps=ps, hn=hn, cc=cc, kc=kc: e.matmul(
                        ps[:, :], lhsT=Wb[:, kc, cc * 128:(cc + 1) * 128], rhs=hn[:, kc, :],
                        start=(kc == 0), stop=(kc == 7)), reads=[bW, hb], writes=[pb])
                P.op("dve", lambda e, ps=ps, zt=zt, cc=cc: e.tensor_scalar(
                    out=zt[:, cc, :], in0=ps[:, :], scalar1=cw[:, cc, 1:2], scalar2=cb[:, cc:cc + 1],
                    op0=ALU.mult, op1=ALU.add), reads=[pb, bcw], writes=[zb])
                for k in (0, 2):
                    s = k - 1
                    lo, hi = max(0, -s), 64 - max(0, s)
                    P.op("dve", lambda e, ps=ps, zt=zt, cc=cc, k=k, s=s, lo=lo, hi=hi: e.scalar_tensor_tensor(
                        out=zt[:, cc, :].rearrange("p (r w) -> p r w", w=64)[:, :, lo:hi],
                        in0=ps[:, :].rearrange("p (r w) -> p r w", w=64)[:, :, lo + s:hi + s],
                        scalar=cw[:, cc, k:k + 1],
                        in1=zt[:, cc, :].rearrange("p (r w) -> p r w", w=64)[:, :, lo:hi],
                        op0=ALU.mult, op1=ALU.add), reads=[pb, bcw], writes=[zb])
            P.dma("sp", K.zT[:, :, g * 512:(g + 1) * 512].rearrange("c p t -> p c t"), zt[:], reads=[zb])
        P.phase_end()


def _wrap_pi(P, t, tb, tmpa, tab, n, cols):
    PI = math.pi
    for _ in range(2):
        P.op("dve", lambda e: e.tensor_scalar(out=tmpa[0:n, 0:cols], in0=t[0:n, 0:cols], scalar1=PI, scalar2=-2 * PI,
                                              op0=ALU.is_gt, op1=ALU.mult), reads=[tb], writes=[tab])
        P.op("dve", lambda e: e.tensor_tensor(out=t[0:n, 0:cols], in0=t[0:n, 0:cols], in1=tmpa[0:n, 0:cols], op=ALU.add),
             reads=[tab], writes=[tb])
        P.op("dve", lambda e: e.tensor_scalar(out=tmpa[0:n, 0:cols], in0=t[0:n, 0:cols], scalar1=-PI, scalar2=2 * PI,
                                              op0=ALU.is_lt, op1=ALU.mult), reads=[tb], writes=[tab])
        P.op("dve", lambda e: e.tensor_tensor(out=t[0:n, 0:cols], in0=t[0:n, 0:cols], in1=tmpa[0:n, 0:cols], op=ALU.add),
             reads=[tab], writes=[tb])


def phase_hyfilt(K):
    nc, P = K.nc, K.P
    with contextlib.ExitStack() as st:
        w1 = sb(st, nc, "hf_w1", [33, 64], F32)
        w2 = sb(st, nc, "hf_w2", [64, 64], F32)
        w3 = sb(st, nc, "hf_w3", [64, 64], F32)
        w4 = sb(st, nc, "hf_w4", [64, 4096], F32)
        pv = sb(st, nc, "hf_pv", [64, 8], F32)
        dl = sb(st, nc, "hf_dl", [128, 8], F32)
        zpr = Rot([sb(st, nc, f"hf_zp{i}", [33, 512], F32) for i in range(2)])
        tpr = Rot([sb(st, nc, f"hf_tp{i}", [128, 512], F32) for i in range(2)])
        hr = Rot([sb(st, nc, f"hf_h{i}", [64, 512], F32) for i in range(3)])
        tmpa = sb(st, nc, "hf_tmp", [64, 512], F32)
        dcr = Rot([sb(st, nc, f"hf_dc{i}", [128, 8, 512], F32) for i in range(2)])
        fr_ = Rot([sb(st, nc, f"hf_f{i}", [128, 512], F32) for i in range(4)])
        psr = ps_banks(st, nc, 4, "hfps")
        bw, btm = Buf(), Buf()
        P.dma("sp", w1[:], K.hy_f_w1, writes=[bw])
        P.dma("sp", w2[:], K.hy_f_w2, writes=[bw])
        P.dma("sp", w3[:], K.hy_f_w3, writes=[bw])
        P.dma("sp", w4[:], K.hy_f_w4, writes=[bw])
        P.dma("sp", pv[:, 0:4], K.hy_fpv_l, writes=[bw])
        P.dma("sp", dl[:], K.hy_ndelta_l, writes=[bw])
        P.op("dve", lambda e: e.tensor_tensor(out=pv[:, 4:7], in0=pv[:, 1:4], in1=pv[:, 0:1].to_broadcast([64, 3]), op=ALU.mult),
             reads=[bw], writes=[bw])
        ws = [w1, w2, w3]
        for pc in range(L // 512):
            zp, zpb = zpr.next()
            tp, tpb = tpr.next()
            P.dma("sp", zp[:], K.hy_zpos[:, pc * 512:(pc + 1) * 512], writes=[zpb])
            P.dma("sp", tp[:], K.hy_tpos[:, pc * 512:(pc + 1) * 512], writes=[tpb])
            cur, curb, kdim = zp, zpb, 33
            for li in range(3):
                ps, pb = psr.next()
                P.op("pe", lambda e, ps=ps, cur=cur, li=li, kdim=kdim: e.matmul(
                    ps[0:64, :], lhsT=ws[li][0:kdim, :], rhs=cur[0:kdim, :], start=True, stop=True),
                    reads=[bw, curb], writes=[pb])
                h, hb = hr.next()
                P.op("dve", lambda e, ps=ps, h=h, li=li: e.tensor_scalar(
                    out=h[:], in0=ps[0:64, :], scalar1=pv[:, 0:1], scalar2=pv[:, 4 + li:5 + li], op0=ALU.mult, op1=ALU.add),
                    reads=[pb, bw], writes=[hb])
                _wrap_pi(P, h, hb, tmpa, btm, 64, 512)
                P.op("act", lambda e, h=h: e.activation(out=h[:], in_=h[:], func=AF.Sin), reads=[hb], writes=[hb])
                cur, curb, kdim = h, hb, 64
            dc, dcb = dcr.next()
            for cc in range(8):
                P.op("act", lambda e, dc=dc, tp=tp, cc=cc: e.activation(out=dc[:, cc, :], in_=tp[:], func=AF.Exp,
                                                                        scale=dl[:, cc:cc + 1]), reads=[tpb, bw], writes=[dcb])
            for o in range(2):
                for d_ in range(2):
                    for cc in range(8):
                        col = o * 2048 + d_ * 1024 + cc * 128
                        ps, pb = psr.next()
                        P.op("pe", lambda e, ps=ps, cur=cur, col=col: e.matmul(
                            ps[:, :], lhsT=w4[:, col:col + 128], rhs=cur[:, :], start=True, stop=True),
                            reads=[bw, curb], writes=[pb])
                        f_, fb = fr_.next()
                        P.op("dve", lambda e, ps=ps, f_=f_, dc=dc, cc=cc: e.tensor_tensor(
                            out=f_[:], in0=ps[:, :], in1=dc[:, cc, :], op=ALU.mult), reads=[pb, dcb], writes=[fb])
                        P.dma("sp", K.filtT[o, d_, cc, :, pc * 512:(pc + 1) * 512], f_[:], reads=[fb])
        P.phase_end()


def phase_hyconv_direct(K):
    nc, P = K.nc, K.P
    with contextlib.ExitStack() as st:
        u = sb(st, nc, "hc_u", [128, L], F32)
        x = sb(st, nc, "hc_x", [128, L], F32)
        hf = sb(st, nc, "hc_hf", [128, L], F32)
        hb_ = sb(st, nc, "hc_hb", [128, L], F32)
        acc = sb(st, nc, "hc_acc", [128, L], F32)
        zo = sb(st, nc, "hc_zo", [128, L], BF16)
        sk = sb(st, nc, "hc_sk", [128, 2, 8], F32)
        s0 = sb(st, nc, "hc_s0", [128, 1], F32)
        bu, bx, bhf, bhb, bacc, bzo, bsk, bs0 = [Buf() for _ in range(8)]
        P.dma("sp", sk[:], K.hy_skip_l, writes=[bsk])
        nl = getattr(K, 'hy_nlags', L)
        for cc in range(getattr(K, 'hy_chunks', 8)):
            P.dma("sp", u[:], K.zT[cc], writes=[bu])
            for o in range(2):
                P.dma("sp", x[:], K.zT[8 * (o + 1) + cc], writes=[bx])
                P.dma("sp", hf[:], K.filtT[o, 0, cc], writes=[bhf])
                P.dma("sp", hb_[:], K.filtT[o, 1, cc], writes=[bhb])
                P.op("dve", lambda e, o=o, cc=cc: e.tensor_tensor(out=s0[:], in0=hf[:, 0:1], in1=sk[:, o, cc:cc + 1], op=ALU.add),
                     reads=[bhf, bsk], writes=[bs0])
                P.op("dve", lambda e: e.tensor_scalar(out=acc[:], in0=u[:], scalar1=s0[:, 0:1], scalar2=None, op0=ALU.mult),
                     reads=[bu, bs0], writes=[bacc])
                for lag in range(1, nl):
                    P.op("dve", lambda e, lag=lag: e.scalar_tensor_tensor(
                        out=acc[:, lag:L], in0=u[:, 0:L - lag], scalar=hf[:, lag:lag + 1], in1=acc[:, lag:L],
                        op0=ALU.mult, op1=ALU.add), reads=[bu, bhf], writes=[bacc])
                    P.op("dve", lambda e, lag=lag: e.scalar_tensor_tensor(
                        out=acc[:, 0:L - lag], in0=u[:, lag:L], scalar=hb_[:, lag:lag + 1], in1=acc[:, 0:L - lag],
                        op0=ALU.mult, op1=ALU.add), reads=[bu, bhb], writes=[bacc])
                if o == 0:
                    P.op("dve", lambda e: e.tensor_tensor(out=u[:], in0=acc[:], in1=x[:], op=ALU.mult),
                         reads=[bacc, bx], writes=[bu])
                else:
                    P.op("dve", lambda e: e.tensor_tensor(out=zo[:], in0=acc[:], in1=x[:], op=ALU.mult),
                         reads=[bacc, bx], writes=[bzo])
                    P.dma("sp", K.z2T[cc], zo[:], reads=[bzo])
        P.phase_end()


def phase_final(K):
    nc, P = K.nc, K.P
    with contextlib.ExitStack() as st:
        g = sb(st, nc, "fn_g", [128, 8], F32)
        hin = Rot([sb(st, nc, f"fn_in{i}", [128, 8, 512], F32) for i in range(2)])
        sq = Rot([sb(st, nc, f"fn_sq{i}", [128, 8, 512], BF16) for i in range(2)])
        sd = Rot([sb(st, nc, f"fn_sd{i}", [128, 512], F32) for i in range(2)])
        hn = Rot([sb(st, nc, f"fn_hn{i}", [128, 8, 512], F32) for i in range(2)])
        outr = Rot([sb(st, nc, f"fn_o{i}", [128, 4, 1024], F32) for i in range(2)])
        psr = ps_banks(st, nc, 2, "fnps")
        tpr = ps_banks(st, nc, 4, "fntp")
        bg = Buf()
        P.dma("sp", g[:], K.finalg_l, writes=[bg])
        n = 0
        for grp in range(L // 512):
            h, hb = hin.next()
            P.dma("sp", h[:], K.hT[:, :, grp * 512:(grp + 1) * 512].rearrange("c p t -> p c t"), writes=[hb])
            q, qb = sq.next()
            P.op("act", lambda e, q=q, h=h: e.activation(out=q[:], in_=h[:], func=AF.Square), reads=[hb], writes=[qb])
            ps, pb = psr.next()
            for dc in range(8):
                P.op("pe", lambda e, ps=ps, q=q, dc=dc: e.matmul(ps[:, :], lhsT=K.onesb[:], rhs=q[:, dc, :],
                                                               start=(dc == 0), stop=(dc == 7)), reads=[qb], writes=[pb])
            d_, db = sd.next()
            P.op("act", lambda e, d_=d_, ps=ps: e.activation(out=d_[:], in_=ps[:, :], func=AF.Sqrt, bias=K.epsc[:, 0:1],
                                                            scale=1.0 / D), reads=[pb], writes=[db])
            P.op("dve", lambda e, d_=d_: e.reciprocal(out=d_[:], in_=d_[:]), reads=[db], writes=[db])
            hn_, hnb = hn.next()
            for dc in range(8):
                P.op("dve", lambda e, hn_=hn_, h=h, d_=d_, dc=dc: e.scalar_tensor_tensor(
                    out=hn_[:, dc, :], in0=h[:, dc, :], scalar=g[:, dc:dc + 1], in1=d_[:], op0=ALU.mult, op1=ALU.mult),
                    reads=[hb, db, bg], writes=[hnb])
            ot, otb = outr.next()
            for a in range(4):
                for half in range(2):
                    tp, tpb = tpr.next()
                    for j in range(4):
                        dc = half * 4 + j
                        P.op("pe", lambda e, tp=tp, hn_=hn_, a=a, j=j, dc=dc: e.transpose(
                            out=tp[:, j * 128:(j + 1) * 128], in_=hn_[:, dc, a * 128:(a + 1) * 128], identity=K.ident[:]),
                            reads=[hnb], writes=[tpb])
                    if n % 2 == 0:
                        P.op("act", lambda e, tp=tp, ot=ot, a=a, half=half: e.activation(
                            out=ot[:, a, half * 512:(half + 1) * 512], in_=tp[:, :], func=AF.Copy), reads=[tpb], writes=[otb])
                    else:
                        P.op("dve", lambda e, tp=tp, ot=ot, a=a, half=half: e.tensor_copy(
                            out=ot[:, a, half * 512:(half + 1) * 512], in_=tp[:, :]), reads=[tpb], writes=[otb])
                    n += 1
            P.dma("sp", K.out[grp * 512:(grp + 1) * 512, :].rearrange("(a p) d -> p a d", p=128), ot[:], reads=[otb])
        P.phase_end()


def fft_consts():
    a = np.arange(128, dtype=np.float64)
    th = 2 * np.pi * np.outer(a, a) / 128.0
    C = np.cos(th)
    S = np.sin(th)
    tw = 2 * np.pi * np.outer(a, a) / 16384.0
    cw, sw = np.cos(tw), np.sin(tw)
    mats = np.stack([C, S, -S, -C], axis=1)
    wide = np.stack([np.concatenate([C, -S], 1), np.concatenate([C, S], 1), np.concatenate([-S, C], 1)], axis=1)
    twt = np.stack([np.concatenate([cw, cw], 1), np.concatenate([sw, -sw], 1)], axis=1)
    return {"fft_mats": mats.astype(np.float32), "fft_wide": wide.astype(np.float32), "fft_tw": twt.astype(np.float32)}


def phase_hyconv_fft(K):
    nc, P = K.nc, K.P
    INV_N = 1.0 / 16384.0
    with contextlib.ExitStack() as st:
        mats = sb(st, nc, "hx_mats", [128, 4, 128], BF16)
        wide = sb(st, nc, "hx_wide", [128, 3, 256], BF16)
        tw = sb(st, nc, "hx_tw", [128, 2, 256], F32)
        sk = sb(st, nc, "hx_sk", [64, 2, 16], F32)
        FMu = sb(st, nc, "hx_FMu", [64, L], F32)
        FMa = sb(st, nc, "hx_FMa", [64, L], F32)
        FMb = sb(st, nc, "hx_FMb", [64, L], F32)
        Tu = sb(st, nc, "hx_Tu", [64, 128, 64], BF16)
        Tf = sb(st, nc, "hx_Tf", [64, 128, 64], BF16)
        Tb = sb(st, nc, "hx_Tb", [64, 128, 64], BF16)
        Ytok = sb(st, nc, "hx_Ytok", [64, 64, 128], F32)
        tAr = Rot([sb(st, nc, f"hx_tA{i}", [128, 4, 256], F32) for i in range(1)])
        tBr = Rot([sb(st, nc, f"hx_tB{i}", [128, 4, 256], F32) for i in range(1)])
        Zr_ = Rot([sb(st, nc, f"hx_Z{i}", [128, 4, 256], BF16) for i in range(2)])
        Hr_ = Rot([sb(st, nc, f"hx_H{i}", [128, 2, 512], F32) for i in range(1)])
        prr = Rot([sb(st, nc, f"hx_pr{i}", [128, 512], F32) for i in range(3)])
        Pbr = Rot([sb(st, nc, f"hx_Pb{i}", [128, 2, 512], BF16) for i in range(1)])
        psA = ps_banks(st, nc, 4, "hxA")
        psB = ps_banks(st, nc, 3, "hxB")
        psC = ps_banks(st, nc, 1, "hxC")
        bC, bu, ba, bb, bTu, bTf, bTb, bY, bzo = [Buf() for _ in range(9)]
        P.dma("pool", mats[:], K.fft_mats, writes=[bC])
        P.dma("pool", wide[:], K.fft_wide, writes=[bC])
        P.dma("sp", tw[:], K.fft_tw, writes=[bC])
        P.dma("sp", sk[:], K.hy_skip_h, writes=[bC])
        Cm, Sm, NSm, NCm = mats[:, 0, :], mats[:, 1, :], mats[:, 2, :], mats[:, 3, :]
        F1, IFA, IFB = wide[0:64, 0, :], wide[:, 1, :], wide[:, 2, :]
        twA = tw[:, 0, :].unsqueeze(1).to_broadcast([128, 4, 256])
        swb = tw[:, 1, 0:128].unsqueeze(1).to_broadcast([128, 4, 128])
        nswb = tw[:, 1, 128:256].unsqueeze(1).to_broadcast([128, 4, 128])
        evn = [0]

        def to_tok(src, sbuf_, dst, dbuf):
            srcv = src[:].rearrange("c (n1 n2) -> c n2 n1", n2=128)
            for g8 in range(16):
                ps, pb = psA.next()
                for j in range(8):
                    n2 = g8 * 8 + j
                    P.op("pe", lambda e, ps=ps, j=j, n2=n2: e.transpose(
                        out=ps[0:64, j * 64:(j + 1) * 64], in_=srcv[:, n2, :], identity=K.ident[0:64, 0:64]),
                        reads=[sbuf_], writes=[pb])
                evn[0] += 1
                o_ = dst[:, g8 * 8:(g8 + 1) * 8, :]
                i_ = ps[0:64, :].rearrange("p (j c) -> p j c", c=64)
                if evn[0] % 2 == 0:
                    P.op("act", lambda e, o_=o_, i_=i_: e.activation(out=o_, in_=i_, func=AF.Copy), reads=[pb], writes=[dbuf])
                else:
                    P.op("dve", lambda e, o_=o_, i_=i_: e.tensor_copy(out=o_, in_=i_), reads=[pb], writes=[dbuf])

        def twiddle(ps2, pbufs, conj):
            tA, tAb = tAr.next()
            tB, tBb = tBr.next()
            Z, Zb = Zr_.next()
            for half in range(2):
                ps = ps2[half]
                yv = ps[:, :].rearrange("p (c w) -> p c w", w=256)
                sl = slice(half * 2, half * 2 + 2)
                P.op("dve", lambda e, yv=yv, sl=sl: e.tensor_tensor(out=tA[:, sl, :], in0=yv, in1=twA[:, 0:2, :], op=ALU.mult),
                     reads=[pbufs[half], bC], writes=[tAb])
                s1, s2 = (swb, nswb) if not conj else (nswb, swb)
                P.op("dve", lambda e, yv=yv, sl=sl, s1=s1: e.tensor_tensor(out=tB[:, sl, 0:128], in0=yv[:, :, 128:256],
                                                                        in1=s1[:, 0:2, :], op=ALU.mult),
                     reads=[pbufs[half], bC], writes=[tBb])
                P.op("dve", lambda e, yv=yv, sl=sl, s2=s2: e.tensor_tensor(out=tB[:, sl, 128:256], in0=yv[:, :, 0:128],
                                                                        in1=s2[:, 0:2, :], op=ALU.mult),
                     reads=[pbufs[half], bC], writes=[tBb])
            P.op("pool", lambda e: e.tensor_tensor(out=Z[:], in0=tA[:], in1=tB[:], op=ALU.add), reads=[tAb, tBb], writes=[Zb])
            return Z, Zb

        def fwd(T, Tbuf, g4):
            ps2, pbufs = [], []
            for half in range(2):
                ps, pb = psA.next()
                for j in range(2):
                    c = g4 * 4 + half * 2 + j
                    P.op("pe", lambda e, ps=ps, j=j, c=c: e.matmul(ps[:, j * 256:(j + 1) * 256], lhsT=T[:, :, c], rhs=F1,
                                                                   start=True, stop=True), reads=[Tbuf, bC], writes=[pb])
                ps2.append(ps)
                pbufs.append(pb)
            return twiddle(ps2, pbufs, False)

        def stage3(terms):
            xr, xrb = psB.next()
            xi, xib = psB.next()
            n = len(terms)
            for t, (Z, Zb, sg) in enumerate(terms):
                Zv = Z[:].rearrange("p c (two k) -> p c two k", two=2)
                P.op("pe", lambda e, Zv=Zv, t=t: e.matmul(xr[:, :], lhsT=Cm, rhs=Zv[:, :, 0, :], start=(t == 0), stop=False),
                     reads=[Zb, bC], writes=[xrb])
                P.op("pe", lambda e, Zv=Zv, t=t: e.matmul(xr[:, :], lhsT=Sm, rhs=Zv[:, :, 1, :], start=False, stop=(t == n - 1)),
                     reads=[Zb, bC], writes=[xrb])
            for t, (Z, Zb, sg) in enumerate(terms):
                Zv = Z[:].rearrange("p c (two k) -> p c two k", two=2)
                m1, m2 = (Cm, NSm) if sg > 0 else (NCm, Sm)
                P.op("pe", lambda e, Zv=Zv, t=t, m1=m1: e.matmul(xi[:, :], lhsT=m1, rhs=Zv[:, :, 1, :], start=(t == 0), stop=False),
                     reads=[Zb, bC], writes=[xib])
                P.op("pe", lambda e, Zv=Zv, t=t, m2=m2: e.matmul(xi[:, :], lhsT=m2, rhs=Zv[:, :, 0, :], start=False, stop=(t == n - 1)),
                     reads=[Zb, bC], writes=[xib])
            return (xr, xrb), (xi, xib)

        for hc in range(getattr(K, 'hy_halfchunks', 16)):
            cc, p0 = hc // 2, (hc % 2) * 64
            P.dma("sp", FMu[:], K.zT[cc, p0:p0 + 64, :], writes=[bu])
            for o in range(2):
                P.dma("sp", FMa[:], K.filtT[o, 0, cc, p0:p0 + 64, :], writes=[ba])
                P.dma("sp", FMb[:], K.filtT[o, 1, cc, p0:p0 + 64, :], writes=[bb])
                P.op("pool", lambda e: e.memset(FMb[:, 0:1], 0.0), writes=[bb])
                to_tok(FMa, ba, Tf, bTf)
                to_tok(FMb, bb, Tb, bTb)
                to_tok(FMu, bu, Tu, bTu)
                P.dma("sp", FMa[:], K.zT[8 * (o + 1) + cc, p0:p0 + 64, :], reads=[bTf], writes=[ba])
                for g4 in range(16):
                    Zf, Zfb = fwd(Tf, bTf, g4)
                    Zb_, Zbb = fwd(Tb, bTb, g4)
                    (hr, hrb), (hi, hib) = stage3([(Zf, Zfb, 1), (Zb_, Zbb, -1)])
                    H, Hb = Hr_.next()
                    P.op("act", lambda e, H=H, hr=hr: e.activation(out=H[:, 0, :], in_=hr[:, :], func=AF.Copy), reads=[hrb], writes=[Hb])
                    P.op("act", lambda e, H=H, hi=hi: e.activation(out=H[:, 1, :], in_=hi[:, :], func=AF.Copy), reads=[hib], writes=[Hb])
                    Zu, Zub = fwd(Tu, bTu, g4)
                    (xr, xrb), (xi, xib) = stage3([(Zu, Zub, 1)])
                    Pb, Pbb = Pbr.next()
                    for (oi, a0, h0, a1, h1, op_) in ((0, 0, 0, 1, 1, ALU.subtract), (1, 0, 1, 1, 0, ALU.add)):
                        t1, t1b = prr.next()
                        t2, t2b = prr.next()
                        P.op("dve", lambda e, t1=t1, xr=xr, H=H, h0=h0: e.tensor_tensor(out=t1[:], in0=xr[:, :], in1=H[:, h0, :], op=ALU.mult),
                             reads=[xrb, Hb], writes=[t1b])
                        P.op("dve", lambda e, t2=t2, xi=xi, H=H, h1=h1: e.tensor_tensor(out=t2[:], in0=xi[:, :], in1=H[:, h1, :], op=ALU.mult),
                             reads=[xib, Hb], writes=[t2b])
                        P.op("pool", lambda e, t1=t1, t2=t2, Pb=Pb, oi=oi, op_=op_: e.tensor_tensor(out=Pb[:, oi, :], in0=t1[:], in1=t2[:], op=op_),
                             reads=[t1b, t2b], writes=[Pbb])
                    q2, qb2 = [], []
                    for half in range(2):
                        ps, pb = psA.next()
                        for j in range(2):
                            c4 = half * 2 + j
                            P.op("pe", lambda e, ps=ps, j=j, c4=c4, Pb=Pb: e.matmul(
                                ps[:, j * 256:(j + 1) * 256], lhsT=Pb[:, 0, c4 * 128:(c4 + 1) * 128], rhs=IFA, start=True, stop=False),
                                reads=[Pbb, bC], writes=[pb])
                            P.op("pe", lambda e, ps=ps, j=j, c4=c4, Pb=Pb: e.matmul(
                                ps[:, j * 256:(j + 1) * 256], lhsT=Pb[:, 1, c4 * 128:(c4 + 1) * 128], rhs=IFB, start=False, stop=True),
                                reads=[Pbb, bC], writes=[pb])
                        q2.append(ps)
                        qb2.append(pb)
                    R, Rb = twiddle(q2, qb2, True)
                    Rv = R[:].rearrange("p c (two k) -> p c two k", two=2)
                    yp, ypb = psC.next()
                    P.op("pe", lambda e, Rv=Rv: e.matmul(yp[0:64, :], lhsT=Cm[:, 0:64], rhs=Rv[:, :, 0, :], start=True, stop=False),
                         reads=[Rb, bC], writes=[ypb])
                    P.op("pe", lambda e, Rv=Rv: e.matmul(yp[0:64, :], lhsT=NSm[:, 0:64], rhs=Rv[:, :, 1, :], start=False, stop=True),
                         reads=[Rb, bC], writes=[ypb])
                    P.op("act", lambda e, g4=g4: e.activation(
                        out=Ytok[:, g4 * 4:(g4 + 1) * 4, :], in_=yp[0:64, :].rearrange("p (c n) -> p c n", n=128),
                        func=AF.Copy, scale=INV_N), reads=[ypb], writes=[bY])
                fmv = FMb[:].rearrange("c (n1 n2) -> c n2 n1", n2=128)
                for g8 in range(16):
                    ps, pb = psA.next()
                    for j in range(8):
                        n2 = g8 * 8 + j
                        P.op("pe", lambda e, ps=ps, j=j, n2=n2: e.transpose(
                            out=ps[0:64, j * 64:(j + 1) * 64], in_=Ytok[:, :, n2], identity=K.ident[0:64, 0:64]),
                            reads=[bY], writes=[pb])
                    o_ = fmv[:, g8 * 8:(g8 + 1) * 8, :]
                    i_ = ps[0:64, :].rearrange("p (j n) -> p j n", n=64)
                    P.op("act", lambda e, o_=o_, i_=i_: e.activation(out=o_, in_=i_, func=AF.Copy), reads=[pb], writes=[bb])
                P.op("dve", lambda e, o=o, hc=hc: e.scalar_tensor_tensor(
                    out=FMb[:], in0=FMu[:], scalar=sk[:, o, hc:hc + 1], in1=FMb[:], op0=ALU.mult, op1=ALU.add),
                    reads=[bu, bC], writes=[bb])
                if o == 0:
                    P.op("dve", lambda e: e.tensor_tensor(out=FMu[:], in0=FMb[:], in1=FMa[:], op=ALU.mult),
                         reads=[bb, ba, bTu], writes=[bu])
                else:
                    P.op("dve", lambda e: e.tensor_tensor(out=FMb[:], in0=FMb[:], in1=FMa[:], op=ALU.mult),
                         reads=[bb, ba], writes=[bb])
                    for c0 in range(0, L, 2048):
                        P.dma("pool", K.z2T[cc, p0:p0 + 64, c0:c0 + 2048], FMb[:, c0:c0 + 2048], reads=[bb])
        P.phase_end()


STAGES = ["xt", "mod", "norm0", "dnin", "dnscan", "dnout", "peer0", "hyena", "peer1", "final"]
_CACHE = {}


def kernel(**inputs):
    inputs = {k: np.asarray(v) for k, v in inputs.items()}
    if "nc" not in _CACHE:
        _CACHE["nc"] = build(STAGES)[0]
    nc = _CACHE["nc"]
    shared = host_inputs(inputs, 0)
    in_maps = []
    for b in range(8):
        m = dict(shared)
        m["x"] = np.ascontiguousarray(inputs["x"][b])
        m["ctx"] = np.ascontiguousarray(inputs["ctx"][b])
        m["c_l"] = np.ascontiguousarray(inputs["c"][b].reshape(8, 128).T)
        in_maps.append(m)
    res = run_bass_kernel_spmd(nc, in_maps, core_ids=list(range(8)))
    out = np.stack([np.asarray(res.results[b]["out"]) for b in range(8)], axis=0)
    return out.astype(np.float32)
```

```python
import contextlib
import math
import numpy as np
import ml_dtypes
import concourse.bass as bass
import concourse.mybir as mybir
from concourse.bass_utils import run_bass_kernel_spmd

F32 = mybir.dt.float32
BF16 = mybir.dt.bfloat16
I32 = mybir.dt.int32
U32 = mybir.dt.uint32
AF = mybir.ActivationFunctionType
ALU = mybir.AluOpType
AX = mybir.AxisListType

N_DMA_SEMS = 12
L = 8192
D = 1024
CTX = 256
TT = L + CTX
EPS = 1e-6


class Buf:
    __slots__ = ("name", "w", "r", "psum", "acc")

    def __init__(self, name="", psum=False):
        self.name = name
        self.w = None
        self.r = []
        self.psum = psum
        self.acc = {}


class Op:
    __slots__ = ("eng", "fn", "deps", "is_dma", "signal", "val", "sem", "idx")


class Prog:
    ENGS = ("pe", "dve", "act", "pool", "sp")
    ENGOBJ = {"pe": "tensor", "dve": "vector", "act": "scalar", "pool": "gpsimd", "sp": "sync"}

    def __init__(self, nc, stack):
        self.nc = nc
        self.nops = 0
        self.cur = []
        self.sems = {e: stack.enter_context(nc.semaphore(f"s_{e}")) for e in ("pe", "dve", "act", "pool")}
        self.dma_sems = {e: [stack.enter_context(nc.semaphore(f"d_{e}_{i}")) for i in range(N_DMA_SEMS)]
                         for e in ("sp", "act", "pool")}
        self.cnt = {e: 0 for e in self.ENGS}
        self.dma_k = {e: 0 for e in self.ENGS}
        self.dma_vals = {e: [0] * N_DMA_SEMS for e in self.ENGS}
        self.seen = {e: {} for e in self.ENGS}
        self.last_op = {e: None for e in self.ENGS}
        self.prev_dmas = []
        self.pending = {}
        self.n_waits = 0

    def op(self, eng, fn, reads=(), writes=(), is_dma=False):
        o = Op()
        o.eng = eng
        o.fn = fn
        o.is_dma = is_dma
        o.signal = False
        o.val = None
        o.sem = None
        o.idx = self.nops
        self.nops += 1
        deps = set()
        for b in reads:
            if b.w is not None and b.w.fn is not None:
                deps.add(b.w)
        for b in writes:
            if b.w is not None and b.w.fn is not None:
                deps.add(b.w)
            for r in b.r:
                if r.fn is not None:
                    deps.add(r)
        for b in tuple(reads) + tuple(writes):
            if b.psum:
                for x, o2 in b.acc.items():
                    if x != eng and o2.fn is not None:
                        deps.add(o2)
                b.acc[eng] = o
        if eng in self.pending:
            deps |= self.pending.pop(eng)
        o.deps = deps
        for b in reads:
            b.r.append(o)
        for b in writes:
            b.w = o
            b.r = []
        self.cur.append(o)
        return o

    def dma(self, eng, out, in_, reads=(), writes=(), **kw):
        return self.op(eng, lambda e: e.dma_start(out=out, in_=in_, **kw), reads, writes, is_dma=True)

    def phase_end(self):
        nc = self.nc
        by_eng = {e: [] for e in self.ENGS}
        for o in self.cur:
            by_eng[o.eng].append(o)
        for o in self.cur:
            best = {}
            for d in o.deps:
                if d.is_dma or (d.eng == o.eng and o.eng == "pe"):
                    continue
                if d.eng not in best or d.idx > best[d.eng].idx:
                    best[d.eng] = d
            for d in best.values():
                if d.val is None:
                    d.signal = True
        for e in self.ENGS:
            for o in reversed(by_eng[e]):
                if not o.is_dma:
                    o.signal = True
                    break
        for e in self.ENGS:
            for o in by_eng[e]:
                if o.is_dma:
                    s = self.dma_k[e] % N_DMA_SEMS
                    self.dma_k[e] += 1
                    self.dma_vals[e][s] += 16
                    o.sem = (e, s)
                    o.val = self.dma_vals[e][s]
                elif o.signal:
                    self.cnt[e] += 1
                    o.val = self.cnt[e]

        def emit_engine(e, eng):
            seen = self.seen[e]

            def wait(key, sem, val):
                if seen.get(key, 0) >= val:
                    return
                eng.wait_ge(sem, val)
                self.n_waits += 1
                seen[key] = val

            for o in by_eng[e]:
                best = {}
                for d in o.deps:
                    if d.is_dma:
                        wait(("dma",) + d.sem, self.dma_sems[d.sem[0]][d.sem[1]], d.val)
                    else:
                        if d.eng == e and e == "pe":
                            continue
                        if d.eng not in best or d.idx > best[d.eng].idx:
                            best[d.eng] = d
                for x, d in best.items():
                    wait(x, self.sems[x], d.val)
                if o.is_dma:
                    if o.val > 16:
                        wait(("dma",) + o.sem, self.dma_sems[e][o.sem[1]], o.val - 16)
                    ins = o.fn(eng)
                    ins.then_inc(self.dma_sems[e][o.sem[1]], 16)
                else:
                    ins = o.fn(eng)
                    if o.signal:
                        ins.then_inc(self.sems[e], 1)
                o.fn = None

        with nc.Block() as block:
            for e in self.ENGS:
                if not by_eng[e]:
                    continue
                dec = getattr(block, self.ENGOBJ[e])

                def mk(e):
                    def _f(eng):
                        emit_engine(e, eng)
                    return _f
                dec(mk(e))
        for e in self.ENGS:
            for o in reversed(by_eng[e]):
                if not o.is_dma:
                    self.last_op[e] = o
                    break
        dmas = [o for o in self.cur if o.is_dma]
        bar = set(dmas)
        for e in self.ENGS:
            if self.last_op[e] is not None:
                bar.add(self.last_op[e])
        for e in self.ENGS:
            self.pending[e] = self.pending.get(e, set()) | bar
        for o in self.cur:
            o.deps = None
        self.cur = []

    def finish(self):
        nc = self.nc
        with nc.Block() as block:
            for e in ("sp", "act", "pool"):
                if self.dma_k[e] == 0:
                    continue
                dec = getattr(block, self.ENGOBJ[e])

                def mk(e):
                    def _f(eng):
                        for s in range(N_DMA_SEMS):
                            v = self.dma_vals[e][s]
                            if v > 0:
                                eng.wait_ge(self.dma_sems[e][s], v)
                    return _f
                dec(mk(e))


class Rot:
    def __init__(self, tiles):
        self.tiles = tiles
        self.bufs = [Buf() for _ in tiles]
        self.i = 0

    def next(self):
        t, b = self.tiles[self.i], self.bufs[self.i]
        self.i = (self.i + 1) % len(self.tiles)
        return t, b


class Ctx:
    pass


_UNIQ = [0]


def sb(st, nc, name, shape, dt):
    _UNIQ[0] += 1
    return st.enter_context(nc.sbuf_tensor(f"{name}_{_UNIQ[0]}", list(shape), dt))


def ps_banks(st, nc, n, prefix):
    _UNIQ[0] += 1
    r = Rot([st.enter_context(nc.psum_tensor(f"{prefix}{i}_{_UNIQ[0]}", [128, 512], F32)) for i in range(n)])
    for b in r.bufs:
        b.psum = True
    return r


def host_consts():
    c = {}
    c["ident"] = np.eye(128, dtype=np.float32)
    c["ones"] = np.ones((128, 128), dtype=np.float32)
    return c


def phase_xt(K):
    nc, P = K.nc, K.P
    with contextlib.ExitStack() as st:
        xin = Rot([sb(st, nc, f"xt_in{i}", [128, 4, D], F32) for i in range(2)])
        hout = Rot([sb(st, nc, f"xt_out{i}", [128, 8, 512], F32) for i in range(2)])
        psr = ps_banks(st, nc, 4, "xtps")
        jobs = [(K.x[g * 512:(g + 1) * 512, :], K.hT[:, :, g * 512:(g + 1) * 512], 4) for g in range(L // 512)]
        jobs.append((K.ctx[:, :], K.ctxT[:, :, :], 2))
        n = 0
        for src, dst, na in jobs:
            xt, xb = xin.next()
            ho, hb = hout.next()
            P.dma("sp", xt[:, 0:na, :], src.rearrange("(a p) d -> p a d", p=128), writes=[xb])
            for dc in range(8):
                ps, pb = psr.next()
                for a in range(na):
                    P.op("pe", lambda e, ps=ps, xt=xt, a=a, dc=dc: e.transpose(
                        out=ps[:, a * 128:(a + 1) * 128], in_=xt[:, a, dc * 128:(dc + 1) * 128], identity=K.ident[:]),
                        reads=[xb], writes=[pb])
                if n % 2 == 0:
                    P.op("act", lambda e, ps=ps, ho=ho, dc=dc, na=na: e.activation(
                        out=ho[:, dc, 0:na * 128], in_=ps[:, 0:na * 128], func=AF.Copy), reads=[pb], writes=[hb])
                else:
                    P.op("dve", lambda e, ps=ps, ho=ho, dc=dc, na=na: e.tensor_copy(
                        out=ho[:, dc, 0:na * 128], in_=ps[:, 0:na * 128]), reads=[pb], writes=[hb])
                n += 1
            P.dma("pool", dst.rearrange("c p t -> p c t"), ho[:, :, 0:na * 128], reads=[hb])
        P.phase_end()


def phase_mod(K):
    nc, P = K.nc, K.P
    with contextlib.ExitStack() as st:
        cin = sb(st, nc, "mod_cin", [128, 2, 8], F32)
        sc2 = sb(st, nc, "mod_sc2", [128, 8, 2], F32)
        ab = sb(st, nc, "mod_ab", [128, 2, 48], F32)
        wr = Rot([sb(st, nc, f"mod_w{i}", [128, 8, 1536], F32) for i in range(2)])
        psr = ps_banks(st, nc, 2, "modps")
        bc, bs, bab = Buf(), Buf(), Buf()
        P.dma("sp", cin[:, 0, :], K.c_l, writes=[bc])
        P.dma("sp", cin[:, 1, :], K.cc_l, writes=[bc])
        P.dma("sp", ab[:], K.adab_l, writes=[bab])
        P.op("act", lambda e: e.activation(out=sc2[:].rearrange("p k t -> p t k"), in_=cin[:], func=AF.Silu), reads=[bc], writes=[bs])
        for l in range(2):
            for nb in range(4):
                w, wb = wr.next()
                P.dma("sp", w[:], K.ada_w[l, :, nb * 1536:(nb + 1) * 1536].rearrange("(kc p) n -> p kc n", p=128),
                      writes=[wb])
                ps, pb = psr.next()
                for j in range(12):
                    for kc in range(8):
                        P.op("pe", lambda e, ps=ps, w=w, j=j, kc=kc: e.matmul(
                            ps[:, j * 2:(j + 1) * 2], lhsT=w[:, kc, j * 128:(j + 1) * 128], rhs=sc2[:, kc, :],
                            start=(kc == 0), stop=(kc == 7)), reads=[wb, bs], writes=[pb])
                P.op("dve", lambda e, ps=ps, l=l, nb=nb: e.tensor_tensor(
                    out=K.MOD[:, l, nb * 12:(nb + 1) * 12, :],
                    in0=ps[:, 0:24].rearrange("p (j t) -> p j t", t=2),
                    in1=ab[:, l, nb * 12:(nb + 1) * 12].unsqueeze(2).to_broadcast([128, 12, 2]),
                    op=ALU.add), reads=[pb, bab], writes=[K.bMOD])
        P.phase_end()


def phase_norm(K, l, s, src, dst, groups, col, gname):
    nc, P = K.nc, K.P
    with contextlib.ExitStack() as st:
        A = sb(st, nc, "nm_A", [128, 8], F32)
        g = sb(st, nc, "nm_g", [128, 8], F32)
        hin = Rot([sb(st, nc, f"nm_in{i}", [128, 8, 512], F32) for i in range(2)])
        sq = Rot([sb(st, nc, f"nm_sq{i}", [128, 8, 512], BF16) for i in range(2)])
        sd = Rot([sb(st, nc, f"nm_sd{i}", [128, 512], F32) for i in range(2)])
        tmp = Rot([sb(st, nc, f"nm_tmp{i}", [128, 512], F32) for i in range(3)])
        hout = Rot([sb(st, nc, f"nm_out{i}", [128, 8, 512], BF16) for i in range(2)])
        psr = ps_banks(st, nc, 2, "nmps")
        bA, bg = Buf(), Buf()
        P.dma("sp", g[:], gname, writes=[bg])
        msc = K.MOD[:, l, (3 * s + 1) * 8:(3 * s + 2) * 8, col]
        msh = K.MOD[:, l, (3 * s) * 8:(3 * s + 1) * 8, col]
        P.op("dve", lambda e: e.scalar_tensor_tensor(out=A[:], in0=msc, scalar=1.0, in1=g[:], op0=ALU.add, op1=ALU.mult),
             reads=[K.bMOD, bg], writes=[bA])
        for (so, do, Tg) in groups:
            h, hb = hin.next()
            P.dma("sp", h[:, :, 0:Tg], src[:, :, so:so + Tg].rearrange("c p t -> p c t"), writes=[hb])
            q, qb = sq.next()
            P.op("act", lambda e, q=q, h=h, Tg=Tg: e.activation(out=q[:, :, 0:Tg], in_=h[:, :, 0:Tg], func=AF.Square),
                 reads=[hb], writes=[qb])
            ps, pb = psr.next()
            for dc in range(8):
                P.op("pe", lambda e, ps=ps, q=q, dc=dc, Tg=Tg: e.matmul(
                    ps[:, 0:Tg], lhsT=K.onesb[:], rhs=q[:, dc, 0:Tg], start=(dc == 0), stop=(dc == 7)),
                    reads=[qb], writes=[pb])
            d_, db = sd.next()
            P.op("act", lambda e, d_=d_, ps=ps, Tg=Tg: e.activation(
                out=d_[:, 0:Tg], in_=ps[:, 0:Tg], func=AF.Sqrt, bias=K.epsc[:, 0:1], scale=1.0 / D),
                reads=[pb], writes=[db])
            P.op("dve", lambda e, d_=d_, Tg=Tg: e.reciprocal(out=d_[:, 0:Tg], in_=d_[:, 0:Tg]), reads=[db], writes=[db])
            ho, hob = hout.next()
            for dc in range(8):
                t, tb = tmp.next()
                P.op("dve", lambda e, t=t, h=h, d_=d_, dc=dc, Tg=Tg: e.tensor_tensor(
                    out=t[:, 0:Tg], in0=h[:, dc, 0:Tg], in1=d_[:, 0:Tg], op=ALU.mult), reads=[hb, db], writes=[tb])
                P.op("pool", lambda e, t=t, ho=ho, dc=dc, Tg=Tg: e.tensor_scalar(
                    out=ho[:, dc, 0:Tg], in0=t[:, 0:Tg], scalar1=A[:, dc:dc + 1], scalar2=msh[:, dc:dc + 1],
                    op0=ALU.mult, op1=ALU.add), reads=[tb, bA, K.bMOD], writes=[hob])
            P.dma("pool", dst[:, :, do:do + Tg].rearrange("c p t -> p c t"), ho[:, :, 0:Tg], reads=[hob])
        P.phase_end()


WEIGHTS = [
    ("ada_w", [2, D, 6 * D]),
    ("dn_w_in", [D, 4128]), ("dn_w_out", [D, D]),
    ("hy_w_in", [D, 3 * D]), ("hy_w_out", [D, D]),
    ("peer_w_q", [2, D, 2048]), ("peer_v", [2, 16384, D]),
]


def build(stages, debug=(), **kw):
    nc = bass.Bass("TRN2", target_bir_lowering=False)
    K = Ctx()
    for k_, v_ in kw.items():
        setattr(K, k_, v_)
    K.nc = nc
    K.debug = set(debug)

    def din(name, shape, dt=F32):
        return nc.dram_tensor(name, list(shape), dt, kind="ExternalInput").ap()

    def dscr(name, shape, dt=F32):
        if name in getattr(K, 'ext_in', ()):
            return nc.dram_tensor(name, list(shape), dt, kind="ExternalInput").ap()
        kind = {"kind": "ExternalOutput"} if name in K.debug else {}
        return nc.dram_tensor(name, list(shape), dt, **kind).ap()

    K.x = din("x", [L, D])
    K.ctx = din("ctx", [CTX, D])
    K.c_l = din("c_l", [128, 8])
    K.cc_l = din("cc_l", [128, 8])
    K.adab_l = din("adab_l", [128, 2, 48])
    K.normg_l = din("normg_l", [2, 2, 128, 8])
    K.finalg_l = din("finalg_l", [128, 8])
    K.cident = din("cident", [128, 128])
    K.dn_cw_l = din("dn_cw_l", [128, 24, 5])
    K.cscan = din("cscan", [6, 128, 128])
    K.dn_gn_l = din("dn_gn_l", [128, 1024])
    K.peer_u_l = din("peer_u_l", [2, 128, 8, 128, 128])
    K.peer_kT_l = din("peer_kT_l", [2, 128, 16, 128])
    K.hy_cw_l = din("hy_cw_l", [128, 24, 3])
    K.hy_cb_l = din("hy_cb_l", [128, 24])
    K.hy_f_w1 = din("hy_f_w1", [33, 64])
    K.hy_f_w2 = din("hy_f_w2", [64, 64])
    K.hy_f_w3 = din("hy_f_w3", [64, 64])
    K.hy_f_w4 = din("hy_f_w4", [64, 4096])
    K.hy_fpv_l = din("hy_fpv_l", [64, 4])
    K.hy_ndelta_l = din("hy_ndelta_l", [128, 8])
    K.hy_zpos = din("hy_zpos", [33, L])
    K.hy_tpos = din("hy_tpos", [128, L])
    K.hy_skip_l = din("hy_skip_l", [128, 2, 8])
    K.hy_skip_h = din("hy_skip_h", [64, 2, 16])
    K.fft_mats = din("fft_mats", [128, 4, 128])
    K.fft_wide = din("fft_wide", [128, 3, 256])
    K.fft_tw = din("fft_tw", [128, 2, 256])
    K.dn_alog_l = din("dn_alog_l", [128, 16])
    K.dn_dtb_l = din("dn_dtb_l", [128, 16])
    for name, shape in WEIGHTS:
        setattr(K, name, din(name, shape))
    K.out = nc.dram_tensor("out", [L, D], F32, kind="ExternalOutput").ap()
    if "hT" in getattr(K, 'ext_in', ()):
        K.hT_in = nc.dram_tensor("hT", [8, 128, L], F32, kind="ExternalInput").ap()
        K.hT = nc.dram_tensor("hTout", [8, 128, L], F32, kind="ExternalOutput").ap()
    else:
        K.hT = dscr("hT", [8, 128, L])
    K.ctxT = dscr("ctxT", [8, 128, CTX])
    K.hnT = dscr("hnT", [8, 128, TT], BF16)
    K.qT = dscr("qT", [8, 128, TT], BF16)
    K.kT = dscr("kT", [8, 128, TT], BF16)
    K.qkvtok = dscr("qkvtok", [TT, 3072])
    K.sgate = dscr("sgate", [TT, 1024])
    K.ba = dscr("ba", [TT, 32])
    K.o_dir = [dscr("o_f", [L, 1024]), dscr("o_b", [L, 1024])]
    K.ygT = dscr("ygT", [8, 128, L], BF16)
    K.Ub = dscr("Ub", [128, 128, 8, 128], BF16)
    K.zT = dscr("zT", [24, 128, L])
    K.filtT = dscr("filtT", [2, 2, 8, 128, L])
    K.z2T = dscr("z2T", [8, 128, L], BF16)
    K.Vb = dscr("Vb", [128, 128, 1024], BF16)
    if getattr(K, 'scan_dbg', False):
        K.dbgwk = nc.dram_tensor("dbgwk", [8, 128, 13, 128], F32, kind="ExternalOutput").ap()
        K.dbgsm = nc.dram_tensor("dbgsm", [128, 12, 8], F32, kind="ExternalOutput").ap()

    with contextlib.ExitStack() as st:
        P = Prog(nc, st)
        K.P = P
        K.ident = sb(st, nc, "ident", [128, 128], F32)
        K.identb = sb(st, nc, "identb", [128, 128], BF16)
        K.onesb = sb(st, nc, "onesb", [128, 128], BF16)
        K.onesf = sb(st, nc, "onesf", [128, 128], F32)
        K.epsc = sb(st, nc, "epsc", [128, 1], F32)
        K.onec = sb(st, nc, "onec", [128, 1], F32)
        K.MOD = sb(st, nc, "MOD", [128, 2, 48, 2], F32)
        K.bMOD = Buf()
        bi = Buf()
        P.dma("sp", K.ident[:], K.cident, writes=[bi])
        P.op("dve", lambda e: e.tensor_copy(out=K.identb[:], in_=K.ident[:]), reads=[bi], writes=[bi])
        P.op("dve", lambda e: e.memset(K.onesb[:], 1.0), writes=[bi])
        P.op("dve", lambda e: e.memset(K.onesf[:], 1.0), writes=[bi])
        P.op("dve", lambda e: e.memset(K.epsc[:], EPS), writes=[bi])
        P.op("dve", lambda e: e.memset(K.onec[:], 1.0), writes=[bi])
        P.phase_end()
        lat_groups = [(g * 512, CTX + g * 512, 512) for g in range(L // 512)]
        if "hT" in getattr(K, 'ext_in', ()):
            with contextlib.ExitStack() as st2:
                cpr = Rot([sb(st2, nc, f"cp{i}", [128, 8, 512], F32) for i in range(2)])
                for g in range(L // 512):
                    t, tb = cpr.next()
                    P.dma("sp", t[:], K.hT_in[:, :, g * 512:(g + 1) * 512].rearrange("c p t -> p c t"), writes=[tb])
                    P.dma("sp", K.hT[:, :, g * 512:(g + 1) * 512].rearrange("c p t -> p c t"), t[:], reads=[tb])
                P.phase_end()
        if "xt" in stages:
            phase_xt(K)
        if "mod" in stages:
            phase_mod(K)
        if "norm0" in stages:
            phase_norm(K, 0, 0, K.hT, K.hnT, lat_groups, 0, K.normg_l[0, 0])
            phase_norm(K, 0, 0, K.ctxT, K.hnT, [(0, 0, CTX)], 1, K.normg_l[0, 0])
        if "dnin" in stages:
            phase_dnin(K)
        if "dnscan" in stages:
            phase_dnscan(K)
        if "dnout" in stages:
            phase_dnout(K)
            phase_outproj(K, K.ygT, K.dn_w_out, 0, 2)
        if "peer0" in stages:
            phase_norm(K, 0, 1, K.hT, K.hnT, lat_groups, 0, K.normg_l[0, 1])
            phase_peer_tables(K, 0)
            phase_peer(K, 0)
        if "hyena" in stages:
            phase_norm(K, 1, 0, K.hT, K.hnT, lat_groups, 0, K.normg_l[1, 0])
            phase_hyin(K)
            phase_hyfilt(K)
            if getattr(K, 'hy_direct', False):
                phase_hyconv_direct(K)
            else:
                phase_hyconv_fft(K)
            phase_outproj(K, K.z2T, K.hy_w_out, 1, 2)
        if "peer1" in stages:
            phase_norm(K, 1, 1, K.hT, K.hnT, lat_groups, 0, K.normg_l[1, 1])
            phase_peer_tables(K, 1)
            phase_peer(K, 1)
        if "final" in stages:
            phase_final(K)
        if "dbgmod" in K.debug:
            dm = nc.dram_tensor("dbgmod", [128, 2 * 48 * 2], F32, kind="ExternalOutput").ap()
            P.dma("sp", dm, K.MOD[:].rearrange("p a b c -> p (a b c)"), reads=[K.bMOD])
            P.phase_end()
        P.finish()
        K.nops = P.nops
        K.nwaits = P.n_waits
    return nc, K


_HC = {}


def hyena_consts():
    if not _HC:
        f32 = np.float32
        pos = np.arange(L, dtype=f32)
        t = (pos / f32(L - 1)).astype(f32)
        bands = np.linspace(1e-4, 15, 16, dtype=f32)
        ang = ((f32(2 * math.pi) * pos / f32(L))[:, None] * bands[None]).astype(f32)
        z = np.concatenate([t[:, None], np.cos(ang), -np.sin(ang)], axis=-1).astype(f32)
        deltas = np.abs(np.linspace(math.log(1e-2) / 0.3, math.log(1e-2) / 1.5, D, dtype=f32)).astype(f32)
        _HC["hy_zpos"] = np.ascontiguousarray(z.T)
        _HC["hy_tpos"] = np.ascontiguousarray(np.tile(t[None, :], (128, 1)))
        _HC["hy_ndelta_l"] = np.ascontiguousarray(-deltas.reshape(8, 128).T)
    return _HC


def host_inputs(inputs, b):
    f = np.ascontiguousarray
    m = {}
    m["x"] = f(inputs["x"][b])
    m["ctx"] = f(inputs["ctx"][b])
    m["c_l"] = f(inputs["c"][b].reshape(8, 128).T)
    m["cc_l"] = f(inputs["c_ctx"].reshape(8, 128).T)
    m["adab_l"] = f(inputs["ada_b"].reshape(2, 48, 128).transpose(2, 0, 1))
    m["normg_l"] = f(inputs["norm_g"].reshape(2, 2, 8, 128).transpose(0, 1, 3, 2))
    m["finalg_l"] = f(inputs["final_g"].reshape(8, 128).T)
    m["cident"] = np.eye(128, dtype=np.float32)
    m["dn_cw_l"] = f(inputs["dn_conv_w"][0].reshape(5, 24, 128).transpose(2, 1, 0))
    i_ = np.arange(128)
    tri_f = (i_[:, None] <= i_[None, :]).astype(np.float32)
    BIG = 30000.0
    maska_f = np.where(i_[None, :] > i_[:, None], BIG, 0.0).astype(np.float32)
    m01s_f = (i_[None, :] < i_[:, None]).astype(np.float32)
    m["cscan"] = f(np.stack([tri_f, tri_f.T, maska_f, maska_f.T, m01s_f, m01s_f.T]))
    m["dn_alog_l"] = f(np.tile(inputs["dn_a_log"][0].reshape(1, 16), (128, 1)))
    m["dn_dtb_l"] = f(np.tile(inputs["dn_dt_bias"][0].reshape(1, 16), (128, 1)))
    m["dn_gn_l"] = f(np.tile(np.tile(inputs["dn_norm_g"][0], 8).reshape(1, 1024), (128, 1)))
    m["hy_cw_l"] = f(inputs["hy_conv_w"][0].reshape(3, 24, 128).transpose(2, 1, 0))
    m["hy_cb_l"] = f(inputs["hy_conv_b"][0].reshape(24, 128).T)
    m["hy_f_w1"] = f(inputs["hy_f_w1"][0]); m["hy_f_w2"] = f(inputs["hy_f_w2"][0])
    m["hy_f_w3"] = f(inputs["hy_f_w3"][0]); m["hy_f_w4"] = f(inputs["hy_f_w4"][0])
    m["hy_fpv_l"] = f(np.stack([inputs["hy_freq"][0], inputs["hy_f_b1"][0], inputs["hy_f_b2"][0], inputs["hy_f_b3"][0]], axis=1))
    m["hy_skip_l"] = f(inputs["hy_skip"][0].reshape(2, 8, 128).transpose(2, 0, 1))
    m.update(hyena_consts())
    m["hy_skip_h"] = f(inputs["hy_skip"][0].reshape(2, 16, 64).transpose(2, 0, 1))
    m.update(fft_consts())
    m["ada_w"] = inputs["ada_w"]
    m["dn_w_in"] = inputs["dn_w_in"][0]
    m["dn_w_out"] = inputs["dn_w_out"][0]
    m["hy_w_in"] = inputs["hy_w_in"][0]
    m["hy_w_out"] = inputs["hy_w_out"][0]
    m["peer_w_q"] = inputs["peer_w_q"]
    m["peer_u_l"] = f(inputs["peer_u"].reshape(2, 128, 128, 8, 128).transpose(0, 1, 3, 4, 2))
    m["peer_kT_l"] = f(inputs["peer_keys"].reshape(2, 16, 128, 128).transpose(0, 3, 1, 2))
    m["peer_v"] = inputs["peer_v"]
    return m


def phase_dnin(K):
    nc, P = K.nc, K.P
    Tg = 256
    with contextlib.ExitStack() as st:
        Wb = sb(st, nc, "dn_Wb", [128, 8, 4128], BF16)
        cw = sb(st, nc, "dn_cw", [128, 24, 5], F32)
        hnr = Rot([sb(st, nc, f"dn_hn{i}", [128, 8, Tg], BF16) for i in range(2)])
        cvr = Rot([sb(st, nc, f"dn_cv{i}", [128, 24, Tg], F32) for i in range(1)])
        sqr = Rot([sb(st, nc, f"dn_sq{i}", [128, 16, Tg], BF16) for i in range(1)])
        qkr = Rot([sb(st, nc, f"dn_qk{i}", [128, 16, Tg], BF16) for i in range(2)])
        tokr = Rot([sb(st, nc, f"dn_tok{i}", [128, 2, 3072], F32) for i in range(1)])
        sgr = Rot([sb(st, nc, f"dn_sg{i}", [128, 2, 1024], F32) for i in range(1)])
        bar = Rot([sb(st, nc, f"dn_ba{i}", [128, 2, 32], F32) for i in range(2)])
        tmr = Rot([sb(st, nc, f"dn_tm{i}", [128, Tg], F32) for i in range(2)])
        zps = ps_banks(st, nc, 2, "dnz")
        sps = ps_banks(st, nc, 2, "dns")
        tps = ps_banks(st, nc, 2, "dnt")
        gps = ps_banks(st, nc, 2, "dng")
        bW, bcw = Buf(), Buf()
        for kc in range(8):
            for c0 in range(0, 4128, 1376):
                P.dma("pool", Wb[:, kc, c0:c0 + 1376], K.dn_w_in[kc * 128:(kc + 1) * 128, c0:c0 + 1376], writes=[bW])
        P.dma("sp", cw[:], K.dn_cw_l, writes=[bcw])
        groups = [(0, 256)] + [(CTX + g * Tg, 64) for g in range(L // Tg)]
        n = 0
        for off, rowlen in groups:
            hn, hb = hnr.next()
            P.dma("sp", hn[:], K.hnT[:, :, off:off + Tg].rearrange("c p t -> p c t"), writes=[hb])
            cv, cb = cvr.next()
            for cc in range(24):
                ps, pb = zps.next()
                for kc in range(8):
                    P.op("pe", lambda e, ps=ps, hn=hn, cc=cc, kc=kc: e.matmul(
                        ps[:, 0:Tg], lhsT=Wb[:, kc, cc * 128:(cc + 1) * 128], rhs=hn[:, kc, :],
                        start=(kc == 0), stop=(kc == 7)), reads=[bW, hb], writes=[pb])
                P.op("dve", lambda e, ps=ps, cv=cv, cc=cc: e.tensor_scalar(
                    out=cv[:, cc, :], in0=ps[:, 0:Tg], scalar1=cw[:, cc, 2:3], scalar2=None, op0=ALU.mult),
                    reads=[pb, bcw], writes=[cb])
                for k in (0, 1, 3, 4):
                    s = k - 2
                    lo, hi = max(0, -s), rowlen - max(0, s)
                    P.op("dve", lambda e, ps=ps, cv=cv, cc=cc, k=k, s=s, lo=lo, hi=hi, rowlen=rowlen: e.scalar_tensor_tensor(
                        out=cv[:, cc, :].rearrange("p (r w) -> p r w", w=rowlen)[:, :, lo:hi],
                        in0=ps[:, 0:Tg].rearrange("p (r w) -> p r w", w=rowlen)[:, :, lo + s:hi + s],
                        scalar=cw[:, cc, k:k + 1],
                        in1=cv[:, cc, :].rearrange("p (r w) -> p r w", w=rowlen)[:, :, lo:hi],
                        op0=ALU.mult, op1=ALU.add), reads=[pb, bcw], writes=[cb])
            P.op("act", lambda e, cv=cv: e.activation(out=cv[:], in_=cv[:], func=AF.Silu), reads=[cb], writes=[cb])
            sg, sgb = sgr.next()
            bt, btb = bar.next()
            for a in range(2):
                for (c0, wd) in ((0, 512), (512, 512), (1024, 32)):
                    ps, pb = gps.next()
                    for kc in range(8):
                        P.op("pe", lambda e, ps=ps, hn=hn, a=a, kc=kc, c0=c0, wd=wd: e.matmul(
                            ps[:, 0:wd], lhsT=hn[:, kc, a * 128:(a + 1) * 128], rhs=Wb[:, kc, 3072 + c0:3072 + c0 + wd],
                            start=(kc == 0), stop=(kc == 7)), reads=[bW, hb], writes=[pb])
                    if wd == 512:
                        P.op("act", lambda e, ps=ps, sg=sg, a=a, c0=c0: e.activation(
                            out=sg[:, a, c0:c0 + 512], in_=ps[:, :], func=AF.Silu), reads=[pb], writes=[sgb])
                    else:
                        P.op("dve", lambda e, ps=ps, bt=bt, a=a: e.tensor_copy(out=bt[:, a, :], in_=ps[:, 0:32]),
                             reads=[pb], writes=[btb])
            P.dma("pool", K.sgate[off:off + Tg, :].rearrange("(a p) c -> p a c", p=128), sg[:], reads=[sgb])
            P.dma("pool", K.ba[off:off + Tg, :].rearrange("(a p) c -> p a c", p=128), bt[:], reads=[btb])
            sq, sqb_ = sqr.next()
            P.op("act", lambda e, sq=sq, cv=cv: e.activation(out=sq[:], in_=cv[:, 0:16, :], func=AF.Square),
                 reads=[cb], writes=[sqb_])
            qk, qkb = qkr.next()
            for cc in range(16):
                ps, pb = sps.next()
                P.op("pe", lambda e, ps=ps, sq=sq, cc=cc: e.matmul(ps[:, 0:Tg], lhsT=K.onesb[:], rhs=sq[:, cc, :],
                                                                  start=True, stop=True), reads=[sqb_], writes=[pb])
                t, tb = tmr.next()
                P.op("act", lambda e, ps=ps, t=t: e.activation(out=t[:], in_=ps[:, 0:Tg], func=AF.Ln,
                                                               bias=K.epsc[:, 0:1], scale=1.0), reads=[pb], writes=[tb])
                P.op("act", lambda e, t=t: e.activation(out=t[:], in_=t[:], func=AF.Exp, scale=-0.5),
                     reads=[tb], writes=[tb])
                scl = (128.0 ** -0.5) if cc < 8 else 1.0
                P.op("dve", lambda e, cv=cv, cc=cc, t=t, scl=scl: e.scalar_tensor_tensor(
                    out=cv[:, cc, :], in0=cv[:, cc, :], scalar=scl, in1=t[:], op0=ALU.mult, op1=ALU.mult),
                    reads=[cb, tb], writes=[cb])
                P.op("pool", lambda e, cv=cv, qk=qk, cc=cc: e.tensor_copy(out=qk[:, cc, :], in_=cv[:, cc, :]),
                     reads=[cb], writes=[qkb])
            P.dma("pool", K.qT[:, :, off:off + Tg].rearrange("c p t -> p c t"), qk[:, 0:8, :], reads=[qkb])
            P.dma("pool", K.kT[:, :, off:off + Tg].rearrange("c p t -> p c t"), qk[:, 8:16, :], reads=[qkb])
            tok, tkb = tokr.next()
            for a in range(2):
                for g6 in range(6):
                    ps, pb = tps.next()
                    for j in range(4):
                        P.op("pe", lambda e, ps=ps, cv=cv, a=a, g6=g6, j=j: e.transpose(
                            out=ps[:, j * 128:(j + 1) * 128], in_=cv[:, g6 * 4 + j, a * 128:(a + 1) * 128],
                            identity=K.ident[:]), reads=[cb], writes=[pb])
                    if n % 2 == 0:
                        P.op("act", lambda e, ps=ps, tok=tok, a=a, g6=g6: e.activation(
                            out=tok[:, a, g6 * 512:(g6 + 1) * 512], in_=ps[:, :], func=AF.Copy), reads=[pb], writes=[tkb])
                    else:
                        P.op("dve", lambda e, ps=ps, tok=tok, a=a, g6=g6: e.tensor_copy(
                            out=tok[:, a, g6 * 512:(g6 + 1) * 512], in_=ps[:, :]), reads=[pb], writes=[tkb])
                    n += 1
            P.dma("pool", K.qkvtok[off:off + Tg, :].rearrange("(a p) c -> p a c", p=128), tok[:], reads=[tkb])
        P.phase_end()


def phase_dnscan(K):
    nc, P = K.nc, K.P
    NB = TT // 128
    lim = getattr(K, 'scan_maxops', None)
    if lim is not None:
        real_op = P.op
        cnt = [0]

        def lim_op(*a, **k):
            cnt[0] += 1
            if cnt[0] > lim:
                return None
            return real_op(*a, **k)
        P.op = lim_op
    with contextlib.ExitStack() as st:
        cs = sb(st, nc, "sc_consts", [128, 6, 128], F32)
        alg = sb(st, nc, "sc_alog", [128, 16], F32)
        dtb = sb(st, nc, "sc_dtb", [128, 16], F32)
        nea = sb(st, nc, "sc_nea", [128, 16], F32)
        S = [[sb(st, nc, f"sc_S{d}{h}", [128, 128], F32) for h in range(8)] for d in range(2)]
        bS = [[Buf() for h in range(8)] for d in range(2)]
        kTr = Rot([sb(st, nc, f"sc_kT{i}", [128, 8, 128], BF16) for i in range(2)])
        qTr = Rot([sb(st, nc, f"sc_qT{i}", [128, 8, 128], BF16) for i in range(2)])
        tokr = Rot([sb(st, nc, f"sc_tok{i}", [128, 3072], F32) for i in range(2)])
        bar = Rot([sb(st, nc, f"sc_ba{i}", [128, 32], F32) for i in range(2)])
        smr = Rot([sb(st, nc, f"sc_sm{i}", [128, 12, 8], F32) for i in range(2)])
        dgr = Rot([sb(st, nc, f"sc_dg{i}", [128, 5, 8, 128], F32) for i in range(2)])
        obr = Rot([sb(st, nc, f"sc_ob{i}", [128, 1024], F32) for i in range(2)])
        NW = 13
        wkr = [Rot([sb(st, nc, f"sc_wk{h}_{i}", [128, NW, 128], F32) for i in range(1)]) for h in range(8)]
        _pst = [st.enter_context(nc.psum_tensor(f"scps{i}", [128, 512], F32)) for i in range(8)]
        _bb = [Buf(psum=True) for i in range(8)]

        def mkpool(banks):
            r = Rot([_pst[i][:, j * 128:(j + 1) * 128] for i in banks for j in range(4)])
            r.bufs = [_bb[i] for i in banks for j in range(4)]
            return r
        psA = mkpool([0, 1, 2, 3])
        psD = mkpool([4, 5, 6, 7])
        bC = Buf()
        P.dma("sp", cs[:], K.cscan.rearrange("c p f -> p c f"), writes=[bC])
        P.dma("sp", alg[:], K.dn_alog_l, writes=[bC])
        P.dma("sp", dtb[:], K.dn_dtb_l, writes=[bC])
        P.op("act", lambda e: e.activation(out=nea[:], in_=alg[:], func=AF.Exp), reads=[bC], writes=[bC])
        P.op("dve", lambda e: e.tensor_scalar(out=nea[:], in0=nea[:], scalar1=-1.0, scalar2=None, op0=ALU.mult),
             reads=[bC], writes=[bC])
        for d in range(2):
            for h in range(8):
                P.op("pool", lambda e, d=d, h=h: e.memset(S[d][h][:], 0.0), writes=[bS[d][h]])
        TRI = [cs[:, 0, :], cs[:, 1, :]]
        MASKA = [cs[:, 2, :], cs[:, 3, :]]
        M01S = [cs[:, 4, :], cs[:, 5, :]]
        order = [list(range(NB)), [1, 0] + list(range(NB - 1, 1, -1))]
        evn = [0]

        def evac(out, in_, rd, wr, eng):
            if eng == "act":
                P.op("act", lambda e: e.activation(out=out, in_=in_, func=AF.Copy), reads=rd, writes=wr)
            else:
                P.op("dve", lambda e: e.tensor_copy(out=out, in_=in_), reads=rd, writes=wr)

        def job(blk, d):
            t0 = blk * 128
            with_out = blk >= 2
            kT, kTb = kTr.next()
            qT, qTb = qTr.next()
            tok, tkb = tokr.next()
            bt, btb = bar.next()
            sm, smb = smr.next()
            dg, dgb = dgr.next()
            P.dma("sp", kT[:], K.kT[:, :, t0:t0 + 128].rearrange("c p t -> p c t"), writes=[kTb])
            P.dma("sp", qT[:], K.qT[:, :, t0:t0 + 128].rearrange("c p t -> p c t"), writes=[qTb])
            P.dma("sp", tok[:], K.qkvtok[t0:t0 + 128, :], writes=[tkb])
            P.dma("sp", bt[:], K.ba[t0:t0 + 128, :], writes=[btb])
            BETA, NBETA, GG, EG, EKD, GL, BG, TMP, G_ = 0, 1, 2, 3, 4, 5, 6, 7, 8
            P.op("act", lambda e: e.activation(out=sm[:, TMP, :], in_=bt[:, d * 8:(d + 1) * 8], func=AF.Exp, scale=-1.0),
                 reads=[btb], writes=[smb])
            P.op("dve", lambda e: e.tensor_scalar(out=sm[:, TMP, :], in0=sm[:, TMP, :], scalar1=1.0, scalar2=None,
                                                  op0=ALU.add), reads=[smb], writes=[smb])
            P.op("dve", lambda e: e.reciprocal(out=sm[:, BETA, :], in_=sm[:, TMP, :]), reads=[smb], writes=[smb])
            P.op("dve", lambda e: e.tensor_scalar(out=sm[:, NBETA, :], in0=sm[:, BETA, :], scalar1=-1.0, scalar2=None,
                                                  op0=ALU.mult), reads=[smb], writes=[smb])
            P.op("dve", lambda e: e.tensor_tensor(out=sm[:, TMP, :], in0=bt[:, 16 + d * 8:16 + (d + 1) * 8],
                                                  in1=dtb[:, d * 8:(d + 1) * 8], op=ALU.add), reads=[btb, bC], writes=[smb])
            P.op("act", lambda e: e.activation(out=sm[:, TMP, :], in_=sm[:, TMP, :], func=AF.Exp), reads=[smb], writes=[smb])
            P.op("act", lambda e: e.activation(out=sm[:, TMP, :], in_=sm[:, TMP, :], func=AF.Ln, bias=K.onec[:, 0:1],
                                               scale=1.0), reads=[smb], writes=[smb])
            P.op("dve", lambda e: e.tensor_tensor(out=sm[:, G_, :], in0=sm[:, TMP, :], in1=nea[:, d * 8:(d + 1) * 8],
                                                  op=ALU.mult), reads=[smb, bC], writes=[smb])
            pg, pgb = psD.next()
            P.op("pe", lambda e: e.matmul(pg[:, 0:8], lhsT=TRI[d], rhs=sm[:, G_, :], start=True, stop=True),
                 reads=[smb, bC], writes=[pgb])
            P.op("pe", lambda e: e.matmul(pg[:, 8:16], lhsT=K.onesf[:], rhs=sm[:, G_, :], start=True, stop=True),
                 reads=[smb], writes=[pgb])
            P.op("dve", lambda e: e.tensor_copy(out=sm[:, GG, :], in_=pg[:, 0:8]), reads=[pgb], writes=[smb])
            P.op("act", lambda e: e.activation(out=sm[:, EG, :], in_=pg[:, 0:8], func=AF.Exp), reads=[pgb], writes=[smb])
            P.op("act", lambda e: e.activation(out=sm[:, GL, :], in_=pg[:, 8:16], func=AF.Exp), reads=[pgb], writes=[smb])
            P.op("dve", lambda e: e.tensor_tensor(out=sm[:, EKD, :], in0=pg[:, 8:16], in1=sm[:, GG, :], op=ALU.subtract),
                 reads=[pgb, smb], writes=[smb])
            P.op("act", lambda e: e.activation(out=sm[:, EKD, :], in_=sm[:, EKD, :], func=AF.Exp), reads=[smb], writes=[smb])
            P.op("dve", lambda e: e.tensor_tensor(out=sm[:, BG, :], in0=sm[:, BETA, :], in1=sm[:, EG, :], op=ALU.mult),
                 reads=[smb], writes=[smb])
            DIAGG, DIAGEG, KBG, KD, VB = 0, 1, 2, 3, 4
            idb = K.ident[:].unsqueeze(1).to_broadcast([128, 8, 128])

            def bc(i):
                return sm[:, i, :].unsqueeze(2).to_broadcast([128, 8, 128])
            ktok = tok[:, 1024:2048].rearrange("p (h c) -> p h c", c=128)
            vtok = tok[:, 2048:3072].rearrange("p (h c) -> p h c", c=128)
            P.op("pool", lambda e: e.tensor_tensor(out=dg[:, DIAGG], in0=idb, in1=bc(GG), op=ALU.mult),
                 reads=[smb], writes=[dgb])
            P.op("pool", lambda e: e.tensor_tensor(out=dg[:, DIAGEG], in0=idb, in1=bc(EG), op=ALU.mult),
                 reads=[smb], writes=[dgb])
            P.op("pool", lambda e: e.tensor_tensor(out=dg[:, KBG], in0=ktok, in1=bc(BG), op=ALU.mult),
                 reads=[smb, tkb], writes=[dgb])
            P.op("dve", lambda e: e.tensor_tensor(out=dg[:, KD], in0=ktok, in1=bc(EKD), op=ALU.mult),
                 reads=[smb, tkb], writes=[dgb])
            P.op("dve", lambda e: e.tensor_tensor(out=dg[:, VB], in0=vtok, in1=bc(BETA), op=ALU.mult),
                 reads=[smb, tkb], writes=[dgb])
            DM, DMS, X0, X1, Y0, Y1, QA, QAT, TT_, U, WT, QGT, VN = range(13)
            wk = []
            wb = []
            for h in range(8):
                t, b = wkr[h].next()
                wk.append(t)
                wb.append(b)
            if getattr(K, 'scan_stage', 99) <= 1:
                return
            if getattr(K, 'scan_serial', False):
                P.phase_end()
            pkk, pqk, pgr = [], [], []
            for h in range(8):
                a, ab = psD.next()
                P.op("pe", lambda e, a=a, h=h: e.matmul(a, lhsT=kT[:, h, :], rhs=kT[:, h, :], start=True, stop=True),
                     reads=[kTb], writes=[ab])
                pkk.append((a, ab))
                a, ab = psD.next()
                P.op("pe", lambda e, a=a, h=h: e.matmul(a, lhsT=qT[:, h, :], rhs=kT[:, h, :], start=True, stop=True),
                     reads=[kTb, qTb], writes=[ab])
                pqk.append((a, ab))
                a, ab = psA.next()
                P.op("pe", lambda e, a=a, h=h: e.matmul(a, lhsT=K.onesf[:], rhs=dg[:, DIAGG, h, :], start=True, stop=False),
                     reads=[dgb], writes=[ab])
                P.op("pe", lambda e, a=a, h=h: e.matmul(a, lhsT=K.ident[:], rhs=MASKA[d], start=False, stop=True),
                     reads=[bC], writes=[ab])
                pgr.append((a, ab))
            if getattr(K, 'scan_stage', 99) <= 2:
                return
            if getattr(K, 'scan_serial', False):
                P.phase_end()
            for h in range(8):
                w = wk[h]
                P.op("act", lambda e, w=w, h=h: e.activation(out=w[:, DM, :], in_=pgr[h][0], func=AF.Exp,
                                                             bias=sm[:, GG, h:h + 1], scale=-1.0),
                     reads=[pgr[h][1], smb], writes=[wb[h]])
                P.op("pool", lambda e, w=w: e.tensor_tensor(out=w[:, DMS, :], in0=w[:, DM, :], in1=M01S[d], op=ALU.mult),
                     reads=[wb[h], bC], writes=[wb[h]])
                P.op("dve", lambda e, w=w, h=h: e.scalar_tensor_tensor(
                    out=w[:, X0, :], in0=pkk[h][0], scalar=sm[:, NBETA, h:h + 1], in1=w[:, DMS, :],
                    op0=ALU.mult, op1=ALU.mult), reads=[pkk[h][1], smb, wb[h]], writes=[wb[h]])
                P.op("dve", lambda e, w=w, h=h: e.tensor_tensor(out=w[:, QA, :], in0=pqk[h][0], in1=w[:, DM, :], op=ALU.mult),
                     reads=[pqk[h][1], wb[h]], writes=[wb[h]])
            if getattr(K, 'scan_stage', 99) <= 3:
                return
            if getattr(K, 'scan_serial', False):
                P.phase_end()
            pa, pb_ = [], []
            for h in range(8):
                w = wk[h]
                a, ab = psA.next()
                P.op("pe", lambda e, a=a, w=w: e.transpose(out=a, in_=w[:, X0, :], identity=K.ident[:]),
                     reads=[wb[h]], writes=[ab])
                pa.append((a, ab))
                a, ab = psD.next()
                P.op("pe", lambda e, a=a, w=w: e.transpose(out=a, in_=w[:, QA, :], identity=K.ident[:]),
                     reads=[wb[h]], writes=[ab])
                pb_.append((a, ab))
            for h in range(8):
                w = wk[h]
                evac(w[:, Y0, :], pa[h][0], [pa[h][1]], [wb[h]], "act")
                evac(w[:, QAT, :], pb_[h][0], [pb_[h][1]], [wb[h]], "dve")
                P.op("pool", lambda e, w=w: e.tensor_tensor(out=w[:, TT_, :], in0=w[:, Y0, :], in1=K.ident[:], op=ALU.add),
                     reads=[wb[h]], writes=[wb[h]])
            if getattr(K, 'scan_dbg', False) and blk == 0 and d == 0:
                for h in range(8):
                    for i_ in (0, 1, 2, 4, 6, 7, 8):
                        P.dma("sp", K.dbgwk[h, :, i_, :], wk[h][:, i_, :], reads=[wb[h]])
                P.dma("sp", K.dbgsm[:, 0:9, :], sm[:, 0:9, :], reads=[smb])
            if getattr(K, 'scan_stage', 99) <= 4:
                return
            if lim is not None and blk == 0 and d == 0:
                print("ops before S5:", cnt[0])
            if getattr(K, 'scan_serial', False):
                P.phase_end()
            for lvl in range(1, 7):
                xo, yo = (X0, Y0) if lvl % 2 == 1 else (X1, Y1)
                xn, yn = (X1, Y1) if lvl % 2 == 1 else (X0, Y0)
                px, py = [], []
                for h in range(8):
                    w = wk[h]
                    a, ab = psA.next()
                    P.op("pe", lambda e, a=a, w=w, xo=xo, yo=yo: e.matmul(a, lhsT=w[:, yo, :], rhs=w[:, xo, :],
                                                                        start=True, stop=True), reads=[wb[h]], writes=[ab])
                    px.append((a, ab))
                    if lvl < 6:
                        a, ab = psD.next()
                        P.op("pe", lambda e, a=a, w=w, xo=xo, yo=yo: e.matmul(a, lhsT=w[:, xo, :], rhs=w[:, yo, :],
                                                                            start=True, stop=True), reads=[wb[h]], writes=[ab])
                        py.append((a, ab))
                for h in range(8):
                    w = wk[h]
                    P.op("act", lambda e, w=w, h=h, xn=xn, px=px: e.activation(out=w[:, xn, :], in_=px[h][0], func=AF.Copy),
                         reads=[px[h][1]], writes=[wb[h]])
                    if lvl < 6:
                        P.op("dve", lambda e, w=w, h=h, yn=yn, py=py: e.tensor_copy(out=w[:, yn, :], in_=py[h][0]),
                             reads=[py[h][1]], writes=[wb[h]])
                if lim is not None and blk == 0 and d == 0:
                    print("lvl", lvl, "ops before pu:", cnt[0])
                pu = []
                for h in range(8):
                    w = wk[h]
                    a, ab = psD.next()
                    P.op("pe", lambda e, a=a, w=w, xn=xn: e.matmul(a, lhsT=w[:, xn, :], rhs=w[:, TT_, :],
                                                                 start=True, stop=True), reads=[wb[h]], writes=[ab])
                    pu.append((a, ab))
                for h in range(8):
                    w = wk[h]
                    P.op("dve", lambda e, w=w, h=h, pu=pu: e.tensor_tensor(out=w[:, TT_, :], in0=pu[h][0], in1=w[:, TT_, :],
                                                                  op=ALU.add), reads=[pu[h][1], wb[h]], writes=[wb[h]])
            if getattr(K, 'scan_stage', 99) <= 5:
                return
            if getattr(K, 'scan_serial', False):
                P.phase_end()
            p1, p2, p3 = [], [], []
            qtok = tok[:, 0:1024].rearrange("p (h c) -> p h c", c=128)
            for h in range(8):
                w = wk[h]
                a, ab = psA.next()
                P.op("pe", lambda e, a=a, w=w, h=h: e.matmul(a, lhsT=w[:, TT_, :], rhs=dg[:, VB, h, :], start=True, stop=True),
                     reads=[wb[h], dgb], writes=[ab])
                p1.append((a, ab))
                a, ab = psD.next()
                P.op("pe", lambda e, a=a, w=w, h=h: e.matmul(a, lhsT=dg[:, KBG, h, :], rhs=w[:, TT_, :], start=True, stop=True),
                     reads=[wb[h], dgb], writes=[ab])
                p2.append((a, ab))
                a, ab = psA.next()
                P.op("pe", lambda e, a=a, h=h: e.matmul(a, lhsT=qtok[:, h, :], rhs=dg[:, DIAGEG, h, :], start=True, stop=True),
                     reads=[tkb, dgb], writes=[ab])
                p3.append((a, ab))
            for h in range(8):
                w = wk[h]
                evac(w[:, U, :], p1[h][0], [p1[h][1]], [wb[h]], "act")
                evac(w[:, WT, :], p2[h][0], [p2[h][1]], [wb[h]], "dve")
                evac(w[:, QGT, :], p3[h][0], [p3[h][1]], [wb[h]], "act")
            if getattr(K, 'scan_stage', 99) <= 6:
                return
            if getattr(K, 'scan_serial', False):
                P.phase_end()
            pv = []
            for h in range(8):
                w = wk[h]
                a, ab = psD.next()
                P.op("pe", lambda e, a=a, w=w, h=h: e.matmul(a, lhsT=w[:, WT, :], rhs=S[d][h][:], start=True, stop=True),
                     reads=[wb[h], bS[d][h]], writes=[ab])
                pv.append((a, ab))
            for h in range(8):
                w = wk[h]
                P.op("dve", lambda e, w=w, h=h: e.tensor_tensor(out=w[:, VN, :], in0=w[:, U, :], in1=pv[h][0], op=ALU.subtract),
                     reads=[pv[h][1], wb[h]], writes=[wb[h]])
            po, pd = [], []
            for h in range(8):
                w = wk[h]
                if with_out:
                    a, ab = psA.next()
                    P.op("pe", lambda e, a=a, w=w, h=h: e.matmul(a, lhsT=w[:, QGT, :], rhs=S[d][h][:], start=True, stop=False),
                         reads=[wb[h], bS[d][h]], writes=[ab])
                    P.op("pe", lambda e, a=a, w=w: e.matmul(a, lhsT=w[:, QAT, :], rhs=w[:, VN, :], start=False, stop=True),
                         reads=[wb[h]], writes=[ab])
                    po.append((a, ab))
                a, ab = psD.next()
                P.op("pe", lambda e, a=a, w=w, h=h: e.matmul(a, lhsT=dg[:, KD, h, :], rhs=w[:, VN, :], start=True, stop=True),
                     reads=[wb[h], dgb], writes=[ab])
                pd.append((a, ab))
            if with_out:
                ob, obb = obr.next()
            for h in range(8):
                P.op("dve", lambda e, h=h: e.scalar_tensor_tensor(
                    out=S[d][h][:], in0=S[d][h][:], scalar=sm[:, GL, h:h + 1], in1=pd[h][0], op0=ALU.mult, op1=ALU.add),
                    reads=[pd[h][1], smb], writes=[bS[d][h]])
                if with_out:
                    P.op("act", lambda e, h=h: e.activation(out=ob[:, h * 128:(h + 1) * 128], in_=po[h][0], func=AF.Copy),
                         reads=[po[h][1]], writes=[obb])
            if with_out:
                dst = K.o_dir[d]
                P.dma("sp", dst[t0 - CTX:t0 - CTX + 128, :], ob[:], reads=[obb])

        for s in range(getattr(K, 'scan_steps', NB)):
            job(order[0][s], 0)
            job(order[1][s], 1)
        P.phase_end()


def phase_dnout(K):
    nc, P = K.nc, K.P
    with contextlib.ExitStack() as st:
        gn = sb(st, nc, "do_gn", [128, 1024], F32)
        ofr = Rot([sb(st, nc, f"do_of{i}", [128, 1024], F32) for i in range(2)])
        obr = Rot([sb(st, nc, f"do_ob{i}", [128, 1024], F32) for i in range(2)])
        sgr = Rot([sb(st, nc, f"do_sg{i}", [128, 1024], F32) for i in range(2)])
        sqr = Rot([sb(st, nc, f"do_sq{i}", [128, 1024], F32) for i in range(2)])
        msr = Rot([sb(st, nc, f"do_ms{i}", [128, 8], F32) for i in range(2)])
        ygr = Rot([sb(st, nc, f"do_yg{i}", [128, 8, 128], BF16) for i in range(2)])
        psr = ps_banks(st, nc, 4, "dops")
        bg = Buf()
        P.dma("sp", gn[:], K.dn_gn_l, writes=[bg])
        n = 0
        for tt in range(L // 128):
            t0 = tt * 128
            of, ofb = ofr.next()
            ob, obb = obr.next()
            sg, sgb = sgr.next()
            sq, sqb = sqr.next()
            ms, msb = msr.next()
            yg, ygb = ygr.next()
            P.dma("sp", of[:], K.o_dir[0][t0:t0 + 128, :], writes=[ofb])
            P.dma("sp", ob[:], K.o_dir[1][t0:t0 + 128, :], writes=[obb])
            P.dma("sp", sg[:], K.sgate[CTX + t0:CTX + t0 + 128, :], writes=[sgb])
            P.op("dve", lambda e, of=of, ob=ob: e.tensor_tensor(out=of[:], in0=of[:], in1=ob[:], op=ALU.add),
                 reads=[obb], writes=[ofb])
            P.op("act", lambda e, of=of, sq=sq: e.activation(out=sq[:], in_=of[:], func=AF.Square), reads=[ofb], writes=[sqb])
            P.op("dve", lambda e, sq=sq, ms=ms: e.tensor_reduce(out=ms[:], in_=sq[:].rearrange("p (h c) -> p h c", c=128),
                                                               axis=AX.X, op=ALU.add), reads=[sqb], writes=[msb])
            P.op("act", lambda e, ms=ms: e.activation(out=ms[:], in_=ms[:], func=AF.Sqrt, bias=K.epsc[:, 0:1], scale=1.0 / 128),
                 reads=[msb], writes=[msb])
            P.op("dve", lambda e, ms=ms: e.reciprocal(out=ms[:], in_=ms[:]), reads=[msb], writes=[msb])
            P.op("dve", lambda e, of=of, ms=ms: e.tensor_tensor(
                out=of[:].rearrange("p (h c) -> p h c", c=128), in0=of[:].rearrange("p (h c) -> p h c", c=128),
                in1=ms[:].unsqueeze(2).to_broadcast([128, 8, 128]), op=ALU.mult), reads=[msb], writes=[ofb])
            P.op("pool", lambda e, of=of, sg=sg: e.tensor_tensor(out=sg[:], in0=sg[:], in1=gn[:], op=ALU.mult),
                 reads=[bg], writes=[sgb])
            P.op("pool", lambda e, of=of, sg=sg: e.tensor_tensor(out=of[:], in0=of[:], in1=sg[:], op=ALU.mult),
                 reads=[sgb], writes=[ofb])
            for half in range(2):
                ps, pb = psr.next()
                for j in range(4):
                    cc = half * 4 + j
                    P.op("pe", lambda e, ps=ps, of=of, j=j, cc=cc: e.transpose(
                        out=ps[:, j * 128:(j + 1) * 128], in_=of[:, cc * 128:(cc + 1) * 128], identity=K.ident[:]),
                        reads=[ofb], writes=[pb])
                if n % 2 == 0:
                    P.op("act", lambda e, ps=ps, yg=yg, half=half: e.activation(
                        out=yg[:, half * 4:(half + 1) * 4, :], in_=ps[:, :].rearrange("p (j t) -> p j t", t=128),
                        func=AF.Copy), reads=[pb], writes=[ygb])
                else:
                    P.op("dve", lambda e, ps=ps, yg=yg, half=half: e.tensor_copy(
                        out=yg[:, half * 4:(half + 1) * 4, :], in_=ps[:, :].rearrange("p (j t) -> p j t", t=128)),
                        reads=[pb], writes=[ygb])
                n += 1
            P.dma("sp", K.ygT[:, :, t0:t0 + 128].rearrange("c p t -> p c t"), yg[:], reads=[ygb])
        P.phase_end()


def phase_outproj(K, srcT, W, l, m):
    nc, P = K.nc, K.P
    with contextlib.ExitStack() as st:
        Wb = sb(st, nc, "op_W", [128, 8, 1024], BF16)
        srr = Rot([sb(st, nc, f"op_src{i}", [128, 8, 512], BF16) for i in range(2)])
        hr = Rot([sb(st, nc, f"op_h{i}", [128, 8, 512], F32) for i in range(2)])
        psr = ps_banks(st, nc, 4, "opps")
        bW = Buf()
        for cc in range(8):
            P.dma("pool", Wb[:, cc, :], W[cc * 128:(cc + 1) * 128, :], writes=[bW])
        for g in range(L // 512):
            src, sbb = srr.next()
            h, hb = hr.next()
            P.dma("sp", src[:], srcT[:, :, g * 512:(g + 1) * 512].rearrange("c p t -> p c t"), writes=[sbb])
            P.dma("sp", h[:], K.hT[:, :, g * 512:(g + 1) * 512].rearrange("c p t -> p c t"), writes=[hb])
            for dc in range(8):
                ps, pb = psr.next()
                for cc in range(8):
                    P.op("pe", lambda e, ps=ps, src=src, cc=cc, dc=dc: e.matmul(
                        ps[:, :], lhsT=Wb[:, cc, dc * 128:(dc + 1) * 128], rhs=src[:, cc, :],
                        start=(cc == 0), stop=(cc == 7)), reads=[bW, sbb], writes=[pb])
                P.op("dve", lambda e, ps=ps, h=h, dc=dc: e.scalar_tensor_tensor(
                    out=h[:, dc, :], in0=ps[:, :], scalar=K.MOD[:, l, m * 8 + dc, 0:1], in1=h[:, dc, :],
                    op0=ALU.mult, op1=ALU.add), reads=[pb, K.bMOD], writes=[hb])
            P.dma("sp", K.hT[:, :, g * 512:(g + 1) * 512].rearrange("c p t -> p c t"), h[:], reads=[hb])
        P.phase_end()


def phase_peer_tables(K, l):
    nc, P = K.nc, K.P
    with contextlib.ExitStack() as st:
        ur = Rot([sb(st, nc, f"pt_u{i}", [128, 8, 128], BF16) for i in range(3)])
        vr = Rot([sb(st, nc, f"pt_v{i}", [128, 1024], BF16) for i in range(3)])
        for i in range(128):
            u, ub = ur.next()
            v, vb = vr.next()
            for kc in range(8):
                P.dma("pool", u[:, kc, :], K.peer_u_l[l, i, kc], writes=[ub])
            P.dma("pool", v[:], K.peer_v[l, i * 128:(i + 1) * 128, :], writes=[vb])
            P.dma("sp", K.Ub[i], u[:], reads=[ub])
            P.dma("sp", K.Vb[i], v[:], reads=[vb])
        P.phase_end()


def phase_peer(K, l):
    nc, P = K.nc, K.P
    NEG = -1.0e30
    with contextlib.ExitStack() as st:
        Wq = sb(st, nc, "pe_Wq", [128, 8, 2048], BF16)
        kT = sb(st, nc, "pe_kT", [128, 16, 128], F32)
        xpr = Rot([sb(st, nc, f"pe_xp{i}", [128, 8, 128], BF16) for i in range(2)])
        qsb = sb(st, nc, "pe_q", [128, 16, 128], F32)
        sc = sb(st, nc, "pe_sc", [128, 16, 128], F32)
        sc2 = sb(st, nc, "pe_sc2", [128, 16, 128], F32)
        v16 = sb(st, nc, "pe_v16", [128, 16, 16], F32)
        cand = sb(st, nc, "pe_cand", [128, 8, 256], F32)
        cand2 = sb(st, nc, "pe_cand2", [128, 8, 256], F32)
        c16 = sb(st, nc, "pe_c16", [128, 8, 16], F32)
        e16 = sb(st, nc, "pe_e16", [128, 8, 16], F32)
        sm = sb(st, nc, "pe_sm", [128, 4, 8], F32)
        sumr = Rot([sb(st, nc, f"pe_sum{i}", [128, 2048], F32) for i in range(2)])
        mkr = Rot([sb(st, nc, f"pe_mk{i}", [128, 2048], BF16) for i in range(2)])
        wbr = Rot([sb(st, nc, f"pe_wb{i}", [128, 2048], BF16) for i in range(2)])
        Er = Rot([sb(st, nc, f"pe_E{i}", [128, 2048], F32) for i in range(2)])
        WallT = sb(st, nc, "pe_WallT", [128, 128, 128], BF16)
        ur = Rot([sb(st, nc, f"pe_u{i}", [128, 8, 128], BF16) for i in range(3)])
        vr = Rot([sb(st, nc, f"pe_v{i}", [128, 1024], BF16) for i in range(3)])
        gr = Rot([sb(st, nc, f"pe_g{i}", [128, 128], F32) for i in range(2)])
        cfr = Rot([sb(st, nc, f"pe_cf{i}", [128, 128], BF16) for i in range(2)])
        ysb = sb(st, nc, "pe_y", [128, 1024], F32)
        hsb = sb(st, nc, "pe_h", [128, 8, 128], F32)
        gps = ps_banks(st, nc, 4, "pegps")
        hps = ps_banks(st, nc, 2, "pehps")
        yps = ps_banks(st, nc, 2, "peyps")
        bW, bk, bq, bsc, bsc2, bv, bc, bc2, bc16, be, bsm, bacc, bwall, by, bh = [Buf() for _ in range(15)]
        for kc in range(8):
            P.dma("pool", Wq[:, kc, :], K.peer_w_q[l, kc * 128:(kc + 1) * 128, :], writes=[bW])
        P.dma("sp", kT[:], K.peer_kT_l[l], writes=[bk])
        for g in range(getattr(K, 'peer_groups', L // 128)):
            t0 = g * 128
            xp, xb = xpr.next()
            P.dma("sp", xp[:], K.hnT[:, :, CTX + t0:CTX + t0 + 128].rearrange("c p t -> p c t"), writes=[xb])
            P.dma("sp", hsb[:], K.hT[:, :, t0:t0 + 128].rearrange("c p t -> p c t"), writes=[bh])
            for b4 in range(4):
                ps, pb = gps.next()
                for j in range(4):
                    hp = b4 * 4 + j
                    for kc in range(8):
                        P.op("pe", lambda e, ps=ps, xp=xp, j=j, hp=hp, kc=kc: e.matmul(
                            ps[:, j * 128:(j + 1) * 128], lhsT=Wq[:, kc, hp * 128:(hp + 1) * 128], rhs=xp[:, kc, :],
                            start=(kc == 0), stop=(kc == 7)), reads=[bW, xb], writes=[pb])
                P.op("act", lambda e, ps=ps, b4=b4: e.activation(
                    out=qsb[:, b4 * 4:(b4 + 1) * 4, :], in_=ps[:, :].rearrange("p (j t) -> p j t", t=128), func=AF.Copy),
                    reads=[pb], writes=[bq])
            for b4 in range(4):
                ps, pb = gps.next()
                for j in range(4):
                    hp = b4 * 4 + j
                    P.op("pe", lambda e, ps=ps, j=j, hp=hp: e.matmul(
                        ps[:, j * 128:(j + 1) * 128], lhsT=qsb[:, hp, :], rhs=kT[:, hp, :], start=True, stop=True),
                        reads=[bq, bk], writes=[pb])
                P.op("act", lambda e, ps=ps, b4=b4: e.activation(
                    out=sc[:, b4 * 4:(b4 + 1) * 4, :], in_=ps[:, :].rearrange("p (j t) -> p j t", t=128), func=AF.Copy),
                    reads=[pb], writes=[bsc])
            for hp in range(16):
                P.op("dve", lambda e, hp=hp: e.max(out=v16[:, hp, 0:8], in_=sc[:, hp, :]), reads=[bsc], writes=[bv])
                P.op("dve", lambda e, hp=hp: e.match_replace(out=sc2[:, hp, :], in_to_replace=v16[:, hp, 0:8],
                                                            in_values=sc[:, hp, :], imm_value=NEG),
                     reads=[bsc, bv], writes=[bsc2])
                P.op("dve", lambda e, hp=hp: e.max(out=v16[:, hp, 8:16], in_=sc2[:, hp, :]), reads=[bsc2], writes=[bv])
            v4 = v16[:].rearrange("p (h two) k -> p h two k", two=2)
            P.op("dve", lambda e: e.tensor_tensor(
                out=cand[:].rearrange("p h (a b) -> p h a b", b=16),
                in0=v4[:, :, 0, :].unsqueeze(3).to_broadcast([128, 8, 16, 16]),
                in1=v4[:, :, 1, :].unsqueeze(2).to_broadcast([128, 8, 16, 16]), op=ALU.add), reads=[bv], writes=[bc])
            for h in range(8):
                P.op("dve", lambda e, h=h: e.max(out=c16[:, h, 0:8], in_=cand[:, h, :]), reads=[bc], writes=[bc16])
                P.op("dve", lambda e, h=h: e.match_replace(out=cand2[:, h, :], in_to_replace=c16[:, h, 0:8],
                                                          in_values=cand[:, h, :], imm_value=NEG),
                     reads=[bc, bc16], writes=[bc2])
                P.op("dve", lambda e, h=h: e.max(out=c16[:, h, 8:16], in_=cand2[:, h, :]), reads=[bc2], writes=[bc16])
            P.op("dve", lambda e: e.tensor_copy(out=sm[:, 0, :], in_=c16[:, :, 15]), reads=[bc16], writes=[bsm])
            P.op("dve", lambda e: e.tensor_scalar(out=sm[:, 1, :], in0=c16[:, :, 0], scalar1=-1.0, scalar2=None,
                                                  op0=ALU.mult), reads=[bc16], writes=[bsm])
            P.op("dve", lambda e: e.tensor_tensor(out=e16[:], in0=c16[:], in1=sm[:, 1, :].unsqueeze(2).to_broadcast([128, 8, 16]),
                                                  op=ALU.add), reads=[bc16, bsm], writes=[be])
            P.op("act", lambda e: e.activation(out=e16[:], in_=e16[:], func=AF.Exp), reads=[be], writes=[be])
            P.op("dve", lambda e: e.tensor_reduce(out=sm[:, 2, :], in_=e16[:], axis=AX.X, op=ALU.add), reads=[be], writes=[bsm])
            P.op("act", lambda e: e.activation(out=sm[:, 3, :], in_=sm[:, 2, :], func=AF.Ln), reads=[bsm], writes=[bsm])
            P.op("dve", lambda e: e.tensor_tensor(out=sm[:, 1, :], in0=sm[:, 1, :], in1=sm[:, 3, :], op=ALU.subtract),
                 reads=[bsm], writes=[bsm])
            for q4 in range(8):
                banks = [gps.next() for _ in range(4)]
                for h in range(8):
                    su, sub_ = sumr.next()
                    mk_, mkb = mkr.next()
                    E_, Eb = Er.next()
                    wb_, wbb = wbr.next()
                    P.op("dve", lambda e, su=su, h=h, q4=q4: e.tensor_tensor(
                        out=su[:].rearrange("p (i j) -> p i j", j=128),
                        in0=sc[:, 2 * h, q4 * 16:(q4 + 1) * 16].unsqueeze(2).to_broadcast([128, 16, 128]),
                        in1=sc[:, 2 * h + 1, :].unsqueeze(1).to_broadcast([128, 16, 128]), op=ALU.add),
                        reads=[bsc], writes=[sub_])
                    P.op("dve", lambda e, su=su, mk_=mk_, h=h: e.tensor_scalar(
                        out=mk_[:], in0=su[:], scalar1=sm[:, 0, h:h + 1], scalar2=None, op0=ALU.is_ge),
                        reads=[sub_, bsm], writes=[mkb])
                    P.op("act", lambda e, su=su, E_=E_, h=h: e.activation(
                        out=E_[:], in_=su[:], func=AF.Exp, bias=sm[:, 1, h:h + 1], scale=1.0), reads=[sub_, bsm], writes=[Eb])
                    P.op("pool", lambda e, E_=E_, mk_=mk_, wb_=wb_: e.tensor_tensor(out=wb_[:], in0=E_[:], in1=mk_[:], op=ALU.mult),
                         reads=[mkb, Eb], writes=[wbb])
                    for b8 in range(4):
                        ps, pb = banks[b8]
                        for j in range(4):
                            ii = b8 * 4 + j
                            P.op("pe", lambda e, ps=ps, j=j, ii=ii, wb_=wb_, h=h: e.matmul(
                                ps[:, j * 128:(j + 1) * 128], lhsT=wb_[:, ii * 128:(ii + 1) * 128], rhs=K.identb[:],
                                start=(h == 0 and j == 0), stop=(h == 7), skip_group_check=True),
                                reads=[wbb], writes=[pb])
                for b8 in range(4):
                    ps, pb = banks[b8]
                    i0 = q4 * 16 + b8 * 4
                    P.op("act", lambda e, ps=ps, i0=i0: e.activation(
                        out=WallT[:, i0:i0 + 4, :], in_=ps[:, :].rearrange("p (j t) -> p j t", t=128), func=AF.Copy),
                        reads=[pb], writes=[bwall])
            y0, y0b = yps.next()
            y1, y1b = yps.next()
            for i in range(128):
                u, ub = ur.next()
                v, vb = vr.next()
                P.dma("sp", u[:], K.Ub[i], writes=[ub])
                P.dma("sp", v[:], K.Vb[i], writes=[vb])
                hp_, hpb = hps.next()
                for kc in range(8):
                    P.op("pe", lambda e, hp_=hp_, u=u, xp=xp, kc=kc: e.matmul(
                        hp_[:, 0:128], lhsT=u[:, kc, :], rhs=xp[:, kc, :], start=(kc == 0), stop=(kc == 7)),
                        reads=[ub, xb], writes=[hpb])
                g_, gb = gr.next()
                P.op("act", lambda e, hp_=hp_, g_=g_: e.activation(out=g_[:], in_=hp_[:, 0:128], func=AF.Gelu),
                     reads=[hpb], writes=[gb])
                cf, cfb = cfr.next()
                P.op("dve", lambda e, g_=g_, cf=cf, i=i: e.tensor_tensor(out=cf[:], in0=g_[:], in1=WallT[:, i, :], op=ALU.mult),
                     reads=[gb, bwall], writes=[cfb])
                P.op("pe", lambda e, cf=cf, v=v, i=i: e.matmul(y0[:, :], lhsT=cf[:], rhs=v[:, 0:512], start=(i == 0), stop=(i == 127)),
                     reads=[cfb, vb], writes=[y0b])
                P.op("pe", lambda e, cf=cf, v=v, i=i: e.matmul(y1[:, :], lhsT=cf[:], rhs=v[:, 512:1024], start=(i == 0), stop=(i == 127)),
                     reads=[cfb, vb], writes=[y1b])
            P.op("act", lambda e: e.activation(out=ysb[:, 0:512], in_=y0[:, :], func=AF.Copy), reads=[y0b], writes=[by])
            P.op("act", lambda e: e.activation(out=ysb[:, 512:1024], in_=y1[:, :], func=AF.Copy), reads=[y1b], writes=[by])
            for half in range(2):
                ps, pb = gps.next()
                for j in range(4):
                    dc = half * 4 + j
                    P.op("pe", lambda e, ps=ps, j=j, dc=dc: e.transpose(
                        out=ps[:, j * 128:(j + 1) * 128], in_=ysb[:, dc * 128:(dc + 1) * 128], identity=K.ident[:]),
                        reads=[by], writes=[pb])
                for j in range(4):
                    dc = half * 4 + j
                    P.op("dve", lambda e, ps=ps, j=j, dc=dc: e.scalar_tensor_tensor(
                        out=hsb[:, dc, :], in0=ps[:, j * 128:(j + 1) * 128], scalar=K.MOD[:, l, 5 * 8 + dc, 0:1],
                        in1=hsb[:, dc, :], op0=ALU.mult, op1=ALU.add), reads=[pb, K.bMOD], writes=[bh])
            P.dma("sp", K.hT[:, :, t0:t0 + 128].rearrange("c p t -> p c t"), hsb[:], reads=[bh])
        P.phase_end()


def phase_hyin(K):
    nc, P = K.nc, K.P
    with contextlib.ExitStack() as st:
        Wb = sb(st, nc, "hi_Wb", [128, 8, 3072], BF16)
        cw = sb(st, nc, "hi_cw", [128, 24, 3], F32)
        cb = sb(st, nc, "hi_cb", [128, 24], F32)
        hnr = Rot([sb(st, nc, f"hi_hn{i}", [128, 8, 512], BF16) for i in range(2)])
        ztr = Rot([sb(st, nc, f"hi_zt{i}", [128, 24, 512], F32) for i in range(2)])
        zps = ps_banks(st, nc, 4, "hiz")
        bW, bcw = Buf(), Buf()
        for kc in range(8):
            for c0 in range(0, 3072, 1536):
                P.dma("pool", Wb[:, kc, c0:c0 + 1536], K.hy_w_in[kc * 128:(kc + 1) * 128, c0:c0 + 1536], writes=[bW])
        P.dma("sp", cw[:], K.hy_cw_l, writes=[bcw])
        P.dma("sp", cb[:], K.hy_cb_l, writes=[bcw])
        for g in range(L // 512):
            hn, hb = hnr.next()
            zt, zb = ztr.next()
            P.dma("sp", hn[:], K.hnT[:, :, CTX + g * 512:CTX + (g + 1) * 512].rearrange("c p t -> p c t"), writes=[hb])
            for cc in range(24):
                ps, pb = zps.next()
                for kc in range(8):
                    P.op("pe", lambda e, ps=ps, hn=hn, cc=cc, kc=kc: e.matmul(
                        ps[:, :], lhsT=Wb[:, kc, cc * 128:(cc + 1) * 128], rhs=hn[:, kc, :],
                        start=(kc == 0), stop=(kc == 7)), reads=[bW, hb], writes=[pb])
                P.op("dve", lambda e, ps=ps, zt=zt, cc=cc: e.tensor_scalar(
                    out=zt[:, cc, :], in0=ps[:, :], scalar1=cw[:, cc, 1:2], scalar2=cb[:, cc:cc + 1],
                    op0=ALU.mult, op1=ALU.add), reads=[pb, bcw], writes=[zb])
                for k in (0, 2):
                    s = k - 1
                    lo, hi = max(0, -s), 64 - max(0, s)
                    P.op("dve", lambda e, ps=ps, zt=zt, cc=cc, k=k, s=s, lo=lo, hi=hi: e.scalar_tensor_tensor(
                        out=zt[:, cc, :].rearrange("p (r w) -> p r w", w=64)[:, :, lo:hi],
                        in0=ps[:, :].rearrange("p (r w) -> p r w", w=64)[:, :, lo + s:hi + s],
                        scalar=cw[:, cc, k:k + 1],
                        in1=zt[:, cc, :].rearrange("p (r w) -> p r w", w=64)[:, :, lo:hi],
                        op0=ALU.mult, op1=ALU.add), reads=[pb, bcw], writes=[zb])
            P.dma("sp", K.zT[:, :, g * 512:(g + 1) * 512].rearrange("c p t -> p c t"), zt[:], reads=[zb])
        P.phase_end()


def _wrap_pi(P, t, tb, tmpa, tab, n, cols):
    PI = math.pi
    for _ in range(2):
        P.op("dve", lambda e: e.tensor_scalar(out=tmpa[0:n, 0:cols], in0=t[0:n, 0:cols], scalar1=PI, scalar2=-2 * PI,
                                              op0=ALU.is_gt, op1=ALU.mult), reads=[tb], writes=[tab])
        P.op("dve", lambda e: e.tensor_tensor(out=t[0:n, 0:cols], in0=t[0:n, 0:cols], in1=tmpa[0:n, 0:cols], op=ALU.add),
             reads=[tab], writes=[tb])
        P.op("dve", lambda e: e.tensor_scalar(out=tmpa[0:n, 0:cols], in0=t[0:n, 0:cols], scalar1=-PI, scalar2=2 * PI,
                                              op0=ALU.is_lt, op1=ALU.mult), reads=[tb], writes=[tab])
        P.op("dve", lambda e: e.tensor_tensor(out=t[0:n, 0:cols], in0=t[0:n, 0:cols], in1=tmpa[0:n, 0:cols], op=ALU.add),
             reads=[tab], writes=[tb])


def phase_hyfilt(K):
    nc, P = K.nc, K.P
    with contextlib.ExitStack() as st:
        w1 = sb(st, nc, "hf_w1", [33, 64], F32)
        w2 = sb(st, nc, "hf_w2", [64, 64], F32)
        w3 = sb(st, nc, "hf_w3", [64, 64], F32)
        w4 = sb(st, nc, "hf_w4", [64, 4096], F32)
        pv = sb(st, nc, "hf_pv", [64, 8], F32)
        dl = sb(st, nc, "hf_dl", [128, 8], F32)
        zpr = Rot([sb(st, nc, f"hf_zp{i}", [33, 512], F32) for i in range(2)])
        tpr = Rot([sb(st, nc, f"hf_tp{i}", [128, 512], F32) for i in range(2)])
        hr = Rot([sb(st, nc, f"hf_h{i}", [64, 512], F32) for i in range(3)])
        tmpa = sb(st, nc, "hf_tmp", [64, 512], F32)
        dcr = Rot([sb(st, nc, f"hf_dc{i}", [128, 8, 512], F32) for i in range(2)])
        fr_ = Rot([sb(st, nc, f"hf_f{i}", [128, 512], F32) for i in range(4)])
        psr = ps_banks(st, nc, 4, "hfps")
        bw, btm = Buf(), Buf()
        P.dma("sp", w1[:], K.hy_f_w1, writes=[bw])
        P.dma("sp", w2[:], K.hy_f_w2, writes=[bw])
        P.dma("sp", w3[:], K.hy_f_w3, writes=[bw])
        P.dma("sp", w4[:], K.hy_f_w4, writes=[bw])
        P.dma("sp", pv[:, 0:4], K.hy_fpv_l, writes=[bw])
        P.dma("sp", dl[:], K.hy_ndelta_l, writes=[bw])
        P.op("dve", lambda e: e.tensor_tensor(out=pv[:, 4:7], in0=pv[:, 1:4], in1=pv[:, 0:1].to_broadcast([64, 3]), op=ALU.mult),
             reads=[bw], writes=[bw])
        ws = [w1, w2, w3]
        for pc in range(L // 512):
            zp, zpb = zpr.next()
            tp, tpb = tpr.next()
            P.dma("sp", zp[:], K.hy_zpos[:, pc * 512:(pc + 1) * 512], writes=[zpb])
            P.dma("sp", tp[:], K.hy_tpos[:, pc * 512:(pc + 1) * 512], writes=[tpb])
            cur, curb, kdim = zp, zpb, 33
            for li in range(3):
                ps, pb = psr.next()
                P.op("pe", lambda e, ps=ps, cur=cur, li=li, kdim=kdim: e.matmul(
                    ps[0:64, :], lhsT=ws[li][0:kdim, :], rhs=cur[0:kdim, :], start=True, stop=True),
                    reads=[bw, curb], writes=[pb])
                h, hb = hr.next()
                P.op("dve", lambda e, ps=ps, h=h, li=li: e.tensor_scalar(
                    out=h[:], in0=ps[0:64, :], scalar1=pv[:, 0:1], scalar2=pv[:, 4 + li:5 + li], op0=ALU.mult, op1=ALU.add),
                    reads=[pb, bw], writes=[hb])
                _wrap_pi(P, h, hb, tmpa, btm, 64, 512)
                P.op("act", lambda e, h=h: e.activation(out=h[:], in_=h[:], func=AF.Sin), reads=[hb], writes=[hb])
                cur, curb, kdim = h, hb, 64
            dc, dcb = dcr.next()
            for cc in range(8):
                P.op("act", lambda e, dc=dc, tp=tp, cc=cc: e.activation(out=dc[:, cc, :], in_=tp[:], func=AF.Exp,
                                                                        scale=dl[:, cc:cc + 1]), reads=[tpb, bw], writes=[dcb])
            for o in range(2):
                for d_ in range(2):
                    for cc in range(8):
                        col = o * 2048 + d_ * 1024 + cc * 128
                        ps, pb = psr.next()
                        P.op("pe", lambda e, ps=ps, cur=cur, col=col: e.matmul(
                            ps[:, :], lhsT=w4[:, col:col + 128], rhs=cur[:, :], start=True, stop=True),
                            reads=[bw, curb], writes=[pb])
                        f_, fb = fr_.next()
                        P.op("dve", lambda e, ps=ps, f_=f_, dc=dc, cc=cc: e.tensor_tensor(
                            out=f_[:], in0=ps[:, :], in1=dc[:, cc, :], op=ALU.mult), reads=[pb, dcb], writes=[fb])
                        P.dma("sp", K.filtT[o, d_, cc, :, pc * 512:(pc + 1) * 512], f_[:], reads=[fb])
        P.phase_end()


def phase_hyconv_direct(K):
    nc, P = K.nc, K.P
    with contextlib.ExitStack() as st:
        u = sb(st, nc, "hc_u", [128, L], F32)
        x = sb(st, nc, "hc_x", [128, L], F32)
        hf = sb(st, nc, "hc_hf", [128, L], F32)
        hb_ = sb(st, nc, "hc_hb", [128, L], F32)
        acc = sb(st, nc, "hc_acc", [128, L], F32)
        zo = sb(st, nc, "hc_zo", [128, L], BF16)
        sk = sb(st, nc, "hc_sk", [128, 2, 8], F32)
        s0 = sb(st, nc, "hc_s0", [128, 1], F32)
        bu, bx, bhf, bhb, bacc, bzo, bsk, bs0 = [Buf() for _ in range(8)]
        P.dma("sp", sk[:], K.hy_skip_l, writes=[bsk])
        nl = getattr(K, 'hy_nlags', L)
        for cc in range(getattr(K, 'hy_chunks', 8)):
            P.dma("sp", u[:], K.zT[cc], writes=[bu])
            for o in range(2):
                P.dma("sp", x[:], K.zT[8 * (o + 1) + cc], writes=[bx])
                P.dma("sp", hf[:], K.filtT[o, 0, cc], writes=[bhf])
                P.dma("sp", hb_[:], K.filtT[o, 1, cc], writes=[bhb])
                P.op("dve", lambda e, o=o, cc=cc: e.tensor_tensor(out=s0[:], in0=hf[:, 0:1], in1=sk[:, o, cc:cc + 1], op=ALU.add),
                     reads=[bhf, bsk], writes=[bs0])
                P.op("dve", lambda e: e.tensor_scalar(out=acc[:], in0=u[:], scalar1=s0[:, 0:1], scalar2=None, op0=ALU.mult),
                     reads=[bu, bs0], writes=[bacc])
                for lag in range(1, nl):
                    P.op("dve", lambda e, lag=lag: e.scalar_tensor_tensor(
                        out=acc[:, lag:L], in0=u[:, 0:L - lag], scalar=hf[:, lag:lag + 1], in1=acc[:, lag:L],
                        op0=ALU.mult, op1=ALU.add), reads=[bu, bhf], writes=[bacc])
                    P.op("dve", lambda e, lag=lag: e.scalar_tensor_tensor(
                        out=acc[:, 0:L - lag], in0=u[:, lag:L], scalar=hb_[:, lag:lag + 1], in1=acc[:, 0:L - lag],
                        op0=ALU.mult, op1=ALU.add), reads=[bu, bhb], writes=[bacc])
                if o == 0:
                    P.op("dve", lambda e: e.tensor_tensor(out=u[:], in0=acc[:], in1=x[:], op=ALU.mult),
                         reads=[bacc, bx], writes=[bu])
                else:
                    P.op("dve", lambda e: e.tensor_tensor(out=zo[:], in0=acc[:], in1=x[:], op=ALU.mult),
                         reads=[bacc, bx], writes=[bzo])
                    P.dma("sp", K.z2T[cc], zo[:], reads=[bzo])
        P.phase_end()


def phase_final(K):
    nc, P = K.nc, K.P
    with contextlib.ExitStack() as st:
        g = sb(st, nc, "fn_g", [128, 8], F32)
        hin = Rot([sb(st, nc, f"fn_in{i}", [128, 8, 512], F32) for i in range(2)])
        sq = Rot([sb(st, nc, f"fn_sq{i}", [128, 8, 512], BF16) for i in range(2)])
        sd = Rot([sb(st, nc, f"fn_sd{i}", [128, 512], F32) for i in range(2)])
        hn = Rot([sb(st, nc, f"fn_hn{i}", [128, 8, 512], F32) for i in range(2)])
        outr = Rot([sb(st, nc, f"fn_o{i}", [128, 4, 1024], F32) for i in range(2)])
        psr = ps_banks(st, nc, 2, "fnps")
        tpr = ps_banks(st, nc, 4, "fntp")
        bg = Buf()
        P.dma("sp", g[:], K.finalg_l, writes=[bg])
        n = 0
        for grp in range(L // 512):
            h, hb = hin.next()
            P.dma("sp", h[:], K.hT[:, :, grp * 512:(grp + 1) * 512].rearrange("c p t -> p c t"), writes=[hb])
            q, qb = sq.next()
            P.op("act", lambda e, q=q, h=h: e.activation(out=q[:], in_=h[:], func=AF.Square), reads=[hb], writes=[qb])
            ps, pb = psr.next()
            for dc in range(8):
                P.op("pe", lambda e, ps=ps, q=q, dc=dc: e.matmul(ps[:, :], lhsT=K.onesb[:], rhs=q[:, dc, :],
                                                               start=(dc == 0), stop=(dc == 7)), reads=[qb], writes=[pb])
            d_, db = sd.next()
            P.op("act", lambda e, d_=d_, ps=ps: e.activation(out=d_[:], in_=ps[:, :], func=AF.Sqrt, bias=K.epsc[:, 0:1],
                                                            scale=1.0 / D), reads=[pb], writes=[db])
            P.op("dve", lambda e, d_=d_: e.reciprocal(out=d_[:], in_=d_[:]), reads=[db], writes=[db])
            hn_, hnb = hn.next()
            for dc in range(8):
                P.op("dve", lambda e, hn_=hn_, h=h, d_=d_, dc=dc: e.scalar_tensor_tensor(
                    out=hn_[:, dc, :], in0=h[:, dc, :], scalar=g[:, dc:dc + 1], in1=d_[:], op0=ALU.mult, op1=ALU.mult),
                    reads=[hb, db, bg], writes=[hnb])
            ot, otb = outr.next()
            for a in range(4):
                for half in range(2):
                    tp, tpb = tpr.next()
                    for j in range(4):
                        dc = half * 4 + j
                        P.op("pe", lambda e, tp=tp, hn_=hn_, a=a, j=j, dc=dc: e.transpose(
                            out=tp[:, j * 128:(j + 1) * 128], in_=hn_[:, dc, a * 128:(a + 1) * 128], identity=K.ident[:]),
                            reads=[hnb], writes=[tpb])
                    if n % 2 == 0:
                        P.op("act", lambda e, tp=tp, ot=ot, a=a, half=half: e.activation(
                            out=ot[:, a, half * 512:(half + 1) * 512], in_=tp[:, :], func=AF.Copy), reads=[tpb], writes=[otb])
                    else:
                        P.op("dve", lambda e, tp=tp, ot=ot, a=a, half=half: e.tensor_copy(
                            out=ot[:, a, half * 512:(half + 1) * 512], in_=tp[:, :]), reads=[tpb], writes=[otb])
                    n += 1
            P.dma("sp", K.out[grp * 512:(grp + 1) * 512, :].rearrange("(a p) d -> p a d", p=128), ot[:], reads=[otb])
        P.phase_end()


def fft_consts():
    a = np.arange(128, dtype=np.float64)
    th = 2 * np.pi * np.outer(a, a) / 128.0
    C = np.cos(th)
    S = np.sin(th)
    tw = 2 * np.pi * np.outer(a, a) / 16384.0
    cw, sw = np.cos(tw), np.sin(tw)
    mats = np.stack([C, S, -S, -C], axis=1)
    wide = np.stack([np.concatenate([C, -S], 1), np.concatenate([C, S], 1), np.concatenate([-S, C], 1)], axis=1)
    twt = np.stack([np.concatenate([cw, cw], 1), np.concatenate([sw, -sw], 1)], axis=1)
    return {"fft_mats": mats.astype(np.float32), "fft_wide": wide.astype(np.float32), "fft_tw": twt.astype(np.float32)}


def phase_hyconv_fft(K):
    nc, P = K.nc, K.P
    INV_N = 1.0 / 16384.0
    with contextlib.ExitStack() as st:
        mats = sb(st, nc, "hx_mats", [128, 4, 128], BF16)
        wide = sb(st, nc, "hx_wide", [128, 3, 256], BF16)
        tw = sb(st, nc, "hx_tw", [128, 2, 256], F32)
        sk = sb(st, nc, "hx_sk", [64, 2, 16], F32)
        FMu = sb(st, nc, "hx_FMu", [64, L], F32)
        FMa = sb(st, nc, "hx_FMa", [64, L], F32)
        FMb = sb(st, nc, "hx_FMb", [64, L], F32)
        Tu = sb(st, nc, "hx_Tu", [64, 128, 64], BF16)
        Tf = sb(st, nc, "hx_Tf", [64, 128, 64], BF16)
        Tb = sb(st, nc, "hx_Tb", [64, 128, 64], BF16)
        Ytok = sb(st, nc, "hx_Ytok", [64, 64, 128], F32)
        tAr = Rot([sb(st, nc, f"hx_tA{i}", [128, 4, 256], F32) for i in range(1)])
        tBr = Rot([sb(st, nc, f"hx_tB{i}", [128, 4, 256], F32) for i in range(1)])
        Zr_ = Rot([sb(st, nc, f"hx_Z{i}", [128, 4, 256], BF16) for i in range(2)])
        Hr_ = Rot([sb(st, nc, f"hx_H{i}", [128, 2, 512], F32) for i in range(1)])
        prr = Rot([sb(st, nc, f"hx_pr{i}", [128, 512], F32) for i in range(3)])
        Pbr = Rot([sb(st, nc, f"hx_Pb{i}", [128, 2, 512], BF16) for i in range(1)])
        psA = ps_banks(st, nc, 4, "hxA")
        psB = ps_banks(st, nc, 3, "hxB")
        psC = ps_banks(st, nc, 1, "hxC")
        bC, bu, ba, bb, bTu, bTf, bTb, bY, bzo = [Buf() for _ in range(9)]
        P.dma("pool", mats[:], K.fft_mats, writes=[bC])
        P.dma("pool", wide[:], K.fft_wide, writes=[bC])
        P.dma("sp", tw[:], K.fft_tw, writes=[bC])
        P.dma("sp", sk[:], K.hy_skip_h, writes=[bC])
        Cm, Sm, NSm, NCm = mats[:, 0, :], mats[:, 1, :], mats[:, 2, :], mats[:, 3, :]
        F1, IFA, IFB = wide[0:64, 0, :], wide[:, 1, :], wide[:, 2, :]
        twA = tw[:, 0, :].unsqueeze(1).to_broadcast([128, 4, 256])
        swb = tw[:, 1, 0:128].unsqueeze(1).to_broadcast([128, 4, 128])
        nswb = tw[:, 1, 128:256].unsqueeze(1).to_broadcast([128, 4, 128])
        evn = [0]

        def to_tok(src, sbuf_, dst, dbuf):
            srcv = src[:].rearrange("c (n1 n2) -> c n2 n1", n2=128)
            for g8 in range(16):
                ps, pb = psA.next()
                for j in range(8):
                    n2 = g8 * 8 + j
                    P.op("pe", lambda e, ps=ps, j=j, n2=n2: e.transpose(
                        out=ps[0:64, j * 64:(j + 1) * 64], in_=srcv[:, n2, :], identity=K.ident[0:64, 0:64]),
                        reads=[sbuf_], writes=[pb])
                evn[0] += 1
                o_ = dst[:, g8 * 8:(g8 + 1) * 8, :]
                i_ = ps[0:64, :].rearrange("p (j c) -> p j c", c=64)
                if evn[0] % 2 == 0:
                    P.op("act", lambda e, o_=o_, i_=i_: e.activation(out=o_, in_=i_, func=AF.Copy), reads=[pb], writes=[dbuf])
                else:
                    P.op("dve", lambda e, o_=o_, i_=i_: e.tensor_copy(out=o_, in_=i_), reads=[pb], writes=[dbuf])

        def twiddle(ps2, pbufs, conj):
            tA, tAb = tAr.next()
            tB, tBb = tBr.next()
            Z, Zb = Zr_.next()
            for half in range(2):
                ps = ps2[half]
                yv = ps[:, :].rearrange("p (c w) -> p c w", w=256)
                sl = slice(half * 2, half * 2 + 2)
                P.op("dve", lambda e, yv=yv, sl=sl: e.tensor_tensor(out=tA[:, sl, :], in0=yv, in1=twA[:, 0:2, :], op=ALU.mult),
                     reads=[pbufs[half], bC], writes=[tAb])
                s1, s2 = (swb, nswb) if not conj else (nswb, swb)
                P.op("dve", lambda e, yv=yv, sl=sl, s1=s1: e.tensor_tensor(out=tB[:, sl, 0:128], in0=yv[:, :, 128:256],
                                                                        in1=s1[:, 0:2, :], op=ALU.mult),
                     reads=[pbufs[half], bC], writes=[tBb])
                P.op("dve", lambda e, yv=yv, sl=sl, s2=s2: e.tensor_tensor(out=tB[:, sl, 128:256], in0=yv[:, :, 0:128],
                                                                        in1=s2[:, 0:2, :], op=ALU.mult),
                     reads=[pbufs[half], bC], writes=[tBb])
            P.op("pool", lambda e: e.tensor_tensor(out=Z[:], in0=tA[:], in1=tB[:], op=ALU.add), reads=[tAb, tBb], writes=[Zb])
            return Z, Zb

        def fwd(T, Tbuf, g4):
            ps2, pbufs = [], []
            for half in range(2):
                ps, pb = psA.next()
                for j in range(2):
                    c = g4 * 4 + half * 2 + j
                    P.op("pe", lambda e, ps=ps, j=j, c=c: e.matmul(ps[:, j * 256:(j + 1) * 256], lhsT=T[:, :, c], rhs=F1,
                                                                   start=True, stop=True), reads=[Tbuf, bC], writes=[pb])
                ps2.append(ps)
                pbufs.append(pb)
            return twiddle(ps2, pbufs, False)

        def stage3(terms):
            xr, xrb = psB.next()
            xi, xib = psB.next()
            n = len(terms)
            for t, (Z, Zb, sg) in enumerate(terms):
                Zv = Z[:].rearrange("p c (two k) -> p c two k", two=2)
                P.op("pe", lambda e, Zv=Zv, t=t: e.matmul(xr[:, :], lhsT=Cm, rhs=Zv[:, :, 0, :], start=(t == 0), stop=False),
                     reads=[Zb, bC], writes=[xrb])
                P.op("pe", lambda e, Zv=Zv, t=t: e.matmul(xr[:, :], lhsT=Sm, rhs=Zv[:, :, 1, :], start=False, stop=(t == n - 1)),
                     reads=[Zb, bC], writes=[xrb])
            for t, (Z, Zb, sg) in enumerate(terms):
                Zv = Z[:].rearrange("p c (two k) -> p c two k", two=2)
                m1, m2 = (Cm, NSm) if sg > 0 else (NCm, Sm)
                P.op("pe", lambda e, Zv=Zv, t=t, m1=m1: e.matmul(xi[:, :], lhsT=m1, rhs=Zv[:, :, 1, :], start=(t == 0), stop=False),
                     reads=[Zb, bC], writes=[xib])
                P.op("pe", lambda e, Zv=Zv, t=t, m2=m2: e.matmul(xi[:, :], lhsT=m2, rhs=Zv[:, :, 0, :], start=False, stop=(t == n - 1)),
                     reads=[Zb, bC], writes=[xib])
            return (xr, xrb), (xi, xib)

        for hc in range(getattr(K, 'hy_halfchunks', 16)):
            cc, p0 = hc // 2, (hc % 2) * 64
            P.dma("sp", FMu[:], K.zT[cc, p0:p0 + 64, :], writes=[bu])
            for o in range(2):
                P.dma("sp", FMa[:], K.filtT[o, 0, cc, p0:p0 + 64, :], writes=[ba])
                P.dma("sp", FMb[:], K.filtT[o, 1, cc, p0:p0 + 64, :], writes=[bb])
                P.op("pool", lambda e: e.memset(FMb[:, 0:1], 0.0), writes=[bb])
                to_tok(FMa, ba, Tf, bTf)
                to_tok(FMb, bb, Tb, bTb)
                to_tok(FMu, bu, Tu, bTu)
                P.dma("sp", FMa[:], K.zT[8 * (o + 1) + cc, p0:p0 + 64, :], reads=[bTf], writes=[ba])
                for g4 in range(16):
                    Zf, Zfb = fwd(Tf, bTf, g4)
                    Zb_, Zbb = fwd(Tb, bTb, g4)
                    (hr, hrb), (hi, hib) = stage3([(Zf, Zfb, 1), (Zb_, Zbb, -1)])
                    H, Hb = Hr_.next()
                    P.op("act", lambda e, H=H, hr=hr: e.activation(out=H[:, 0, :], in_=hr[:, :], func=AF.Copy), reads=[hrb], writes=[Hb])
                    P.op("act", lambda e, H=H, hi=hi: e.activation(out=H[:, 1, :], in_=hi[:, :], func=AF.Copy), reads=[hib], writes=[Hb])
                    Zu, Zub = fwd(Tu, bTu, g4)
                    (xr, xrb), (xi, xib) = stage3([(Zu, Zub, 1)])
                    Pb, Pbb = Pbr.next()
                    for (oi, a0, h0, a1, h1, op_) in ((0, 0, 0, 1, 1, ALU.subtract), (1, 0, 1, 1, 0, ALU.add)):
                        t1, t1b = prr.next()
                        t2, t2b = prr.next()
                        P.op("dve", lambda e, t1=t1, xr=xr, H=H, h0=h0: e.tensor_tensor(out=t1[:], in0=xr[:, :], in1=H[:, h0, :], op=ALU.mult),
                             reads=[xrb, Hb], writes=[t1b])
                        P.op("dve", lambda e, t2=t2, xi=xi, H=H, h1=h1: e.tensor_tensor(out=t2[:], in0=xi[:, :], in1=H[:, h1, :], op=ALU.mult),
                             reads=[xib, Hb], writes=[t2b])
                        P.op("pool", lambda e, t1=t1, t2=t2, Pb=Pb, oi=oi, op_=op_: e.tensor_tensor(out=Pb[:, oi, :], in0=t1[:], in1=t2[:], op=op_),
                             reads=[t1b, t2b], writes=[Pbb])
                    q2, qb2 = [], []
                    for half in range(2):
                        ps, pb = psA.next()
                        for j in range(2):
                            c4 = half * 2 + j
                            P.op("pe", lambda e, ps=ps, j=j, c4=c4, Pb=Pb: e.matmul(
                                ps[:, j * 256:(j + 1) * 256], lhsT=Pb[:, 0, c4 * 128:(c4 + 1) * 128], rhs=IFA, start=True, stop=False),
                                reads=[Pbb, bC], writes=[pb])
                            P.op("pe", lambda e, ps=ps, j=j, c4=c4, Pb=Pb: e.matmul(
                                ps[:, j * 256:(j + 1) * 256], lhsT=Pb[:, 1, c4 * 128:(c4 + 1) * 128], rhs=IFB, start=False, stop=True),
                                reads=[Pbb, bC], writes=[pb])
                        q2.append(ps)
                        qb2.append(pb)
                    R, Rb = twiddle(q2, qb2, True)
                    Rv = R[:].rearrange("p c (two k) -> p c two k", two=2)
                    yp, ypb = psC.next()
                    P.op("pe", lambda e, Rv=Rv: e.matmul(yp[0:64, :], lhsT=Cm[:, 0:64], rhs=Rv[:, :, 0, :], start=True, stop=False),
                         reads=[Rb, bC], writes=[ypb])
                    P.op("pe", lambda e, Rv=Rv: e.matmul(yp[0:64, :], lhsT=NSm[:, 0:64], rhs=Rv[:, :, 1, :], start=False, stop=True),
                         reads=[Rb, bC], writes=[ypb])
                    P.op("act", lambda e, g4=g4: e.activation(
                        out=Ytok[:, g4 * 4:(g4 + 1) * 4, :], in_=yp[0:64, :].rearrange("p (c n) -> p c n", n=128),
                        func=AF.Copy, scale=INV_N), reads=[ypb], writes=[bY])
                fmv = FMb[:].rearrange("c (n1 n2) -> c n2 n1", n2=128)
                for g8 in range(16):
                    ps, pb = psA.next()
                    for j in range(8):
                        n2 = g8 * 8 + j
                        P.op("pe", lambda e, ps=ps, j=j, n2=n2: e.transpose(
                            out=ps[0:64, j * 64:(j + 1) * 64], in_=Ytok[:, :, n2], identity=K.ident[0:64, 0:64]),
                            reads=[bY], writes=[pb])
                    o_ = fmv[:, g8 * 8:(g8 + 1) * 8, :]
                    i_ = ps[0:64, :].rearrange("p (j n) -> p j n", n=64)
                    P.op("act", lambda e, o_=o_, i_=i_: e.activation(out=o_, in_=i_, func=AF.Copy), reads=[pb], writes=[bb])
                P.op("dve", lambda e, o=o, hc=hc: e.scalar_tensor_tensor(
                    out=FMb[:], in0=FMu[:], scalar=sk[:, o, hc:hc + 1], in1=FMb[:], op0=ALU.mult, op1=ALU.add),
                    reads=[bu, bC], writes=[bb])
                if o == 0:
                    P.op("dve", lambda e: e.tensor_tensor(out=FMu[:], in0=FMb[:], in1=FMa[:], op=ALU.mult),
                         reads=[bb, ba, bTu], writes=[bu])
                else:
                    P.op("dve", lambda e: e.tensor_tensor(out=FMb[:], in0=FMb[:], in1=FMa[:], op=ALU.mult),
                         reads=[bb, ba], writes=[bb])
                    for c0 in range(0, L, 2048):
                        P.dma("pool", K.z2T[cc, p0:p0 + 64, c0:c0 + 2048], FMb[:, c0:c0 + 2048], reads=[bb])
        P.phase_end()


STAGES = ["xt", "mod", "norm0", "dnin", "dnscan", "dnout", "peer0", "hyena", "peer1", "final"]
_CACHE = {}


def kernel(**inputs):
    inputs = {k: np.asarray(v) for k, v in inputs.items()}
    if "nc" not in _CACHE:
        _CACHE["nc"] = build(STAGES)[0]
    nc = _CACHE["nc"]
    shared = host_inputs(inputs, 0)
    in_maps = []
    for b in range(8):
        m = dict(shared)
        m["x"] = np.ascontiguousarray(inputs["x"][b])
        m["ctx"] = np.ascontiguousarray(inputs["ctx"][b])
        m["c_l"] = np.ascontiguousarray(inputs["c"][b].reshape(8, 128).T)
        in_maps.append(m)
    res = run_bass_kernel_spmd(nc, in_maps, core_ids=list(range(8)))
    out = np.stack([np.asarray(res.results[b]["out"]) for b in range(8)], axis=0)
    return out.astype(np.float32)
```
